# Optimizing a Trainium2 kernel written in Bass

```python
import math
import jax, jax.numpy as jnp
from jax import lax
import numpy as np


D_MODEL = 2048
BATCH = 2
SEQ = 8192
DEPTH = 4

HEAD_DIM = 128
HEADS_PER_GROUP = 4
DILATION_GROUPS = ((128, 1), (512, 4), (2048, 16))
N_ATTN_HEADS = HEADS_PER_GROUP * len(DILATION_GROUPS)
ATTN_WIDTH = N_ATTN_HEADS * HEAD_DIM
ATTN_OUT = HEADS_PER_GROUP * HEAD_DIM
ROT_DIM = HEAD_DIM // 4
ROPE_THETA = 500000.0

SSM_WIDTH = D_MODEL // 2
SSM_GROUP = 16
SSM_GROUPS = SSM_WIDTH // SSM_GROUP
SSM_STATE = 64
SSM_DT_MIN = 0.001
SSM_DT_MAX = 0.1

N_BRANCHES = 2
N_IN = 3 * ATTN_WIDTH + SSM_WIDTH + N_BRANCHES * D_MODEL
IN_SPLITS = (ATTN_WIDTH, 2 * ATTN_WIDTH, 3 * ATTN_WIDTH, 3 * ATTN_WIDTH + SSM_WIDTH)

N_EXPERTS = 16
EXPERT_FF = D_MODEL // 2
CAPACITY_FACTOR = 2

PLE_DIM = 256

NORM_EPS = 1e-6
MASK_VALUE = -1e30

kernel_name = "hybrid_dilated_attn_s5_ec_moe_block"


def rms_norm(x, gain):
    xf = x.astype(jnp.float32)
    var = jnp.mean(xf * xf, axis=-1, keepdims=True)
    return (xf * lax.rsqrt(var + NORM_EPS) * gain.astype(jnp.float32)).astype(x.dtype)


def partial_rope(x, positions):
    half = ROT_DIM // 2
    inv_freq = jnp.power(ROPE_THETA, -jnp.arange(half, dtype=jnp.float32) * 2.0 / ROT_DIM)
    ang = positions.astype(jnp.float32)[..., None] * inv_freq
    cos = jnp.cos(ang)[:, :, None, :]
    sin = jnp.sin(ang)[:, :, None, :]
    xf = x.astype(jnp.float32)
    x1 = xf[..., :half]
    x2 = xf[..., half:ROT_DIM]
    out = jnp.concatenate([x1 * cos - x2 * sin, x2 * cos + x1 * sin, xf[..., ROT_DIM:]], axis=-1)
    return out.astype(x.dtype)


def dilated_window_attention(q, k, v, window, dilation):
    b, s, h, hd = q.shape
    n_side = (window // 2) // dilation
    blk = n_side
    sub_len = s // dilation
    nb = -(-sub_len // blk)
    pad_end = nb * blk - sub_len
    scale = HEAD_DIM ** -0.5

    def to_sub(t):
        return t.reshape(b, sub_len, dilation, h, hd).transpose(0, 2, 3, 1, 4)

    def key_windows(t):
        tp = jnp.pad(t, ((0, 0), (0, 0), (0, 0), (blk, pad_end + blk), (0, 0)))
        tp = tp.reshape(b, dilation, h, nb + 2, blk, hd)
        return jnp.concatenate([tp[:, :, :, :-2], tp[:, :, :, 1:-1], tp[:, :, :, 2:]], axis=4)

    qs = to_sub(q)
    qb = jnp.pad(qs, ((0, 0), (0, 0), (0, 0), (0, pad_end), (0, 0))).reshape(b, dilation, h, nb, blk, hd)
    kw = key_windows(to_sub(k))
    vw = key_windows(to_sub(v))

    q_pos = jnp.arange(nb)[:, None] * blk + jnp.arange(blk)[None, :]
    k_pos = jnp.arange(nb)[:, None] * blk - blk + jnp.arange(3 * blk)[None, :]
    dist = q_pos[:, :, None] - k_pos[:, None, :]
    valid = (jnp.abs(dist) <= n_side) & (k_pos[:, None, :] >= 0) & (k_pos[:, None, :] < sub_len)

    scores = jnp.einsum('bdhcqe,bdhcke->bdhcqk', qb, kw).astype(jnp.float32) * scale
    scores = jnp.where(valid, scores, MASK_VALUE)
    m = jnp.max(scores, axis=-1, keepdims=True)
    e = jnp.exp(scores - m)
    den = jnp.sum(e, axis=-1, keepdims=True)
    out = jnp.einsum('bdhcqk,bdhcke->bdhcqe', e / den, vw.astype(jnp.float32))
    lse = (m + jnp.log(den))[..., 0]

    out = out.reshape(b, dilation, h, nb * blk, hd)[:, :, :, :sub_len]
    out = out.transpose(0, 3, 1, 2, 4).reshape(b, s, h, hd)
    lse = lse.reshape(b, dilation, h, nb * blk)[:, :, :, :sub_len]
    lse = lse.transpose(0, 3, 1, 2).reshape(b, s, h)
    return out, lse


def attention_branch(q, k, v, q_gain, k_gain, positions):
    bsz, s = q.shape[0], q.shape[1]
    q = partial_rope(rms_norm(q, q_gain), positions)
    k = partial_rope(rms_norm(k, k_gain), positions)
    outs, lses = [], []
    for g, (window, dilation) in enumerate(DILATION_GROUPS):
        sl = slice(g * HEADS_PER_GROUP, (g + 1) * HEADS_PER_GROUP)
        o, l = dilated_window_attention(q[:, :, sl], k[:, :, sl], v[:, :, sl], window, dilation)
        outs.append(o)
        lses.append(l)
    w = jax.nn.softmax(jnp.stack(lses), axis=0)
    out = jnp.sum(w[..., None] * jnp.stack(outs), axis=0)
    return out.reshape(bsz, s, ATTN_OUT).astype(v.dtype)


def _complex_linear_combine(earlier, later):
    ar1, ai1, br1, bi1 = earlier
    ar2, ai2, br2, bi2 = later
    return (ar2 * ar1 - ai2 * ai1,
            ar2 * ai1 + ai2 * ar1,
            ar2 * br1 - ai2 * bi1 + br2,
            ar2 * bi1 + ai2 * br1 + bi2)


def s5_scan_direction(u, a_re, a_im, log_dt, b_re, b_im, c_re, c_im):
    f32 = jnp.float32
    a_re, a_im = a_re.astype(f32), a_im.astype(f32)
    b_re, b_im = b_re.astype(f32), b_im.astype(f32)
    dt = jnp.exp(log_dt.astype(f32))[:, None]
    mag = jnp.exp(a_re * dt)
    ang = a_im * dt
    lb_re = mag * jnp.cos(ang)
    lb_im = mag * jnp.sin(ang)
    den = a_re * a_re + a_im * a_im
    nr = lb_re - 1.0
    ni = lb_im
    coef_re = ((nr * a_re + ni * a_im) / den)[..., None]
    coef_im = ((ni * a_re - nr * a_im) / den)[..., None]
    bb_re = coef_re * b_re - coef_im * b_im
    bb_im = coef_re * b_im + coef_im * b_re
    bu_re = jnp.einsum('bsgh,gph->bsgp', u, bb_re)
    bu_im = jnp.einsum('bsgh,gph->bsgp', u, bb_im)
    ar = jnp.broadcast_to(lb_re, bu_re.shape)
    ai = jnp.broadcast_to(lb_im, bu_re.shape)
    _, _, x_re, x_im = lax.associative_scan(_complex_linear_combine, (ar, ai, bu_re, bu_im), axis=1)
    return (jnp.einsum('bsgp,ghp->bsgh', x_re, c_re.astype(f32))
            - jnp.einsum('bsgp,ghp->bsgh', x_im, c_im.astype(f32)))


def ssm_branch(u, d_skip, a_re, a_im, log_dt, b_re, b_im, c_re, c_im):
    bsz, s, _ = u.shape
    uf = u.astype(jnp.float32)
    ug = uf.reshape(bsz, s, SSM_GROUPS, SSM_GROUP)
    y_fwd = s5_scan_direction(ug, a_re[0], a_im[0], log_dt[0], b_re[0], b_im[0], c_re[0], c_im[0])
    y_bwd = jnp.flip(s5_scan_direction(jnp.flip(ug, axis=1), a_re[1], a_im[1], log_dt[1],
                                       b_re[1], b_im[1], c_re[1], c_im[1]), axis=1)
    return (y_fwd + y_bwd).reshape(bsz, s, SSM_WIDTH) + d_skip.astype(jnp.float32) * uf


def expert_choice_ffn(x, w_router, w_gate, w_up, w_down):
    bsz, s, d = x.shape
    capacity = CAPACITY_FACTOR * s // N_EXPERTS
    logits = jnp.einsum('bsd,de->bse', x, w_router).astype(jnp.float32)
    affinity = jax.nn.softmax(logits, axis=-1)
    gate, idx = lax.top_k(jnp.swapaxes(affinity, 1, 2), capacity)
    xg = jax.vmap(lambda xb, ib: xb[ib])(x, idx)
    hid = (jax.nn.silu(jnp.einsum('becd,edf->becf', xg, w_gate))
           * jnp.einsum('becd,edf->becf', xg, w_up))
    yg = jnp.einsum('becf,efd->becd', hid, w_down) * gate[..., None].astype(x.dtype)
    return jax.vmap(lambda yb, ib: jnp.zeros((s, d), x.dtype).at[ib.reshape(-1)].add(yb.reshape(-1, d)))(yg, idx)


def setup_inputs(seed: int = 0) -> dict:
    key = jax.random.key(seed)
    ks = jax.random.split(key, 26)
    f32 = jnp.float32
    L = DEPTH

    def normal(k, shape, scale):
        return jax.random.normal(k, shape, f32) * scale

    x = normal(ks[0], (BATCH, SEQ, D_MODEL), 1.0)
    p = normal(ks[1], (DEPTH, BATCH, SEQ, PLE_DIM), 1.0)
    positions = (jnp.arange(SEQ, dtype=jnp.int32)[None, :]
                 + jax.random.randint(ks[2], (BATCH, 1), 0, 1024, dtype=jnp.int32))
    norm_mix = 1.0 + normal(ks[3], (L, D_MODEL), 0.02)
    w_in = normal(ks[4], (L, D_MODEL, N_IN), D_MODEL ** -0.5)
    q_norm = 1.0 + normal(ks[5], (L, HEAD_DIM), 0.02)
    k_norm = 1.0 + normal(ks[6], (L, HEAD_DIM), 0.02)
    w_attn_br = normal(ks[7], (L, ATTN_OUT, D_MODEL), ATTN_OUT ** -0.5)
    n_idx = jnp.arange(SSM_STATE, dtype=f32)
    ssm_a_re = -0.5 + normal(ks[8], (L, 2, SSM_GROUPS, SSM_STATE), 0.01)
    ssm_a_im = jnp.pi * n_idx + normal(ks[9], (L, 2, SSM_GROUPS, SSM_STATE), 0.01)
    ssm_log_dt = jax.random.uniform(ks[10], (L, 2, SSM_GROUPS), f32,
                                    math.log(SSM_DT_MIN), math.log(SSM_DT_MAX))
    ssm_b_re = normal(ks[11], (L, 2, SSM_GROUPS, SSM_STATE, SSM_GROUP), (2 * SSM_GROUP) ** -0.5)
    ssm_b_im = normal(ks[12], (L, 2, SSM_GROUPS, SSM_STATE, SSM_GROUP), (2 * SSM_GROUP) ** -0.5)
    ssm_c_re = normal(ks[13], (L, 2, SSM_GROUPS, SSM_GROUP, SSM_STATE), 0.5)
    ssm_c_im = normal(ks[14], (L, 2, SSM_GROUPS, SSM_GROUP, SSM_STATE), 0.5)
    ssm_d = normal(ks[15], (L, SSM_WIDTH), 1.0)
    w_ssm_br = normal(ks[16], (L, SSM_WIDTH, 2 * D_MODEL), SSM_WIDTH ** -0.5)
    w_out = normal(ks[17], (L, D_MODEL, D_MODEL), D_MODEL ** -0.5)
    norm_ffn = 1.0 + normal(ks[18], (L, D_MODEL), 0.02)
    w_router = normal(ks[19], (L, D_MODEL, N_EXPERTS), D_MODEL ** -0.5)
    w_exp_gate = normal(ks[20], (L, N_EXPERTS, D_MODEL, EXPERT_FF), D_MODEL ** -0.5)
    w_exp_up = normal(ks[21], (L, N_EXPERTS, D_MODEL, EXPERT_FF), D_MODEL ** -0.5)
    w_exp_down = normal(ks[22], (L, N_EXPERTS, EXPERT_FF, D_MODEL), EXPERT_FF ** -0.5)
    norm_ple = 1.0 + normal(ks[23], (L, D_MODEL), 0.02)
    w_ple_gate = normal(ks[24], (L, D_MODEL, D_MODEL), D_MODEL ** -0.5)
    w_ple_proj = normal(ks[25], (L, PLE_DIM, D_MODEL), PLE_DIM ** -0.5)
    return {"x": x, "p": p, "positions": positions, "norm_mix": norm_mix, "w_in": w_in,
            "q_norm": q_norm, "k_norm": k_norm, "w_attn_br": w_attn_br,
            "ssm_a_re": ssm_a_re, "ssm_a_im": ssm_a_im, "ssm_log_dt": ssm_log_dt,
            "ssm_b_re": ssm_b_re, "ssm_b_im": ssm_b_im, "ssm_c_re": ssm_c_re, "ssm_c_im": ssm_c_im,
            "ssm_d": ssm_d, "w_ssm_br": w_ssm_br, "w_out": w_out, "norm_ffn": norm_ffn,
            "w_router": w_router, "w_exp_gate": w_exp_gate, "w_exp_up": w_exp_up,
            "w_exp_down": w_exp_down, "norm_ple": norm_ple, "w_ple_gate": w_ple_gate,
            "w_ple_proj": w_ple_proj}


def reference(x, p, positions, norm_mix, w_in, q_norm, k_norm, w_attn_br,
              ssm_a_re, ssm_a_im, ssm_log_dt, ssm_b_re, ssm_b_im, ssm_c_re, ssm_c_im,
              ssm_d, w_ssm_br, w_out, norm_ffn, w_router, w_exp_gate, w_exp_up,
              w_exp_down, norm_ple, w_ple_gate, w_ple_proj):
    bsz, s, d = x.shape
    h = x
    for l in range(DEPTH):
        xn = rms_norm(h, norm_mix[l])
        z = xn @ w_in[l]
        q, k, v, u, gate_logits = jnp.split(z, IN_SPLITS, axis=-1)
        q = q.reshape(bsz, s, N_ATTN_HEADS, HEAD_DIM)
        k = k.reshape(bsz, s, N_ATTN_HEADS, HEAD_DIM)
        v = v.reshape(bsz, s, N_ATTN_HEADS, HEAD_DIM)

        a_branch = attention_branch(q, k, v, q_norm[l], k_norm[l], positions) @ w_attn_br[l]

        y = ssm_branch(u, ssm_d[l], ssm_a_re[l], ssm_a_im[l], ssm_log_dt[l],
                       ssm_b_re[l], ssm_b_im[l], ssm_c_re[l], ssm_c_im[l])
        zs = jax.nn.gelu(y).astype(h.dtype) @ w_ssm_br[l]
        s_branch = zs[..., :d] * jax.nn.sigmoid(zs[..., d:])

        g_attn = jax.nn.sigmoid(gate_logits[..., :d])
        g_ssm = jax.nn.sigmoid(gate_logits[..., d:])
        h = h + (g_attn * a_branch + g_ssm * s_branch) @ w_out[l]

        h = h + expert_choice_ffn(rms_norm(h, norm_ffn[l]), w_router[l],
                                  w_exp_gate[l], w_exp_up[l], w_exp_down[l])

        ple = p[l] @ w_ple_proj[l]
        h = h + jax.nn.sigmoid(rms_norm(h, norm_ple[l]) @ w_ple_gate[l]) * ple
    return h
```

```python
import math
import contextlib
import numpy as np
import concourse.bass as bass
import concourse.mybir as mybir
from concourse.bass_utils import run_bass_kernel_spmd


F32 = mybir.dt.float32
BF16 = mybir.dt.bfloat16
I32 = mybir.dt.int32
U32 = mybir.dt.uint32
AF = mybir.ActivationFunctionType
ALU = mybir.AluOpType
AX = mybir.AxisListType


class Prog:
    ENGS = ["pe", "dve", "act", "pool", "sp"]

    def __init__(self, nc, stack, ndma=16):
        self.nc = nc
        self.ins = {e: [] for e in self.ENGS}
        self.sems = {e: stack.enter_context(nc.semaphore("cs_" + e)) for e in self.ENGS}
        self.qslots = {"sp": list(range(0, 10)), "pool": list(range(10, 18)), "act": list(range(18, 20))}
        self.ndma = ndma = 20
        self.dsems = [stack.enter_context(nc.semaphore("ds%d" % i)) for i in range(ndma)]
        self.dcount = [0] * ndma
        self.qnext = {q: 0 for q in self.qslots}
        self.last_w = {}
        self.readers = {}
        self.psum_keys = set()

    def _deps(self, reads, writes):
        deps = []
        for b in reads:
            ev = self.last_w.get(b)
            if ev is not None:
                deps.append(ev)
            if b in self.psum_keys:
                deps.extend(self.readers.get(b, ()))
        for b in writes:
            ev = self.last_w.get(b)
            if ev is not None:
                deps.append(ev)
            deps.extend(self.readers.get(b, ()))
        return deps

    def _update(self, ev, reads, writes):
        for b in reads:
            lst = self.readers.setdefault(b, [])
            if ev[0] == "c":
                lst[:] = [x for x in lst if not (x[0] == "c" and x[1] == ev[1])]
            lst.append(ev)
        for b in writes:
            self.last_w[b] = ev
            self.readers[b] = []

    def op(self, eng, fn, r=(), w=()):
        deps = self._deps(r, w)
        idx = len(self.ins[eng])
        self.ins[eng].append(dict(fn=fn, deps=deps, dma=None, sig=False))
        self._update(("c", eng, idx), r, w)

    def dma(self, q, out, in_, r=(), w=(), **kw):
        fn = lambda e: e.dma_start(out=out, in_=in_, **kw)
        self.dma_fn(q, fn, r, w)

    def dma_fn(self, q, fn, r=(), w=()):
        deps = self._deps(r, w)
        sl = self.qslots[q]
        s = sl[self.qnext[q] % len(sl)]
        self.qnext[q] += 1
        if self.dcount[s] > 0:
            deps.append(("d", s, 16 * self.dcount[s]))
        self.dcount[s] += 1
        tgt = 16 * self.dcount[s]
        self.ins[q].append(dict(fn=fn, deps=deps, dma=(s, tgt), sig=False))
        self._update(("d", s, tgt), r, w)

    def barrier(self):
        evs = []
        for e in self.ENGS:
            n = len(self.ins[e])
            for i in range(n - 1, -1, -1):
                if self.ins[e][i]["dma"] is None and self.ins[e][i]["fn"] is not None:
                    evs.append(("c", e, i))
                    break
        for s in range(self.ndma):
            if self.dcount[s] > 0:
                evs.append(("d", s, 16 * self.dcount[s]))
        for e in self.ENGS:
            self.ins[e].append(dict(fn=None, deps=list(evs), dma=None, sig=False))
        self.last_w = {}
        self.readers = {}

    def emit(self):
        nc = self.nc
        plans = {}
        for e in self.ENGS:
            wc = {x: -1 for x in self.ENGS}
            wd = [0] * self.ndma
            plan = []
            for idx, it in enumerate(self.ins[e]):
                waits = []
                for ev in it["deps"]:
                    if ev[0] == "c":
                        _, e2, i2 = ev
                        if e2 == e and e == "pe":
                            continue
                        if e2 == e and i2 >= idx:
                            continue
                        if wc[e2] >= i2:
                            continue
                        wc[e2] = i2
                        self.ins[e2][i2]["sig"] = True
                        waits.append(("c", e2, i2))
                    else:
                        _, s, tgt = ev
                        if wd[s] >= tgt:
                            continue
                        wd[s] = tgt
                        waits.append(ev)
                plan.append(waits)
            plans[e] = plan
        sigval = {}
        for e in self.ENGS:
            c = 0
            for idx, it in enumerate(self.ins[e]):
                if it["sig"]:
                    c += 1
                    sigval[(e, idx)] = c
        self.sig_totals = {e: sum(1 for it in self.ins[e] if it["sig"]) for e in self.ENGS}

        def run(e, eng):
            for idx, it in enumerate(self.ins[e]):
                for wv in plans[e][idx]:
                    if wv[0] == "c":
                        eng.wait_ge(self.sems[wv[1]], sigval[(wv[1], wv[2])])
                    else:
                        eng.wait_ge(self.dsems[wv[1]], wv[2])
                if it["fn"] is None:
                    continue
                ins = it["fn"](eng)
                if it["dma"] is not None:
                    ins.then_inc(self.dsems[it["dma"][0]], 16)
                elif it["sig"]:
                    ins.then_inc(self.sems[e], 1)

        with nc.Block() as block:
            @block.tensor
            def _(eng):
                run("pe", eng)

            @block.vector
            def _(eng):
                run("dve", eng)

            @block.scalar
            def _(eng):
                run("act", eng)

            @block.gpsimd
            def _(eng):
                run("pool", eng)

            @block.sync
            def _(eng):
                run("sp", eng)

    def finish(self):
        evs = [("d", s, 16 * self.dcount[s]) for s in range(self.ndma) if self.dcount[s] > 0]
        self.ins["sp"].append(dict(fn=None, deps=evs, dma=None, sig=False))


D = 2048
NIN = 9728
HD = 128
NH = 12
EPS = 1e-6
TWO_PI = 2.0 * math.pi


def make_consts():
    c = {}
    c["ident"] = np.eye(128, dtype=np.float32)
    c["ones"] = np.ones((128, 128), dtype=np.float32)
    rm = np.zeros((128, 128), dtype=np.float32)
    for i in range(16):
        rm[i + 16, i] = -1.0
        rm[i, i + 16] = 1.0
    c["rmat"] = rm
    invf = np.zeros((128, 1), dtype=np.float32)
    fr = np.power(np.float32(500000.0), -np.arange(16, dtype=np.float32) * np.float32(2.0) / np.float32(32.0)).astype(np.float32)
    invf[0:16, 0] = fr
    invf[16:32, 0] = fr
    c["invf"] = invf
    kk = np.arange(128)[:, None]
    qq = np.arange(128)[None, :]
    m = np.zeros((128, 3, 128), dtype=np.float32)
    m[:, 0, :] = (kk - qq >= 64)
    m[:, 1, :] = (np.abs(kk - qq) <= 64)
    m[:, 2, :] = (kk - qq <= -64)
    c["amask"] = m.reshape(128, 384)
    c["ltri"] = (np.arange(128)[:, None] < np.arange(128)[None, :]).astype(np.float32)
    return c


def prep_s5(inp):
    L = inp["ssm_a_re"].shape[0]
    o = {}
    def st(a):
        return np.ascontiguousarray(a.reshape(L, 2, 32, 128).transpose(0, 1, 3, 2))
    o["s5_are"] = st(inp["ssm_a_re"])
    o["s5_aim"] = st(inp["ssm_a_im"])
    ldt = np.repeat(inp["ssm_log_dt"][:, :, :, None], 64, axis=3)
    o["s5_ldt"] = st(ldt)
    for nm, src in (("s5_bre", "ssm_b_re"), ("s5_bim", "ssm_b_im")):
        b = inp[src].reshape(L, 2, 32, 2, 64, 16)
        out = np.zeros((L, 2, 32, 32, 128), np.float32)
        for gi in range(2):
            out[:, :, :, gi * 16:(gi + 1) * 16, gi * 64:(gi + 1) * 64] = b[:, :, :, gi].transpose(0, 1, 2, 4, 3)
        o[nm] = out
    for nm, src in (("s5_cre", "ssm_c_re"), ("s5_cim", "ssm_c_im")):
        c = inp[src].reshape(L, 2, 32, 2, 16, 64)
        out = np.zeros((L, 2, 32, 128, 32), np.float32)
        for gi in range(2):
            out[:, :, :, gi * 64:(gi + 1) * 64, gi * 16:(gi + 1) * 16] = c[:, :, :, gi].transpose(0, 1, 2, 4, 3)
        o[nm] = out
    o["s5_d"] = np.ascontiguousarray(inp["ssm_d"].reshape(L, 32, 32, 1))
    return o


class MK:
    def __init__(self, S, L, dbg=False, stop=None):
        self.S, self.L, self.dbg, self.stop = S, L, dbg, stop
        self.TS = min(S, 2048)
        self.nc = nc = bass.Bass("TRN2", target_bir_lowering=False)
        self.stack = contextlib.ExitStack()
        self.P = Prog(nc, self.stack)
        self.din = {}
        self.dscr = {}

    def reg(self, eng, val):
        if not hasattr(self, "_regs"):
            self._regs = {}
        if val not in self._regs:
            self._regs[val] = eng.to_reg(val)
        return self._regs[val]

    def inp(self, name, shape, dt=F32):
        t = self.nc.dram_tensor(name, list(shape), dt, kind="ExternalInput").ap()
        self.din[name] = t
        return t

    def scr(self, name, shape, dt, out=False):
        kind = "ExternalOutput" if (out or self.dbg) else "Internal"
        t = self.nc.dram_tensor(name, list(shape), dt, kind=kind).ap()
        self.dscr[name] = t
        return t

    def sb(self, st, name, shape, dt):
        self._uid = getattr(self, "_uid", 0) + 1
        return st.enter_context(self.nc.sbuf_tensor("%s__%d" % (name, self._uid), list(shape), dt))

    def ps(self, st, name, shape, dt=F32):
        self.P.psum_keys.add(name)
        self._uid = getattr(self, "_uid", 0) + 1
        return st.enter_context(self.nc.psum_tensor("%s__%d" % (name, self._uid), list(shape), dt))

    def declare(self):
        S, L = self.S, self.L
        i = self.inp
        self.x = i("x", [S, D])
        self.pT = i("pT", [L, 256, S])
        self.pos = i("positions", [1, S], I32)
        self.norm_mix = i("norm_mix", [L, D])
        self.w_in = i("w_in", [L, D, NIN])
        self.q_norm = i("q_norm", [L, 128])
        self.k_norm = i("k_norm", [L, 128])
        self.w_attn_br = i("w_attn_br", [L, 512, D])
        self.w_ssm_br = i("w_ssm_br", [L, 1024, 2 * D])
        self.w_out = i("w_out", [L, D, D])
        self.norm_ffn = i("norm_ffn", [L, D])
        self.norm_ple = i("norm_ple", [L, D])
        self.w_ple_gate = i("w_ple_gate", [L, D, D])
        self.w_ple_proj = i("w_ple_proj", [L, 256, D])
        self.w_router = i("w_router", [L, D, 16])
        self.w_exp_gate = i("w_exp_gate", [L, 16, D, 1024])
        self.w_exp_up = i("w_exp_up", [L, 16, D, 1024])
        self.w_exp_down = i("w_exp_down", [L, 16, 1024, D])
        self.c_ltri = i("c_ltri", [128, 128])
        self.s5_are = i("s5_are", [L, 2, 128, 32])
        self.s5_aim = i("s5_aim", [L, 2, 128, 32])
        self.s5_ldt = i("s5_ldt", [L, 2, 128, 32])
        self.s5_bre = i("s5_bre", [L, 2, 32, 32, 128])
        self.s5_bim = i("s5_bim", [L, 2, 32, 32, 128])
        self.s5_cre = i("s5_cre", [L, 2, 32, 128, 32])
        self.s5_cim = i("s5_cim", [L, 2, 32, 128, 32])
        self.s5_d = i("s5_d", [L, 32, 32, 1])
        for n in ["ident", "ones", "rmat"]:
            setattr(self, "c_" + n, i("c_" + n, [128, 128]))
        self.c_invf = i("c_invf", [128, 1])
        self.c_amask = i("c_amask", [128, 384])
        s = self.scr
        self.h = s("h", [S, D], F32, out=True)
        self.cosT = s("cosT", [128, S], F32)
        self.sinT = s("sinT", [128, S], F32)
        self.qkT = s("qkT", [24, 128, S], BF16)
        self.v = s("v", [S, 1536], BF16)
        self.uT = s("uT", [1024, S], BF16)
        self.gT = s("gT", [4096, S], BF16)
        self.attT = s("attT", [512, S], BF16)
        self.ygT = s("ygT", [1024, S], BF16)
        self.mergedT = s("mergedT", [2048, S], BF16)
        self.xrows = s("xrows", [S, 2112], BF16)
        self.xg = [s("xg%d" % e_, [S // 8, 2112], BF16) for e_ in range(16)]
        if self.dbg:
            self.dbg_posi = s("dbg_posi", [128, (S // 128) * 16], I32)
            self.dbg_aff = s("dbg_aff", [128, (S // 128) * 16], F32)
        if self.stop == "s5raw":
            self.dbg_y = s("dbg_y", [1024, S], F32)

    def load_consts(self):
        P, st = self.P, self.stack
        self.ident_b = self.sb(st, "ident_b", [128, 128], BF16)
        self.ones_b = self.sb(st, "ones_b", [128, 128], BF16)
        self.rmat_b = self.sb(st, "rmat_b", [128, 128], BF16)
        self.ident_f = self.sb(st, "ident_f", [128, 128], F32)
        self.ones_f = self.sb(st, "ones_f", [128, 128], F32)
        self.invf = self.sb(st, "invf", [128, 1], F32)
        P.dma("pool", self.ident_b[:], self.c_ident, w=["ident_b"])
        P.dma("pool", self.ones_b[:], self.c_ones, w=["ones_b"])
        P.dma("pool", self.rmat_b[:], self.c_rmat, w=["rmat_b"])
        P.dma("sp", self.ident_f[:], self.c_ident, w=["ident_f"])
        P.dma("sp", self.ones_f[:], self.c_ones, w=["ones_f"])
        P.dma("sp", self.invf[:], self.c_invf, w=["invf"])

    def sin_reduced(self, x, xk, out, ok, ti, tik, tf, tfk, shift):
        P = self.P
        P.op("dve", lambda e: e.tensor_scalar(tf[:], x[:], shift, 1.0 / TWO_PI, ALU.add, ALU.mult), r=[xk], w=[tfk])
        P.op("dve", lambda e: e.tensor_copy(ti[:], tf[:]), r=[tfk], w=[tik])
        P.op("dve", lambda e: e.tensor_copy(tf[:], ti[:]), r=[tik], w=[tfk])
        P.op("dve", lambda e: e.scalar_tensor_tensor(tf[:], tf[:], -TWO_PI, x[:], ALU.mult, ALU.add), r=[tfk, xk], w=[tfk])
        P.op("dve", lambda e: e.tensor_scalar(tf[:], tf[:], shift - math.pi, -2.0 * math.pi + 2 * math.pi, ALU.max, ALU.add) if False else
             e.tensor_scalar(tf[:], tf[:], shift, -math.pi, ALU.add, ALU.max), r=[tfk], w=[tfk])
        P.op("dve", lambda e: e.tensor_scalar(tf[:], tf[:], math.pi, None, ALU.min), r=[tfk], w=[tfk])
        P.op("act", lambda e: e.activation(out=out[:], in_=tf[:], func=AF.Sin), r=[tfk], w=[ok])

    def rope_tables(self):
        P, S = self.P, self.S
        CH = min(S, 2048)
        with contextlib.ExitStack() as st:
            pi_ = self.sb(st, "rp_i", [128, CH], I32)
            pf = self.sb(st, "rp_f", [128, CH], F32)
            m1 = self.sb(st, "rp_m1", [128, CH], F32)
            m2 = self.sb(st, "rp_m2", [128, CH], F32)
            sn = self.sb(st, "rp_sn", [128, CH], F32)
            cs = self.sb(st, "rp_cs", [128, CH], F32)
            for c in range(S // CH):
                sl = slice(c * CH, (c + 1) * CH)
                P.dma("sp", pi_[:], self.pos[0:1, sl].partition_broadcast(128), w=["rp_i"])
                P.op("dve", lambda e: e.tensor_copy(pf[:], pi_[:]), r=["rp_i"], w=["rp_f"])
                P.op("dve", lambda e: e.tensor_scalar(m1[:], pf[:], self.invf[:, 0:1], None, ALU.mult), r=["rp_f", "invf"], w=["rp_m1"])
                self.sin_reduced(m1, "rp_m1", sn, "rp_sn", pi_, "rp_i", m2, "rp_m2", 0.0)
                self.sin_reduced(m1, "rp_m1", cs, "rp_cs", pi_, "rp_i", m2, "rp_m2", math.pi / 2)
                P.dma("sp", self.sinT[:, sl], sn[:], r=["rp_sn"], w=["sinT"])
                P.dma("sp", self.cosT[:, sl], cs[:], r=["rp_cs"], w=["cosT"])
            P.barrier()

    def norm_transpose(self, st, src, T0, ntok, gain_row, hnT, key, pfx):
        P = self.P
        gb = self.sb(st, pfx + "gb", [128, D], F32)
        P.dma("sp", gb[:], gain_row.partition_broadcast(128), w=[pfx + "gb"])
        hb = [self.sb(st, pfx + "hb%d" % i, [128, D], F32) for i in range(2)]
        junk = self.sb(st, pfx + "junk", [128, D], BF16)
        ss = [self.sb(st, pfx + "ss%d" % i, [128, 1], F32) for i in range(2)]
        rs = [self.sb(st, pfx + "rs%d" % i, [128, 1], F32) for i in range(2)]
        xn = [self.sb(st, pfx + "xn%d" % i, [128, D], BF16) for i in range(2)]
        pT = [self.ps(st, pfx + "pT%d" % i, [128, 4, 128], BF16) for i in range(2)]
        for tt in range(ntok // 128):
            b = tt % 2
            hbk, ssk, rsk, xnk = pfx + "hb%d" % b, pfx + "ss%d" % b, pfx + "rs%d" % b, pfx + "xn%d" % b
            t0 = T0 + tt * 128
            P.dma("sp", hb[b][:], src[t0:t0 + 128, :], w=[hbk])
            P.op("act", lambda e, b=b: e.activation(out=junk[:], in_=hb[b][:], func=AF.Square, accum_out=ss[b][:, 0:1]),
                 r=[hbk], w=[pfx + "junk", ssk])
            P.op("act", lambda e, b=b: e.activation(out=rs[b][:], in_=ss[b][:], func=AF.Sqrt, scale=1.0 / D, bias=self.epsb[:, 0:1]), r=[ssk, "epsb"], w=[rsk])
            P.op("dve", lambda e, b=b: e.reciprocal(rs[b][:], rs[b][:]), r=[rsk], w=[rsk])
            P.op("dve", lambda e, b=b: e.scalar_tensor_tensor(xn[b][:], hb[b][:], rs[b][:, 0:1], gb[:], ALU.mult, ALU.mult),
                 r=[hbk, rsk, pfx + "gb"], w=[xnk])
            for g4 in range(4):
                pb = (tt * 4 + g4) % 2
                pk = pfx + "pT%d" % pb
                for j in range(4):
                    kc = g4 * 4 + j
                    P.op("pe", lambda e, b=b, pb=pb, j=j, kc=kc: e.transpose(pT[pb][:, j, :], xn[b][:, kc * 128:(kc + 1) * 128], self.ident_b[:]),
                         r=[xnk, "ident_b"], w=[pk])
                eng = "act" if g4 % 2 == 0 else "dve"
                if eng == "act":
                    P.op("act", lambda e, pb=pb, g4=g4, tt=tt: e.copy(hnT[:, g4 * 4:(g4 + 1) * 4, tt * 128:(tt + 1) * 128], pT[pb][:]),
                         r=[pk], w=[(key, tt)])
                else:
                    P.op("dve", lambda e, pb=pb, g4=g4, tt=tt: e.tensor_copy(hnT[:, g4 * 4:(g4 + 1) * 4, tt * 128:(tt + 1) * 128], pT[pb][:]),
                         r=[pk], w=[(key, tt)])

    def stage_a(self, l):
        P, S, TS = self.P, self.S, self.TS
        nsub = TS // 512
        ntt = TS // 128
        for sup in range(S // TS):
            T0 = sup * TS
            with contextlib.ExitStack() as st:
                hnT = self.sb(st, "a_hnT", [128, 16, TS], BF16)
                with contextlib.ExitStack() as st2:
                    self.norm_transpose(st2, self.h, T0, TS, self.norm_mix[l:l + 1, :], hnT, "a_hnT", "an_")
                    P.barrier()
                if self.stop == "norm":
                    self.dbg_hnT = self.scr("dbg_hnT", [128, 16, TS], BF16)
                    P.dma("sp", self.dbg_hnT, hnT[:], r=[("a_hnT", tt) for tt in range(ntt)], w=["dbg_hnT"])
                    P.barrier()
                    return
                hkeys = [("a_hnT", tt) for tt in range(ntt)]
                cosb = self.sb(st, "a_cos", [128, TS], F32)
                sinb = self.sb(st, "a_sin", [128, TS], F32)
                P.dma("sp", cosb[:], self.cosT[:, T0:T0 + TS], r=["cosT"], w=["a_cos"])
                P.dma("sp", sinb[:], self.sinT[:, T0:T0 + TS], r=["sinT"], w=["a_sin"])
                gq = self.sb(st, "a_gq", [128, 2], F32)
                import os
                DBG = os.environ.get("DBG", "")
                if "nogq" in DBG:
                    P.op("dve", lambda e: e.memset(gq[:], 1.0), w=["a_gq"])
                else:
                    P.dma("sp", gq[:, 0:1], self.q_norm[l:l + 1, :].rearrange("o p -> p o"), w=["a_gq"])
                    P.dma("sp", gq[:, 1:2], self.k_norm[l:l + 1, :].rearrange("o p -> p o"), w=["a_gq"])
                wb = [self.sb(st, "a_wb%d" % i, [128, 16, 256], BF16) for i in range(3)]
                psm = [self.ps(st, "a_ps%d" % i, [128, 512], F32) for i in range(3)]
                ps_ss = self.ps(st, "a_psss", [128, 512], F32)
                ps_rot = self.ps(st, "a_psrot", [128, 512], F32)
                sq = self.sb(st, "a_sq", [128, 512], BF16)
                qg = self.sb(st, "a_qg", [128, 512], BF16)
                rstd = self.sb(st, "a_rstd", [128, 512], F32)
                t1 = self.sb(st, "a_t1", [128, 512], F32)
                t2 = self.sb(st, "a_t2", [128, 512], F32)
                ob = [self.sb(st, "a_ob%d" % i, [128, TS], BF16) for i in range(2)]
                wcount = [0]
                mmcount = [0]
                ocount = [0]

                def load_w(col0, ncols):
                    i = wcount[0] % 3
                    wcount[0] += 1
                    src = self.w_in[l, :, col0:col0 + ncols].rearrange("(kc p) n -> p kc n", p=128)
                    P.dma("pool", wb[i][:, :, 0:ncols], src, w=["a_wb%d" % i])
                    return i

                fm_cols = [(j * 128, "qk", j) for j in range(24)] + \
                          [(4608 + j * 128, "u", j) for j in range(8)] + \
                          [(5632 + j * 128, "g", j) for j in range(32)]
                for pair in range(len(fm_cols) // 2):
                    if self.stop == 'a1' and pair not in (0, 12, 16):
                        continue
                    col0 = fm_cols[2 * pair][0]
                    wi = load_w(col0, 256)
                    for half in range(2):
                        _, kind, j = fm_cols[2 * pair + half]
                        oi = ocount[0] % 2
                        ocount[0] += 1
                        okey = "a_ob%d" % oi
                        for sub in range(nsub):
                            pi = mmcount[0] % 3
                            mmcount[0] += 1
                            pk = "a_ps%d" % pi
                            tsl = slice(sub * 512, (sub + 1) * 512)
                            for kc in range(16):
                                P.op("pe", lambda e, pi=pi, wi=wi, half=half, kc=kc, tsl=tsl: e.matmul(
                                    psm[pi][:], wb[wi][:, kc, half * 128:(half + 1) * 128], hnT[:, kc, tsl],
                                    start=(kc == 0), stop=(kc == 15)),
                                    r=["a_wb%d" % wi] + hkeys[sub * 4:(sub + 1) * 4], w=[pk])
                            if kind == "qk" and "noepi" in DBG:
                                P.op("act", lambda e, pi=pi, oi=oi, tsl=tsl: e.copy(ob[oi][:, tsl], psm[pi][:]), r=[pk], w=[okey])
                            elif kind == "qk":
                                gi = 0 if j < 12 else 1
                                P.op("act", lambda e, pi=pi: e.activation(out=sq[:], in_=psm[pi][:], func=AF.Square), r=[pk], w=["a_sq"])
                                P.op("dve", lambda e, pi=pi, gi=gi: e.tensor_scalar(qg[:], psm[pi][:], gq[:, gi:gi + 1], None, ALU.mult),
                                     r=[pk, "a_gq"], w=["a_qg"])
                                P.op("pe", lambda e: e.matmul(ps_ss[:], self.ones_b[:], sq[:], start=True, stop=True),
                                     r=["ones_b", "a_sq"], w=["a_psss"])
                                P.op("pe", lambda e: e.matmul(ps_rot[:], self.rmat_b[:], qg[:], start=True, stop=True),
                                     r=["rmat_b", "a_qg"], w=["a_psrot"])
                                if "e1" in DBG:
                                    P.op("act", lambda e, oi=oi, tsl=tsl: e.copy(ob[oi][:, tsl], ps_rot[:]), r=["a_psrot"], w=[okey])
                                    P.op("act", lambda e, oi=oi, tsl=tsl: e.copy(t1[:], ps_ss[:]), r=["a_psss"], w=["a_t1"])
                                    continue
                                P.op("act", lambda e: e.activation(out=rstd[:], in_=ps_ss[:], func=AF.Sqrt, scale=1.0 / HD, bias=self.epsb[:, 0:1]),
                                     r=["a_psss", "epsb"], w=["a_rstd"])
                                if "e2" in DBG:
                                    P.op("dve", lambda e: e.reciprocal(t1[:], rstd[:]), r=["a_rstd"], w=["a_t1"])
                                    P.op("act", lambda e, oi=oi, tsl=tsl: e.copy(ob[oi][:, tsl], ps_rot[:]), r=["a_psrot"], w=[okey])
                                    continue
                                P.op("dve", lambda e: e.reciprocal(rstd[:], rstd[:]), r=["a_rstd"], w=["a_rstd"])
                                P.op("dve", lambda e, tsl=tsl: e.tensor_tensor(t1[:], qg[:], cosb[:, tsl], ALU.mult), r=["a_qg", "a_cos"], w=["a_t1"])
                                P.op("dve", lambda e, tsl=tsl: e.tensor_tensor(t2[:], ps_rot[:], sinb[:, tsl], ALU.mult), r=["a_psrot", "a_sin"], w=["a_t2"])
                                P.op("dve", lambda e: e.tensor_tensor(t1[:], t1[:], t2[:], ALU.add), r=["a_t1", "a_t2"], w=["a_t1"])
                                P.op("dve", lambda e, oi=oi, tsl=tsl: e.tensor_tensor(ob[oi][:, tsl], t1[:], rstd[:], ALU.mult),
                                     r=["a_t1", "a_rstd"], w=[okey])
                            elif kind == "u":
                                P.op("act", lambda e, pi=pi, oi=oi, tsl=tsl: e.copy(ob[oi][:, tsl], psm[pi][:]), r=[pk], w=[okey])
                            else:
                                P.op("act", lambda e, pi=pi, oi=oi, tsl=tsl: e.activation(out=ob[oi][:, tsl], in_=psm[pi][:], func=AF.Sigmoid),
                                     r=[pk], w=[okey])
                        if kind == "qk":
                            P.dma("sp", self.qkT[j, :, T0:T0 + TS], ob[oi][:], r=[okey], w=["qkT"])
                        elif kind == "u":
                            P.dma("sp", self.uT[j * 128:(j + 1) * 128, T0:T0 + TS], ob[oi][:], r=[okey], w=["uT"])
                        else:
                            P.dma("sp", self.gT[j * 128:(j + 1) * 128, T0:T0 + TS], ob[oi][:], r=[okey], w=["gT"])
                vo = [self.sb(st, "a_vo%d" % i, [128, 256], BF16) for i in range(2)]
                vcount = 0
                for c in range(6):
                    if self.stop == 'a1':
                        continue
                    wi = load_w(3072 + c * 256, 256)
                    for tt in range(ntt):
                        pi = mmcount[0] % 3
                        mmcount[0] += 1
                        pk = "a_ps%d" % pi
                        for kc in range(16):
                            P.op("pe", lambda e, pi=pi, wi=wi, kc=kc, tt=tt: e.matmul(
                                psm[pi][:, 0:256], hnT[:, kc, tt * 128:(tt + 1) * 128], wb[wi][:, kc, :],
                                start=(kc == 0), stop=(kc == 15)),
                                r=["a_wb%d" % wi, ("a_hnT", tt)], w=[pk])
                        vi = vcount % 2
                        vcount += 1
                        P.op("act", lambda e, pi=pi, vi=vi: e.copy(vo[vi][:], psm[pi][:, 0:256]), r=[pk], w=["a_vo%d" % vi])
                        P.dma("sp", self.v[T0 + tt * 128:T0 + (tt + 1) * 128, c * 256:(c + 1) * 256], vo[vi][:],
                              r=["a_vo%d" % vi], w=["v"])
                P.barrier()
        return

    def stage_attn(self, l):
        P, S = self.P, self.S
        scale = float(HD) ** -0.5
        with contextlib.ExitStack() as st:
            amask = self.sb(st, "t_amask", [128, 3, 128], BF16)
            P.dma("pool", amask[:], self.c_amask.rearrange("p (j q) -> p j q", j=3), w=["t_amask"])
            acc_n = self.sb(st, "t_accn", [128, S], F32)
            acc_d = self.sb(st, "t_accd", [128, S], F32)
            qT = self.sb(st, "t_qT", [128, S], BF16)
            kT = self.sb(st, "t_kT", [128, S], BF16)
            vt = self.sb(st, "t_vt", [128, S // 128, 128], BF16)
            es = [self.sb(st, "t_es%d" % i, [128, 3, 128], BF16) for i in range(2)]
            em = [self.sb(st, "t_em%d" % i, [128, 3, 128], BF16) for i in range(2)]
            ps_s = [self.ps(st, "t_pss%d" % i, [128, 3, 128], F32) for i in range(2)]
            ps_o = [self.ps(st, "t_pso%d" % i, [128, 128], F32) for i in range(2)]
            ps_d = [self.ps(st, "t_psd%d" % i, [128, 128], F32) for i in range(2)]
            ob = self.sb(st, "t_ob", [128, S], BF16)
            cnt = 0
            for slot in range(4):
                for g, d in enumerate((1, 4, 16)):
                    head = 4 * g + slot
                    n = S // d // 128
                    P.dma("sp", qT[:], self.qkT[head, :, :], r=["qkT"], w=["t_qT"])
                    P.dma("sp", kT[:], self.qkT[12 + head, :, :], r=["qkT"], w=["t_kT"])
                    vh = self.v[:, head * 128:(head + 1) * 128].rearrange("(jt jp d) c -> d jp jt c", d=d, jp=128)
                    for r in range(d):
                        P.dma("sp", vt[:, r * n:(r + 1) * n, :], vh[r], r=["v"], w=["t_vt"])
                    qv = qT[:].rearrange("p (j d) -> p d j", d=d)
                    kv = kT[:].rearrange("p (j d) -> p d j", d=d)
                    anv = acc_n[:].rearrange("p (j d) -> p d j", d=d)
                    adv = acc_d[:].rearrange("p (j d) -> p d j", d=d)
                    for r in range(d):
                        for i in range(n):
                            b = cnt % 2
                            cnt += 1
                            kts = [kt for kt in (i - 1, i, i + 1) if 0 <= kt < n]
                            jlo, jhi = kts[0] - i + 1, kts[-1] - i + 2
                            qs = slice(i * 128, (i + 1) * 128)
                            for kt in kts:
                                jj = kt - i + 1
                                P.op("pe", lambda e, b=b, jj=jj, r=r, kt=kt, qs=qs, kv=kv, qv=qv: e.matmul(
                                    ps_s[b][:, jj, :], kv[:, r, kt * 128:(kt + 1) * 128], qv[:, r, qs], start=True, stop=True),
                                    r=["t_kT", "t_qT"], w=["t_pss%d" % b])
                            P.op("act", lambda e, b=b, jlo=jlo, jhi=jhi: e.activation(out=es[b][:, jlo:jhi, :], in_=ps_s[b][:, jlo:jhi, :], func=AF.Exp, scale=scale),
                                 r=["t_pss%d" % b], w=["t_es%d" % b])
                            P.op("dve", lambda e, b=b, jlo=jlo, jhi=jhi: e.tensor_tensor(em[b][:, jlo:jhi, :], es[b][:, jlo:jhi, :], amask[:, jlo:jhi, :], ALU.mult),
                                 r=["t_es%d" % b, "t_amask"], w=["t_em%d" % b])
                            for kt in kts:
                                jj = kt - i + 1
                                P.op("pe", lambda e, b=b, jj=jj, r=r, kt=kt, n=n, kts=kts: e.matmul(
                                    ps_o[b][:], vt[:, r * n + kt, :], em[b][:, jj, :], start=(kt == kts[0]), stop=(kt == kts[-1])),
                                    r=["t_vt", "t_em%d" % b], w=["t_pso%d" % b])
                            for kt in kts:
                                jj = kt - i + 1
                                P.op("pe", lambda e, b=b, jj=jj, kt=kt, kts=kts: e.matmul(
                                    ps_d[b][:], self.ones_b[:], em[b][:, jj, :], start=(kt == kts[0]), stop=(kt == kts[-1])),
                                    r=["ones_b", "t_em%d" % b], w=["t_psd%d" % b])
                            if g == 0:
                                P.op("act", lambda e, b=b, r=r, qs=qs, anv=anv: e.copy(anv[:, r, qs], ps_o[b][:]), r=["t_pso%d" % b], w=["t_accn"])
                                P.op("dve", lambda e, b=b, r=r, qs=qs, adv=adv: e.tensor_copy(adv[:, r, qs], ps_d[b][:]), r=["t_psd%d" % b], w=["t_accd"])
                            else:
                                P.op("dve", lambda e, b=b, r=r, qs=qs, anv=anv: e.tensor_tensor(anv[:, r, qs], anv[:, r, qs], ps_o[b][:], ALU.add),
                                     r=["t_pso%d" % b, "t_accn"], w=["t_accn"])
                                P.op("dve", lambda e, b=b, r=r, qs=qs, adv=adv: e.tensor_tensor(adv[:, r, qs], adv[:, r, qs], ps_d[b][:], ALU.add),
                                     r=["t_psd%d" % b, "t_accd"], w=["t_accd"])
                P.op("dve", lambda e: e.reciprocal(acc_d[:], acc_d[:]), r=["t_accd"], w=["t_accd"])
                P.op("dve", lambda e: e.tensor_tensor(ob[:], acc_n[:], acc_d[:], ALU.mult), r=["t_accn", "t_accd"], w=["t_ob"])
                P.dma("sp", self.attT[slot * 128:(slot + 1) * 128, :], ob[:], r=["t_ob"], w=["attT"])
            P.barrier()

    def stage_s5(self, l):
        P, S = self.P, self.S
        Lc = 512
        nch = S // Lc
        GC = 1.5957691216057308
        with contextlib.ExitStack() as st:
            sb = lambda n, shp, dt: self.sb(st, n, shp, dt)
            ioti = sb("s_ioti", [128, Lc + 1], I32)
            iot = sb("s_iot", [128, Lc + 1], F32)
            P.op("pool", lambda e: e.iota(ioti[:], pattern=[[1, Lc + 1]], base=0, channel_multiplier=0), w=["s_ioti"])
            P.op("dve", lambda e: e.tensor_copy(iot[:], ioti[:]), r=["s_ioti"], w=["s_iot"])
            prm = {}
            pti = sb("s_pti", [128, 32], I32)
            ptf = sb("s_ptf", [128, 32], F32)
            for dr in range(2):
                names = ["are", "aim", "ldt", "dt", "ar", "th", "rho", "sn", "cs", "lbr", "lbi", "den", "nr", "cr", "ci", "nci", "t"]
                T = {n: sb("s_%s%d" % (n, dr), [128, 32], F32) for n in names}
                K = {n: "s_%s%d" % (n, dr) for n in names}
                P.dma("sp", T["are"][:], self.s5_are[l, dr], w=[K["are"]])
                P.dma("sp", T["aim"][:], self.s5_aim[l, dr], w=[K["aim"]])
                P.dma("sp", T["ldt"][:], self.s5_ldt[l, dr], w=[K["ldt"]])
                P.op("act", lambda e, T=T: e.activation(out=T["dt"][:], in_=T["ldt"][:], func=AF.Exp), r=[K["ldt"]], w=[K["dt"]])
                tt = lambda o, a, b, op, T=T, K=K: P.op("dve", lambda e: e.tensor_tensor(T[o][:], T[a][:], T[b][:], op), r=[K[a], K[b]], w=[K[o]])
                tt("ar", "are", "dt", ALU.mult)
                tt("th", "aim", "dt", ALU.mult)
                P.op("act", lambda e, T=T: e.activation(out=T["rho"][:], in_=T["ar"][:], func=AF.Exp), r=[K["ar"]], w=[K["rho"]])
                self.sin_reduced(T["th"], K["th"], T["sn"], K["sn"], pti, "s_pti", ptf, "s_ptf", 0.0)
                self.sin_reduced(T["th"], K["th"], T["cs"], K["cs"], pti, "s_pti", ptf, "s_ptf", math.pi / 2)
                tt("lbr", "rho", "cs", ALU.mult)
                tt("lbi", "rho", "sn", ALU.mult)
                tt("t", "are", "are", ALU.mult)
                tt("den", "aim", "aim", ALU.mult)
                tt("den", "den", "t", ALU.add)
                P.op("dve", lambda e, T=T: e.reciprocal(T["den"][:], T["den"][:]), r=[K["den"]], w=[K["den"]])
                P.op("dve", lambda e, T=T: e.tensor_scalar(T["nr"][:], T["lbr"][:], -1.0, None, ALU.add), r=[K["lbr"]], w=[K["nr"]])
                tt("cr", "nr", "are", ALU.mult)
                tt("t", "lbi", "aim", ALU.mult)
                tt("cr", "cr", "t", ALU.add)
                tt("cr", "cr", "den", ALU.mult)
                tt("ci", "lbi", "are", ALU.mult)
                tt("t", "nr", "aim", ALU.mult)
                tt("ci", "ci", "t", ALU.subtract)
                tt("ci", "ci", "den", ALU.mult)
                P.op("dve", lambda e, T=T: e.tensor_scalar(T["nci"][:], T["ci"][:], -1.0, None, ALU.mult), r=[K["ci"]], w=[K["nci"]])
                prm[dr] = (T, K)
            uP = sb("s_uP", [32, S], BF16)
            yacc = sb("s_yacc", [32, S], F32)
            dcol = sb("s_dcol", [32, 1], F32)
            Bre = sb("s_Bre", [32, 128], BF16)
            Bim = sb("s_Bim", [32, 128], BF16)
            Cre = sb("s_Cre", [128, 32], F32)
            Cim = sb("s_Cim", [128, 32], F32)
            Ct = sb("s_Ct", [128, 32], F32)
            Cpr = sb("s_Cpr", [128, 32], BF16)
            Cni = sb("s_Cni", [128, 32], BF16)
            ang = sb("s_ang", [128, Lc + 1], F32)
            tsn = sb("s_tsn", [128, Lc + 1], F32)
            tcs = sb("s_tcs", [128, Lc + 1], F32)
            tti = sb("s_tti", [128, Lc + 1], I32)
            ttf = sb("s_ttf", [128, Lc + 1], F32)
            rhob = sb("s_rhob", [128, Lc], F32)
            br = [sb("s_br%d" % i, [128, Lc], F32) for i in range(2)]
            bi = [sb("s_bi%d" % i, [128, Lc], F32) for i in range(2)]
            p1 = sb("s_p1", [128, Lc], F32)
            p2 = sb("s_p2", [128, Lc], F32)
            btr = [sb("s_btr%d" % i, [128, Lc], F32) for i in range(2)]
            bti = [sb("s_bti%d" % i, [128, Lc], F32) for i in range(2)]
            wr = [sb("s_wr%d" % i, [128, Lc], F32) for i in range(2)]
            wi_ = [sb("s_wi%d" % i, [128, Lc], F32) for i in range(2)]
            d1 = sb("s_d1", [128, Lc], F32)
            d2 = sb("s_d2", [128, Lc], F32)
            xr = [sb("s_xr%d" % i, [128, Lc], BF16) for i in range(2)]
            xi = [sb("s_xi%d" % i, [128, Lc], BF16) for i in range(2)]
            ini = [sb("s_ini%d" % i, [128, 2], F32) for i in range(2)]
            it_ = sb("s_it", [128, 1], F32)
            GW = min(S, 2048)
            g1 = sb("s_g1", [32, GW], F32)
            g2 = sb("s_g2", [32, GW], F32)
            yo = sb("s_yo", [32, GW], BF16)
            ps_br = [self.ps(st, "s_psbr%d" % i, [128, Lc], F32) for i in range(2)]
            ps_bi = [self.ps(st, "s_psbi%d" % i, [128, Lc], F32) for i in range(2)]
            ps_y = [self.ps(st, "s_psy%d" % i, [32, Lc], F32) for i in range(2)]
            cnt = 0
            import os
            for gp in range(1 if 's5one' in os.environ.get('DBG', '') else 32):
                P.dma("sp", uP[:], self.uT[gp * 32:(gp + 1) * 32, :], r=["uT"], w=["s_uP"])
                P.dma("sp", dcol[:], self.s5_d[l, gp], w=["s_dcol"])
                for dr in range(2):
                    T, K = prm[dr]
                    col = lambda n, T=T, gp=gp: T[n][:, gp:gp + 1]
                    P.dma("pool", Bre[:], self.s5_bre[l, dr, gp], w=["s_Bre"])
                    P.dma("pool", Bim[:], self.s5_bim[l, dr, gp], w=["s_Bim"])
                    P.dma("sp", Cre[:], self.s5_cre[l, dr, gp], w=["s_Cre"])
                    P.dma("sp", Cim[:], self.s5_cim[l, dr, gp], w=["s_Cim"])
                    P.op("dve", lambda e, col=col: e.tensor_scalar(Ct[:], Cim[:], col("ci"), None, ALU.mult), r=["s_Cim", K["ci"]], w=["s_Ct"])
                    P.op("dve", lambda e, col=col: e.scalar_tensor_tensor(Cpr[:], Cre[:], col("cr"), Ct[:], ALU.mult, ALU.subtract),
                         r=["s_Cre", "s_Ct", K["cr"]], w=["s_Cpr"])
                    P.op("dve", lambda e, col=col: e.tensor_scalar(Ct[:], Cim[:], col("cr"), None, ALU.mult), r=["s_Cim", K["cr"]], w=["s_Ct"])
                    P.op("dve", lambda e, col=col: e.scalar_tensor_tensor(Cni[:], Cre[:], col("nci"), Ct[:], ALU.mult, ALU.subtract),
                         r=["s_Cre", "s_Ct", K["nci"]], w=["s_Cni"])
                    P.op("dve", lambda e, col=col: e.tensor_scalar(ang[:], iot[:], col("th"), None, ALU.mult), r=["s_iot", K["th"]], w=["s_ang"])
                    self.sin_reduced(ang, "s_ang", tsn, "s_tsn", tti, "s_tti", ttf, "s_ttf", 0.0)
                    self.sin_reduced(ang, "s_ang", tcs, "s_tcs", tti, "s_tti", ttf, "s_ttf", math.pi / 2)
                    P.op("dve", lambda e, col=col: e.tensor_scalar(rhob[:], iot[:, 0:Lc], 0.0, col("rho"), ALU.mult, ALU.add),
                         r=["s_iot", K["rho"]], w=["s_rhob"])
                    sn, cs = tsn[:, 0:Lc], tcs[:, 0:Lc]
                    snL, csL = tsn[:, Lc:Lc + 1], tcs[:, Lc:Lc + 1]
                    prev = None
                    for ci_ in range(nch):
                        c = ci_ if dr == 0 else nch - 1 - ci_
                        b = cnt % 2
                        cnt += 1
                        csl = slice(c * Lc, (c + 1) * Lc)
                        rv = (lambda ap: ap) if dr == 0 else (lambda ap: ap[:, ::-1])
                        kb = lambda n, b=b: "s_%s%d" % (n, b)
                        P.op("pe", lambda e, b=b, csl=csl: e.matmul(ps_br[b][:], Bre[:], uP[:, csl], start=True, stop=True), r=["s_Bre", "s_uP"], w=[kb("psbr")])
                        P.op("pe", lambda e, b=b, csl=csl: e.matmul(ps_bi[b][:], Bim[:], uP[:, csl], start=True, stop=True), r=["s_Bim", "s_uP"], w=[kb("psbi")])
                        P.op("act", lambda e, b=b, rv=rv: e.copy(br[b][:], rv(ps_br[b][:])), r=[kb("psbr")], w=[kb("br")])
                        P.op("act", lambda e, b=b, rv=rv: e.copy(bi[b][:], rv(ps_bi[b][:])), r=[kb("psbi")], w=[kb("bi")])
                        P.op("pool", lambda e, b=b, cs=cs: e.tensor_tensor(p1[:], br[b][:], cs, ALU.mult), r=[kb("br"), "s_tcs"], w=["s_p1"])
                        P.op("pool", lambda e, b=b, sn=sn: e.tensor_tensor(p2[:], bi[b][:], sn, ALU.mult), r=[kb("bi"), "s_tsn"], w=["s_p2"])
                        P.op("pool", lambda e, b=b: e.tensor_tensor(btr[b][:], p1[:], p2[:], ALU.add), r=["s_p1", "s_p2"], w=[kb("btr")])
                        P.op("pool", lambda e, b=b, cs=cs: e.tensor_tensor(p1[:], bi[b][:], cs, ALU.mult), r=[kb("bi"), "s_tcs"], w=["s_p1"])
                        P.op("pool", lambda e, b=b, sn=sn: e.tensor_tensor(p2[:], br[b][:], sn, ALU.mult), r=[kb("br"), "s_tsn"], w=["s_p2"])
                        P.op("pool", lambda e, b=b: e.tensor_tensor(bti[b][:], p1[:], p2[:], ALU.subtract), r=["s_p1", "s_p2"], w=[kb("bti")])
                        if prev is None:
                            P.op("dve", lambda e, b=b: e.memset(ini[b][:], 0.0), w=[kb("ini")])
                        else:
                            pb = prev
                            wre, wie = wr[pb][:, Lc - 1:Lc], wi_[pb][:, Lc - 1:Lc]
                            P.op("dve", lambda e, wie=wie, snL=snL: e.tensor_tensor(it_[:], wie, snL, ALU.mult), r=["s_wi%d" % pb, "s_tsn"], w=["s_it"])
                            P.op("dve", lambda e, b=b, wre=wre, csL=csL: e.tensor_tensor(ini[b][:, 0:1], wre, csL, ALU.mult), r=["s_wr%d" % pb, "s_tcs"], w=[kb("ini")])
                            P.op("dve", lambda e, b=b: e.tensor_tensor(ini[b][:, 0:1], ini[b][:, 0:1], it_[:], ALU.subtract), r=[kb("ini"), "s_it"], w=[kb("ini")])
                            P.op("dve", lambda e, wie=wie, csL=csL: e.tensor_tensor(it_[:], wie, csL, ALU.mult), r=["s_wi%d" % pb, "s_tcs"], w=["s_it"])
                            P.op("dve", lambda e, b=b, wre=wre, snL=snL: e.tensor_tensor(ini[b][:, 1:2], wre, snL, ALU.mult), r=["s_wr%d" % pb, "s_tsn"], w=[kb("ini")])
                            P.op("dve", lambda e, b=b: e.tensor_tensor(ini[b][:, 1:2], ini[b][:, 1:2], it_[:], ALU.add), r=[kb("ini"), "s_it"], w=[kb("ini")])
                        P.op("dve", lambda e, b=b: e.tensor_tensor_scan(wr[b][:], rhob[:], btr[b][:], ini[b][:, 0:1], ALU.mult, ALU.add),
                             r=["s_rhob", kb("btr"), kb("ini")], w=[kb("wr")])
                        P.op("dve", lambda e, b=b: e.tensor_tensor_scan(wi_[b][:], rhob[:], bti[b][:], ini[b][:, 1:2], ALU.mult, ALU.add),
                             r=["s_rhob", kb("bti"), kb("ini")], w=[kb("wi")])
                        P.op("dve", lambda e, b=b, cs=cs: e.tensor_tensor(d1[:], wr[b][:], cs, ALU.mult), r=[kb("wr"), "s_tcs"], w=["s_d1"])
                        P.op("dve", lambda e, b=b, sn=sn: e.tensor_tensor(d2[:], wi_[b][:], sn, ALU.mult), r=[kb("wi"), "s_tsn"], w=["s_d2"])
                        P.op("dve", lambda e, b=b: e.tensor_tensor(xr[b][:], d1[:], d2[:], ALU.subtract), r=["s_d1", "s_d2"], w=[kb("xr")])
                        P.op("dve", lambda e, b=b, sn=sn: e.tensor_tensor(d1[:], wr[b][:], sn, ALU.mult), r=[kb("wr"), "s_tsn"], w=["s_d1"])
                        P.op("dve", lambda e, b=b, cs=cs: e.tensor_tensor(d2[:], wi_[b][:], cs, ALU.mult), r=[kb("wi"), "s_tcs"], w=["s_d2"])
                        P.op("dve", lambda e, b=b: e.tensor_tensor(xi[b][:], d1[:], d2[:], ALU.add), r=["s_d1", "s_d2"], w=[kb("xi")])
                        P.op("pe", lambda e, b=b: e.matmul(ps_y[b][:], Cpr[:], xr[b][:], start=True, stop=False), r=["s_Cpr", kb("xr")], w=[kb("psy")])
                        P.op("pe", lambda e, b=b: e.matmul(ps_y[b][:], Cni[:], xi[b][:], start=False, stop=True), r=["s_Cni", kb("xi")], w=[kb("psy")])
                        if dr == 0:
                            P.op("act", lambda e, b=b, csl=csl: e.copy(yacc[:, csl], ps_y[b][:]), r=[kb("psy")], w=["s_yacc"])
                        else:
                            P.op("dve", lambda e, b=b, csl=csl: e.tensor_tensor(yacc[:, csl][:, ::-1], yacc[:, csl][:, ::-1], ps_y[b][:], ALU.add),
                                 r=[kb("psy"), "s_yacc"], w=["s_yacc"])
                        prev = b
                for gc in range(S // GW):
                    gsl = slice(gc * GW, (gc + 1) * GW)
                    P.op("dve", lambda e, gsl=gsl: e.scalar_tensor_tensor(g1[:], uP[:, gsl], dcol[:, 0:1], yacc[:, gsl], ALU.mult, ALU.add), r=["s_uP", "s_dcol", "s_yacc"], w=["s_g1"])
                    if self.stop == "s5raw":
                        P.dma("sp", self.dbg_y[gp * 32:(gp + 1) * 32, gsl], g1[:], r=["s_g1"], w=["dbg_y"])
                    P.op("pool", lambda e: e.tensor_tensor(g2[:], g1[:], g1[:], ALU.mult), r=["s_g1"], w=["s_g2"])
                    P.op("pool", lambda e: e.tensor_scalar(g2[:], g2[:], 0.044715, 1.0, ALU.mult, ALU.add), r=["s_g2"], w=["s_g2"])
                    P.op("pool", lambda e: e.tensor_tensor(g2[:], g2[:], g1[:], ALU.mult), r=["s_g2", "s_g1"], w=["s_g2"])
                    P.op("act", lambda e: e.activation(out=g2[:], in_=g2[:], func=AF.Sigmoid, scale=GC), r=["s_g2"], w=["s_g2"])
                    P.op("pool", lambda e: e.tensor_tensor(yo[:], g2[:], g1[:], ALU.mult), r=["s_g2", "s_g1"], w=["s_yo"])
                    P.dma("sp", self.ygT[gp * 32:(gp + 1) * 32, gsl], yo[:], r=["s_yo"], w=["ygT"])
            P.barrier()

    def stage_c1(self, l):
        P, S, TS = self.P, self.S, self.TS
        nsub = TS // 512
        for sup in range(S // TS):
            T0 = sup * TS
            with contextlib.ExitStack() as st:
                sb = lambda n, shp, dt: self.sb(st, n, shp, dt)
                aT = sb("c_aT", [128, 4, TS], BF16)
                yT = sb("c_yT", [128, 8, TS], BF16)
                P.dma("sp", aT[:], self.attT[:, T0:T0 + TS].rearrange("(kc p) t -> p kc t", p=128), r=["attT"], w=["c_aT"])
                P.dma("sp", yT[:], self.ygT[:, T0:T0 + TS].rearrange("(kc p) t -> p kc t", p=128), r=["ygT"], w=["c_yT"])
                wa = [sb("c_wa%d" % i, [128, 4, 128], BF16) for i in range(2)]
                wv = [sb("c_wv%d" % i, [128, 8, 128], BF16) for i in range(2)]
                wg = [sb("c_wg%d" % i, [128, 8, 128], BF16) for i in range(2)]
                ga = [sb("c_ga%d" % i, [128, TS], BF16) for i in range(2)]
                gs = [sb("c_gs%d" % i, [128, TS], BF16) for i in range(2)]
                mo = [sb("c_mo%d" % i, [128, TS], BF16) for i in range(2)]
                sg = sb("c_sg", [128, 512], F32)
                sbr = sb("c_sbr", [128, 512], F32)
                ta = sb("c_ta", [128, 512], F32)
                psA = [self.ps(st, "c_psA%d" % i, [128, 512], F32) for i in range(2)]
                psV = [self.ps(st, "c_psV%d" % i, [128, 512], F32) for i in range(2)]
                psG = [self.ps(st, "c_psG%d" % i, [128, 512], F32) for i in range(2)]
                cnt = 0
                for m in range(16):
                    wb_ = m % 2
                    cs_ = slice(m * 128, (m + 1) * 128)
                    P.dma("pool", wa[wb_][:], self.w_attn_br[l, :, cs_].rearrange("(kc p) n -> p kc n", p=128), w=["c_wa%d" % wb_])
                    P.dma("pool", wv[wb_][:], self.w_ssm_br[l, :, cs_].rearrange("(kc p) n -> p kc n", p=128), w=["c_wv%d" % wb_])
                    P.dma("pool", wg[wb_][:], self.w_ssm_br[l, :, 2048 + m * 128:2048 + (m + 1) * 128].rearrange("(kc p) n -> p kc n", p=128), w=["c_wg%d" % wb_])
                    P.dma("sp", ga[wb_][:], self.gT[m * 128:(m + 1) * 128, T0:T0 + TS], r=["gT"], w=["c_ga%d" % wb_])
                    P.dma("sp", gs[wb_][:], self.gT[2048 + m * 128:2048 + (m + 1) * 128, T0:T0 + TS], r=["gT"], w=["c_gs%d" % wb_])
                    for sub in range(nsub):
                        b = cnt % 2
                        cnt += 1
                        tsl = slice(sub * 512, (sub + 1) * 512)
                        for kc in range(4):
                            P.op("pe", lambda e, b=b, wb_=wb_, kc=kc, tsl=tsl: e.matmul(psA[b][:], wa[wb_][:, kc, :], aT[:, kc, tsl], start=(kc == 0), stop=(kc == 3)),
                                 r=["c_wa%d" % wb_, "c_aT"], w=["c_psA%d" % b])
                        for kc in range(8):
                            P.op("pe", lambda e, b=b, wb_=wb_, kc=kc, tsl=tsl: e.matmul(psV[b][:], wv[wb_][:, kc, :], yT[:, kc, tsl], start=(kc == 0), stop=(kc == 7)),
                                 r=["c_wv%d" % wb_, "c_yT"], w=["c_psV%d" % b])
                        for kc in range(8):
                            P.op("pe", lambda e, b=b, wb_=wb_, kc=kc, tsl=tsl: e.matmul(psG[b][:], wg[wb_][:, kc, :], yT[:, kc, tsl], start=(kc == 0), stop=(kc == 7)),
                                 r=["c_wg%d" % wb_, "c_yT"], w=["c_psG%d" % b])
                        P.op("act", lambda e, b=b: e.activation(out=sg[:], in_=psG[b][:], func=AF.Sigmoid), r=["c_psG%d" % b], w=["c_sg"])
                        P.op("dve", lambda e, b=b: e.tensor_tensor(sbr[:], psV[b][:], sg[:], ALU.mult), r=["c_psV%d" % b, "c_sg"], w=["c_sbr"])
                        P.op("dve", lambda e, wb_=wb_, tsl=tsl: e.tensor_tensor(sbr[:], sbr[:], gs[wb_][:, tsl], ALU.mult), r=["c_sbr", "c_gs%d" % wb_], w=["c_sbr"])
                        P.op("dve", lambda e, b=b, wb_=wb_, tsl=tsl: e.tensor_tensor(ta[:], psA[b][:], ga[wb_][:, tsl], ALU.mult), r=["c_psA%d" % b, "c_ga%d" % wb_], w=["c_ta"])
                        P.op("dve", lambda e, wb_=wb_, tsl=tsl: e.tensor_tensor(mo[wb_][:, tsl], ta[:], sbr[:], ALU.add), r=["c_ta", "c_sbr"], w=["c_mo%d" % wb_])
                    P.dma("sp", self.mergedT[m * 128:(m + 1) * 128, T0:T0 + TS], mo[wb_][:], r=["c_mo%d" % wb_], w=["mergedT"])
                P.barrier()

    def stage_c2(self, l):
        P, S = self.P, self.S
        with contextlib.ExitStack() as st:
            sb = lambda n, shp, dt: self.sb(st, n, shp, dt)
            W = sb("o_W", [128, 16, 2048], BF16)
            for c in range(8):
                P.dma("pool", W[:, :, c * 256:(c + 1) * 256], self.w_out[l, :, c * 256:(c + 1) * 256].rearrange("(kc p) n -> p kc n", p=128), w=[("o_W", c)])
            wkeys = [("o_W", c) for c in range(8)]
            mT = [sb("o_mT%d" % i, [128, 16, 128], BF16) for i in range(2)]
            hb = [sb("o_hb%d" % i, [128, 2048], F32) for i in range(2)]
            psm = [self.ps(st, "o_ps%d" % i, [128, 512], F32) for i in range(4)]
            for tt in range(S // 128):
                b = tt % 2
                t0 = tt * 128
                P.dma("sp", mT[b][:], self.mergedT[:, t0:t0 + 128].rearrange("(kc p) t -> p kc t", p=128), r=["mergedT"], w=["o_mT%d" % b])
                P.dma("sp", hb[b][:], self.h[t0:t0 + 128, :], r=["h"], w=["o_hb%d" % b])
                for c4 in range(4):
                    for kc in range(16):
                        P.op("pe", lambda e, b=b, c4=c4, kc=kc: e.matmul(psm[c4][:], mT[b][:, kc, :], W[:, kc, c4 * 512:(c4 + 1) * 512], start=(kc == 0), stop=(kc == 15)),
                             r=["o_mT%d" % b] + wkeys[c4 * 2:c4 * 2 + 2], w=["o_ps%d" % c4])
                    P.op("dve", lambda e, b=b, c4=c4: e.tensor_tensor(hb[b][:, c4 * 512:(c4 + 1) * 512], hb[b][:, c4 * 512:(c4 + 1) * 512], psm[c4][:], ALU.add),
                         r=["o_ps%d" % c4, "o_hb%d" % b], w=["o_hb%d" % b])
                P.dma("sp", self.h[t0:t0 + 128, :], hb[b][:], r=["o_hb%d" % b], w=["h"])
            P.barrier()

    def norm_tile(self, hb, hbk, gb, gbk, xn, xnk, ss, ssk, rs, rsk, junk, junkk):
        P = self.P
        P.op("act", lambda e: e.activation(out=junk[:], in_=hb[:], func=AF.Square, accum_out=ss[:, 0:1]), r=[hbk], w=[junkk, ssk])
        P.op("act", lambda e: e.activation(out=rs[:], in_=ss[:], func=AF.Sqrt, scale=1.0 / D, bias=self.epsb[:, 0:1]), r=[ssk, "epsb"], w=[rsk])
        P.op("dve", lambda e: e.reciprocal(rs[:], rs[:]), r=[rsk], w=[rsk])
        P.op("dve", lambda e: e.scalar_tensor_tensor(xn, hb[:], rs[:, 0:1], gb[:], ALU.mult, ALU.mult), r=[hbk, rsk, gbk], w=[xnk])

    def transpose_tile(self, xn, xnk, pT, pTk, dst_fn, dkey, cnt0=0):
        P = self.P
        for g4 in range(4):
            pb = (cnt0 + g4) % 2
            for j in range(4):
                kc = g4 * 4 + j
                P.op("pe", lambda e, pb=pb, j=j, kc=kc: e.transpose(pT[pb][:, j, :], xn[:, kc * 128:(kc + 1) * 128], self.ident_b[:]),
                     r=[xnk, "ident_b"], w=[pTk % pb])
            if g4 % 2 == 0:
                P.op("act", lambda e, pb=pb, g4=g4: e.copy(dst_fn(g4), pT[pb][:]), r=[pTk % pb], w=[dkey])
            else:
                P.op("dve", lambda e, pb=pb, g4=g4: e.tensor_copy(dst_fn(g4), pT[pb][:]), r=[pTk % pb], w=[dkey])

    def stage_moe(self, l):
        P, S = self.P, self.S
        NT = S // 128
        C = S // 8
        NCT = C // 128
        RW = 2112
        BIG = float(1 << 20)
        NIT = 34
        with contextlib.ExitStack() as st0:
            aff = self.sb(st0, "m_aff", [128, NT, 16], F32)
            posi = self.p_posi
            with contextlib.ExitStack() as st:
                sb = lambda n, shp, dt: self.sb(st, n, shp, dt)
                gb = sb("m_gb", [128, D], F32)
                P.dma("sp", gb[:], self.norm_ffn[l:l + 1, :].partition_broadcast(128), w=["m_gb"])
                wr = sb("m_wr", [128, 16, 16], BF16)
                P.dma("pool", wr[:], self.w_router[l].rearrange("(kc p) n -> p kc n", p=128), w=["m_wr"])
                hb = [sb("m_hb%d" % i, [128, D], F32) for i in range(2)]
                junk = sb("m_junk", [128, D], BF16)
                ss = [sb("m_ss%d" % i, [128, 1], F32) for i in range(2)]
                rs = [sb("m_rs%d" % i, [128, 1], F32) for i in range(2)]
                xrow = [sb("m_xrow%d" % i, [128, RW], BF16) for i in range(2)]
                xT = [sb("m_xT%d" % i, [128, 16, 128], BF16) for i in range(2)]
                ex = sb("m_ex", [128, 16], F32)
                sm = sb("m_sm", [128, 1], F32)
                pT = [self.ps(st, "m_pT%d" % i, [128, 4, 128], BF16) for i in range(2)]
                psl = [self.ps(st, "m_psl%d" % i, [128, 16], F32) for i in range(2)]
                for tt in range(NT):
                    b = tt % 2
                    t0 = tt * 128
                    k = lambda n, b=b: "m_%s%d" % (n, b)
                    P.dma("sp", hb[b][:], self.h[t0:t0 + 128, :], r=["h"], w=[k("hb")])
                    self.norm_tile(hb[b], k("hb"), gb, "m_gb", xrow[b][:, 0:D], k("xrow"), ss[b], k("ss"), rs[b], k("rs"), junk, "m_junk")
                    self.transpose_tile(xrow[b][:, 0:D], k("xrow"), pT, "m_pT%d", (lambda g4, b=b: xT[b][:, g4 * 4:(g4 + 1) * 4, :]), k("xT"), cnt0=tt * 4)
                    for kc in range(16):
                        P.op("pe", lambda e, b=b, kc=kc: e.matmul(psl[b][:], xT[b][:, kc, :], wr[:, kc, :], start=(kc == 0), stop=(kc == 15)),
                             r=[k("xT"), "m_wr"], w=[k("psl")])
                    P.op("act", lambda e, b=b: e.activation(out=ex[:], in_=psl[b][:], func=AF.Exp, accum_out=sm[:, 0:1]), r=[k("psl")], w=["m_ex", "m_sm"])
                    P.op("dve", lambda e: e.reciprocal(sm[:], sm[:]), r=["m_sm"], w=["m_sm"])
                    P.op("dve", lambda e, tt=tt: e.tensor_scalar(aff[:, tt, :], ex[:], sm[:, 0:1], None, ALU.mult), r=["m_ex", "m_sm"], w=["m_aff"])
                    P.op("dve", lambda e, b=b, tt=tt: e.tensor_copy(xrow[b][:, D:D + 32].bitcast(F32), aff[:, tt, :]), r=["m_aff"], w=[k("xrow")])
                    P.op("pool", lambda e, b=b, t0=t0: e.iota(xrow[b][:, D + 32:D + 34].bitcast(I32), pattern=[[0, 1]], base=t0, channel_multiplier=1), w=[k("xrow")])
                    P.dma("sp", self.xrows[t0:t0 + 128, :], xrow[b][:], r=[k("xrow")], w=["xrows"])
                P.barrier()
            with contextlib.ExitStack() as st:
                sb = lambda n, shp, dt: self.sb(st, n, shp, dt)
                ltri = sb("m_ltri", [128, 128], BF16)
                P.dma("pool", ltri[:], self.c_ltri, w=["m_ltri"])
                lo = sb("m_lo", [128, 16], F32)
                hi = sb("m_hi", [128, 16], F32)
                mid = sb("m_mid", [128, 16], F32)
                cmpt = sb("m_cmp", [128, NT, 16], F32)
                cntp = sb("m_cntp", [128, 16], BF16)
                cntpf = sb("m_cntpf", [128, 16], F32)
                gei = sb("m_gei", [128, 16], I32)
                lti = sb("m_lti", [128, 16], I32)
                pst = self.ps(st, "m_pst", [128, 16], F32)
                P.op("dve", lambda e: e.memset(lo[:], 0.0), w=["m_lo"])
                P.op("dve", lambda e: e.memset(hi[:], 1.0), w=["m_hi"])
                affv = aff[:].rearrange("p t e -> p e t")
                cmpv = cmpt[:].rearrange("p t e -> p e t")

                def count_ge(thr, thrk):
                    P.op("dve", lambda e: e.tensor_tensor(cmpt[:], aff[:], thr[:].unsqueeze(1).to_broadcast([128, NT, 16]), ALU.is_ge),
                         r=["m_aff", thrk], w=["m_cmp"])
                    P.op("dve", lambda e: e.tensor_reduce(cntpf[:], cmpv, AX.X, ALU.add), r=["m_cmp"], w=["m_cntpf"])
                    P.op("dve", lambda e: e.tensor_copy(cntp[:], cntpf[:]), r=["m_cntpf"], w=["m_cntp"])
                for it in range(NIT):
                    P.op("dve", lambda e: e.tensor_tensor(mid[:], lo[:], hi[:], ALU.add), r=["m_lo", "m_hi"], w=["m_mid"])
                    P.op("dve", lambda e: e.tensor_scalar(mid[:], mid[:], 0.5, None, ALU.mult), r=["m_mid"], w=["m_mid"])
                    count_ge(mid, "m_mid")
                    P.op("pe", lambda e: e.matmul(pst[:], self.ones_b[:], cntp[:], start=True, stop=True), r=["ones_b", "m_cntp"], w=["m_pst"])
                    P.op("dve", lambda e: e.tensor_scalar(gei[:], pst[:], float(C), None, ALU.is_ge), r=["m_pst"], w=["m_gei"])
                    P.op("dve", lambda e: e.tensor_scalar(lti[:], pst[:], float(C), None, ALU.is_lt), r=["m_pst"], w=["m_lti"])
                    P.op("dve", lambda e: e.copy_predicated(lo[:], gei[:], mid[:]), r=["m_gei", "m_mid", "m_lo"], w=["m_lo"])
                    P.op("dve", lambda e: e.copy_predicated(hi[:], lti[:], mid[:]), r=["m_lti", "m_mid", "m_hi"], w=["m_hi"])
                count_ge(lo, "m_lo")
                P.op("pe", lambda e: e.matmul(pst[:], ltri[:], cntp[:], start=True, stop=True), r=["m_ltri", "m_cntp"], w=["m_pst"])
                offs = sb("m_offs", [128, 16], F32)
                P.op("act", lambda e: e.copy(offs[:], pst[:]), r=["m_pst"], w=["m_offs"])
                cum = sb("m_cum", [128, NT, 16], F32)
                cumv = cum[:].rearrange("p t e -> p e t")
                onesr = sb("m_onesr", [128, NT], F32)
                P.op("dve", lambda e: e.memset(onesr[:], 1.0), w=["m_onesr"])
                for ex_ in range(16):
                    P.op("dve", lambda e, ex_=ex_: e.tensor_tensor_scan(cumv[:, ex_, :], onesr[:], cmpv[:, ex_, :], 0.0, ALU.mult, ALU.add),
                         r=["m_cmp", "m_onesr"], w=["m_cum"])
                P.op("dve", lambda e: e.tensor_tensor(cum[:], cum[:], cmpt[:], ALU.subtract), r=["m_cum", "m_cmp"], w=["m_cum"])
                P.op("dve", lambda e: e.tensor_tensor(cum[:], cum[:], offs[:].unsqueeze(1).to_broadcast([128, NT, 16]), ALU.add), r=["m_cum", "m_offs"], w=["m_cum"])
                P.op("dve", lambda e: e.tensor_scalar(cum[:], cum[:], -BIG, None, ALU.add), r=["m_cum"], w=["m_cum"])
                P.op("dve", lambda e: e.tensor_tensor(cum[:], cum[:], cmpt[:], ALU.mult), r=["m_cum", "m_cmp"], w=["m_cum"])
                P.op("dve", lambda e: e.tensor_scalar(cum[:], cum[:], BIG, None, ALU.add), r=["m_cum"], w=["m_cum"])
                P.op("dve", lambda e: e.tensor_copy(posi[:], cum[:].rearrange("p t e -> p (t e)")), r=["m_cum"], w=["m_posi"])
                if self.dbg:
                    P.dma("sp", self.dbg_posi, posi[:], r=["m_posi"], w=["dbg_posi"])
                    P.dma("sp", self.dbg_aff, aff[:].rearrange("p t e -> p (t e)"), r=["m_aff"], w=["dbg_aff"])
                P.barrier()
            with contextlib.ExitStack() as st:
                xr = self.p_xr
                for tt in range(NT):
                    b = tt % 2
                    P.dma("sp", xr[b][:], self.xrows[tt * 128:(tt + 1) * 128, :], r=["xrows"], w=["m_dr%d" % b])
                    for ex_ in range(16):
                        col = tt * 16 + ex_
                        P.dma_fn("pool", lambda e, b=b, ex_=ex_, col=col: e.indirect_dma_start(
                            out=self.xg[ex_][:, :], out_offset=bass.IndirectOffsetOnAxis(ap=posi[:, col:col + 1], axis=0),
                            in_=xr[b][:], in_offset=None, bounds_check=self.reg(e, C - 1), oob_is_err=False),
                            r=["m_dr%d" % b, "m_posi"], w=[("xg", ex_)])
                P.barrier()
        with contextlib.ExitStack() as st:
            sb = lambda n, shp, dt: self.sb(st, n, shp, dt)
            xgT = sb("e_xgT", [128, 16, C], BF16)
            hidT = sb("e_hidT", [128, 8, C], BF16)
            Wd = sb("e_Wd", [128, 8, D], BF16)
            wgb = [sb("e_wg%d" % i, [128, 16, 128], BF16) for i in range(2)]
            wub = [sb("e_wu%d" % i, [128, 16, 128], BF16) for i in range(2)]
            xrw = [sb("e_xr%d" % i, [128, RW], BF16) for i in range(2)]
            gates = sb("e_gates", [128, NCT], F32)
            tid = self.p_tid
            sg = sb("e_sg", [128, 512], F32)
            yrow = self.p_yrow
            pT = [self.ps(st, "e_pT%d" % i, [128, 4, 128], BF16) for i in range(2)]
            psG = self.ps(st, "e_psG", [128, 512], F32)
            psU = self.ps(st, "e_psU", [128, 512], F32)
            psY = [self.ps(st, "e_psY%d" % i, [128, 512], F32) for i in range(2)]
            nsubc = max(1, C // 512)
            subw = min(C, 512)
            tcnt = 0
            ycnt = 0
            for ex_ in range(16):
                for c in range(8):
                    P.dma("pool", Wd[:, :, c * 256:(c + 1) * 256], self.w_exp_down[l, ex_, :, c * 256:(c + 1) * 256].rearrange("(kc p) n -> p kc n", p=128), w=[("e_Wd", c)])
                for ct in range(NCT):
                    b = ct % 2
                    P.dma("sp", xrw[b][:], self.xg[ex_][ct * 128:(ct + 1) * 128, :], r=[("xg", ex_)], w=["e_xr%d" % b])
                    self.transpose_tile(xrw[b][:, 0:D], "e_xr%d" % b, pT, "e_pT%d", (lambda g4, ct=ct: xgT[:, g4 * 4:(g4 + 1) * 4, ct * 128:(ct + 1) * 128]), ("e_xgT", ct), cnt0=tcnt)
                    tcnt += 4
                    P.op("dve", lambda e, b=b, ct=ct, ex_=ex_: e.tensor_copy(gates[:, ct:ct + 1], xrw[b][:, D + 2 * ex_:D + 2 * ex_ + 2].bitcast(F32)), r=["e_xr%d" % b], w=["e_gates"])
                    P.op("dve", lambda e, b=b, ct=ct: e.tensor_copy(tid[:, ct:ct + 1], xrw[b][:, D + 32:D + 34].bitcast(I32)), r=["e_xr%d" % b], w=["e_tid"])
                xkeys = [("e_xgT", ct) for ct in range(NCT)]
                for fb in range(8):
                    wb_ = fb % 2
                    P.dma("pool", wgb[wb_][:], self.w_exp_gate[l, ex_, :, fb * 128:(fb + 1) * 128].rearrange("(kc p) n -> p kc n", p=128), w=["e_wg%d" % wb_])
                    P.dma("pool", wub[wb_][:], self.w_exp_up[l, ex_, :, fb * 128:(fb + 1) * 128].rearrange("(kc p) n -> p kc n", p=128), w=["e_wu%d" % wb_])
                    for sub in range(nsubc):
                        tsl = slice(sub * subw, (sub + 1) * subw)
                        for kc in range(16):
                            P.op("pe", lambda e, wb_=wb_, kc=kc, tsl=tsl: e.matmul(psG[:, 0:subw], wgb[wb_][:, kc, :], xgT[:, kc, tsl], start=(kc == 0), stop=(kc == 15)),
                                 r=["e_wg%d" % wb_] + xkeys, w=["e_psG"])
                        for kc in range(16):
                            P.op("pe", lambda e, wb_=wb_, kc=kc, tsl=tsl: e.matmul(psU[:, 0:subw], wub[wb_][:, kc, :], xgT[:, kc, tsl], start=(kc == 0), stop=(kc == 15)),
                                 r=["e_wu%d" % wb_] + xkeys, w=["e_psU"])
                        P.op("act", lambda e: e.activation(out=sg[:, 0:subw], in_=psG[:, 0:subw], func=AF.Silu), r=["e_psG"], w=["e_sg"])
                        P.op("dve", lambda e, fb=fb, tsl=tsl: e.tensor_tensor(hidT[:, fb, tsl], sg[:, 0:subw], psU[:, 0:subw], ALU.mult), r=["e_sg", "e_psU"], w=[("e_hidT", fb)])
                hkeys = [("e_hidT", fb) for fb in range(8)]
                for ct in range(NCT):
                    yb = ycnt % 2
                    ycnt += 1
                    for c4 in range(4):
                        pb = c4 % 2
                        for fc in range(8):
                            P.op("pe", lambda e, pb=pb, fc=fc, ct=ct, c4=c4: e.matmul(psY[pb][:], hidT[:, fc, ct * 128:(ct + 1) * 128], Wd[:, fc, c4 * 512:(c4 + 1) * 512], start=(fc == 0), stop=(fc == 7)),
                                 r=hkeys + [("e_Wd", 2 * c4), ("e_Wd", 2 * c4 + 1)], w=["e_psY%d" % pb])
                        P.op("dve" if c4 % 2 == 0 else "act",
                             (lambda e, pb=pb, yb=yb, c4=c4, ct=ct: e.tensor_scalar(yrow[yb][:, c4 * 512:(c4 + 1) * 512], psY[pb][:], gates[:, ct:ct + 1], None, ALU.mult)) if c4 % 2 == 0 else
                             (lambda e, pb=pb, yb=yb, c4=c4, ct=ct: e.activation(out=yrow[yb][:, c4 * 512:(c4 + 1) * 512], in_=psY[pb][:], func=AF.Copy, scale=gates[:, ct:ct + 1])),
                             r=["e_psY%d" % pb, "e_gates"], w=["e_yrow%d" % yb])
                    P.dma_fn("pool", lambda e, yb=yb, ct=ct: e.indirect_dma_start(
                        out=self.h[:, :], out_offset=bass.IndirectOffsetOnAxis(ap=tid[:, ct:ct + 1], axis=0),
                        in_=yrow[yb][:], in_offset=None, bounds_check=self.reg(e, S - 1), oob_is_err=True, compute_op=ALU.add),
                        r=["e_yrow%d" % yb, "e_tid"], w=["h"])
            P.barrier()

    def stage_ple(self, l):
        P, S = self.P, self.S
        with contextlib.ExitStack() as st:
            sb = lambda n, shp, dt: self.sb(st, n, shp, dt)
            Wg = sb("l_Wg", [128, 16, D], BF16)
            for c in range(8):
                P.dma("pool", Wg[:, :, c * 256:(c + 1) * 256], self.w_ple_gate[l, :, c * 256:(c + 1) * 256].rearrange("(kc p) n -> p kc n", p=128), w=[("l_Wg", c)])
            Wp = sb("l_Wp", [128, 2, D], BF16)
            for c in range(2):
                P.dma("pool", Wp[:, :, c * 1024:(c + 1) * 1024], self.w_ple_proj[l, :, c * 1024:(c + 1) * 1024].rearrange("(kc p) n -> p kc n", p=128), w=[("l_Wp", c)])
            gb = sb("l_gb", [128, D], F32)
            P.dma("sp", gb[:], self.norm_ple[l:l + 1, :].partition_broadcast(128), w=["l_gb"])
            hb = [sb("l_hb%d" % i, [128, D], F32) for i in range(2)]
            junk = sb("l_junk", [128, D], BF16)
            ss = [sb("l_ss%d" % i, [128, 1], F32) for i in range(2)]
            rs = [sb("l_rs%d" % i, [128, 1], F32) for i in range(2)]
            xn = [sb("l_xn%d" % i, [128, D], BF16) for i in range(2)]
            hT = [sb("l_hT%d" % i, [128, 16, 128], BF16) for i in range(2)]
            pTt = [sb("l_pTt%d" % i, [128, 2, 128], BF16) for i in range(2)]
            sg = sb("l_sg", [128, 512], F32)
            pT = [self.ps(st, "l_pT%d" % i, [128, 4, 128], BF16) for i in range(2)]
            psG = [self.ps(st, "l_psG%d" % i, [128, 512], F32) for i in range(2)]
            psP = [self.ps(st, "l_psP%d" % i, [128, 512], F32) for i in range(2)]
            for tt in range(S // 128):
                b = tt % 2
                t0 = tt * 128
                k = lambda n, b=b: "l_%s%d" % (n, b)
                P.dma("sp", hb[b][:], self.h[t0:t0 + 128, :], r=["h"], w=[k("hb")])
                P.dma("pool", pTt[b][:], self.pT[l, :, t0:t0 + 128].rearrange("(kc p) t -> p kc t", p=128), w=[k("pTt")])
                self.norm_tile(hb[b], k("hb"), gb, "l_gb", xn[b][:], k("xn"), ss[b], k("ss"), rs[b], k("rs"), junk, "l_junk")
                self.transpose_tile(xn[b][:], k("xn"), pT, "l_pT%d", (lambda g4, b=b: hT[b][:, g4 * 4:(g4 + 1) * 4, :]), k("hT"), cnt0=tt * 4)
                for c4 in range(4):
                    pb = c4 % 2
                    cs_ = slice(c4 * 512, (c4 + 1) * 512)
                    for kc in range(16):
                        P.op("pe", lambda e, b=b, pb=pb, kc=kc, cs_=cs_: e.matmul(psG[pb][:], hT[b][:, kc, :], Wg[:, kc, cs_], start=(kc == 0), stop=(kc == 15)),
                             r=[k("hT"), ("l_Wg", 2 * c4), ("l_Wg", 2 * c4 + 1)], w=["l_psG%d" % pb])
                    for kc in range(2):
                        P.op("pe", lambda e, b=b, pb=pb, kc=kc, cs_=cs_: e.matmul(psP[pb][:], pTt[b][:, kc, :], Wp[:, kc, cs_], start=(kc == 0), stop=(kc == 1)),
                             r=[k("pTt"), ("l_Wp", c4 // 2)], w=["l_psP%d" % pb])
                    P.op("act", lambda e, pb=pb: e.activation(out=sg[:], in_=psG[pb][:], func=AF.Sigmoid), r=["l_psG%d" % pb], w=["l_sg"])
                    P.op("dve", lambda e, pb=pb: e.tensor_tensor(sg[:], sg[:], psP[pb][:], ALU.mult), r=["l_sg", "l_psP%d" % pb], w=["l_sg"])
                    P.op("dve", lambda e, b=b, cs_=cs_: e.tensor_tensor(hb[b][:, cs_], hb[b][:, cs_], sg[:], ALU.add), r=["l_sg", k("hb")], w=[k("hb")])
                P.dma("sp", self.h[t0:t0 + 128, :], hb[b][:], r=[k("hb")], w=["h"])
            P.barrier()

    def build(self):
        self.declare()
        P = self.P
        self.load_consts()
        self.pib = self.sb(self.stack, "pib", [128, 1], F32)
        P.op("pool", lambda e: e.memset(self.pib[:], math.pi), w=["pib"])
        self.p_posi = self.sb(self.stack, "m_posi", [128, (self.S // 128) * 16], I32)
        self.p_xr = [self.sb(self.stack, "m_dr%d" % i, [128, 2112], BF16) for i in range(2)]
        self.p_tid = self.sb(self.stack, "e_tid", [128, max(1, self.S // 1024)], I32)
        self.p_yrow = [self.sb(self.stack, "e_yrow%d" % i, [128, D], F32) for i in range(2)]
        self.epsb = self.sb(self.stack, "epsb", [128, 1], F32)
        P.op("pool", lambda e: e.memset(self.epsb[:], EPS), w=["epsb"])
        P.dma("sp", self.h[:, :], self.x[:, :], w=["h"])
        if self.stop != "init":
            self.rope_tables()
        for l in range(self.L):
            if self.stop in ("init", "rope"):
                break
            self.stage_a(l)
            if self.stop in ("a", "a1"):
                break
            self.stage_attn(l)
            if self.stop == "attn":
                break
            self.stage_s5(l)
            if self.stop in ("s5", "s5raw"):
                break
            self.stage_c1(l)
            self.stage_c2(l)
            if self.stop == "c":
                break
            self.stage_moe(l)
            if self.stop == "moe":
                break
            self.stage_ple(l)
        P.finish()
        P.emit()
        self.stack.close()
        return self.nc


SEQ = 8192
DEPTH = 4
N_CORES = 8


def kernel(**inputs):
    inp = {k: np.asarray(v) for k, v in inputs.items()}
    B = inp["x"].shape[0]
    m = MK(SEQ, DEPTH, dbg=False)
    nc = m.build()
    consts = make_consts()
    s5 = prep_s5(inp)
    shared = {}
    for name in ["norm_mix", "w_in", "q_norm", "k_norm", "w_attn_br", "w_ssm_br", "w_out", "norm_ffn", "w_router",
                 "w_exp_gate", "w_exp_up", "w_exp_down", "norm_ple", "w_ple_gate", "w_ple_proj"]:
        shared[name] = np.ascontiguousarray(inp[name], dtype=np.float32)
    shared.update(s5)
    for k, v in consts.items():
        shared["c_" + k] = v
    per_b = []
    for b in range(B):
        per_b.append({
            "x": np.ascontiguousarray(inp["x"][b], dtype=np.float32),
            "pT": np.ascontiguousarray(inp["p"][:, b].transpose(0, 2, 1), dtype=np.float32),
            "positions": np.ascontiguousarray(inp["positions"][b:b + 1], dtype=np.int32),
        })
    in_maps = []
    for c in range(N_CORES):
        b = (c * B) // N_CORES
        d = dict(shared)
        d.update(per_b[b])
        in_maps.append({k: v for k, v in d.items() if k in m.din})
    res = run_bass_kernel_spmd(nc, in_maps, core_ids=list(range(N_CORES)))
    outs = []
    for b in range(B):
        c = (b * N_CORES) // B
        outs.append(np.asarray(res.results[c]["h"], dtype=np.float32))
    return np.stack(outs, axis=0)
```

```python
import math
import contextlib
import numpy as np
import concourse.bass as bass
import concourse.mybir as mybir
from concourse.bass_utils import run_bass_kernel_spmd


F32 = mybir.dt.float32
BF16 = mybir.dt.bfloat16
I32 = mybir.dt.int32
U32 = mybir.dt.uint32
AF = mybir.ActivationFunctionType
ALU = mybir.AluOpType
AX = mybir.AxisListType


class Prog:
    ENGS = ["pe", "dve", "act", "pool", "sp"]

    def __init__(self, nc, stack, ndma=16):
        self.nc = nc
        self.ins = {e: [] for e in self.ENGS}
        self.sems = {e: stack.enter_context(nc.semaphore("cs_" + e)) for e in self.ENGS}
        self.qslots = {"sp": list(range(0, 10)), "pool": list(range(10, 18)), "act": list(range(18, 20))}
        self.ndma = ndma = 20
        self.dsems = [stack.enter_context(nc.semaphore("ds%d" % i)) for i in range(ndma)]
        self.dcount = [0] * ndma
        self.qnext = {q: 0 for q in self.qslots}
        self.last_w = {}
        self.readers = {}
        self.psum_keys = set()

    def _deps(self, reads, writes):
        deps = []
        for b in reads:
            ev = self.last_w.get(b)
            if ev is not None:
                deps.append(ev)
            if b in self.psum_keys:
                deps.extend(self.readers.get(b, ()))
        for b in writes:
            ev = self.last_w.get(b)
            if ev is not None:
                deps.append(ev)
            deps.extend(self.readers.get(b, ()))
        return deps

    def _update(self, ev, reads, writes):
        for b in reads:
            lst = self.readers.setdefault(b, [])
            if ev[0] == "c":
                lst[:] = [x for x in lst if not (x[0] == "c" and x[1] == ev[1])]
            lst.append(ev)
        for b in writes:
            self.last_w[b] = ev
            self.readers[b] = []

    def op(self, eng, fn, r=(), w=()):
        deps = self._deps(r, w)
        idx = len(self.ins[eng])
        self.ins[eng].append(dict(fn=fn, deps=deps, dma=None, sig=False))
        self._update(("c", eng, idx), r, w)

    def dma(self, q, out, in_, r=(), w=(), **kw):
        fn = lambda e: e.dma_start(out=out, in_=in_, **kw)
        self.dma_fn(q, fn, r, w)

    def dma_fn(self, q, fn, r=(), w=()):
        deps = self._deps(r, w)
        sl = self.qslots[q]
        s = sl[self.qnext[q] % len(sl)]
        self.qnext[q] += 1
        if self.dcount[s] > 0:
            deps.append(("d", s, 16 * self.dcount[s]))
        self.dcount[s] += 1
        tgt = 16 * self.dcount[s]
        self.ins[q].append(dict(fn=fn, deps=deps, dma=(s, tgt), sig=False))
        self._update(("d", s, tgt), r, w)

    def barrier(self):
        evs = []
        for e in self.ENGS:
            n = len(self.ins[e])
            for i in range(n - 1, -1, -1):
                if self.ins[e][i]["dma"] is None and self.ins[e][i]["fn"] is not None:
                    evs.append(("c", e, i))
                    break
        for s in range(self.ndma):
            if self.dcount[s] > 0:
                evs.append(("d", s, 16 * self.dcount[s]))
        for e in self.ENGS:
            self.ins[e].append(dict(fn=None, deps=list(evs), dma=None, sig=False))
        self.last_w = {}
        self.readers = {}

    def emit(self):
        nc = self.nc
        plans = {}
        for e in self.ENGS:
            wc = {x: -1 for x in self.ENGS}
            wd = [0] * self.ndma
            plan = []
            for idx, it in enumerate(self.ins[e]):
                waits = []
                for ev in it["deps"]:
                    if ev[0] == "c":
                        _, e2, i2 = ev
                        if e2 == e and e == "pe":
                            continue
                        if e2 == e and i2 >= idx:
                            continue
                        if wc[e2] >= i2:
                            continue
                        wc[e2] = i2
                        self.ins[e2][i2]["sig"] = True
                        waits.append(("c", e2, i2))
                    else:
                        _, s, tgt = ev
                        if wd[s] >= tgt:
                            continue
                        wd[s] = tgt
                        waits.append(ev)
                plan.append(waits)
            plans[e] = plan
        sigval = {}
        for e in self.ENGS:
            c = 0
            for idx, it in enumerate(self.ins[e]):
                if it["sig"]:
                    c += 1
                    sigval[(e, idx)] = c
        self.sig_totals = {e: sum(1 for it in self.ins[e] if it["sig"]) for e in self.ENGS}

        def run(e, eng):
            for idx, it in enumerate(self.ins[e]):
                for wv in plans[e][idx]:
                    if wv[0] == "c":
                        eng.wait_ge(self.sems[wv[1]], sigval[(wv[1], wv[2])])
                    else:
                        eng.wait_ge(self.dsems[wv[1]], wv[2])
                if it["fn"] is None:
                    continue
                ins = it["fn"](eng)
                if it["dma"] is not None:
                    ins.then_inc(self.dsems[it["dma"][0]], 16)
                elif it["sig"]:
                    ins.then_inc(self.sems[e], 1)

        with nc.Block() as block:
            @block.tensor
            def _(eng):
                run("pe", eng)

            @block.vector
            def _(eng):
                run("dve", eng)

            @block.scalar
            def _(eng):
                run("act", eng)

            @block.gpsimd
            def _(eng):
                run("pool", eng)

            @block.sync
            def _(eng):
                run("sp", eng)

    def finish(self):
        evs = [("d", s, 16 * self.dcount[s]) for s in range(self.ndma) if self.dcount[s] > 0]
        self.ins["sp"].append(dict(fn=None, deps=evs, dma=None, sig=False))


D = 2048
NIN = 9728
HD = 128
NH = 12
EPS = 1e-6
TWO_PI = 2.0 * math.pi


def make_consts():
    c = {}
    c["ident"] = np.eye(128, dtype=np.float32)
    c["ones"] = np.ones((128, 128), dtype=np.float32)
    rm = np.zeros((128, 128), dtype=np.float32)
    for i in range(16):
        rm[i + 16, i] = -1.0
        rm[i, i + 16] = 1.0
    c["rmat"] = rm
    invf = np.zeros((128, 1), dtype=np.float32)
    fr = np.power(np.float32(500000.0), -np.arange(16, dtype=np.float32) * np.float32(2.0) / np.float32(32.0)).astype(np.float32)
    invf[0:16, 0] = fr
    invf[16:32, 0] = fr
    c["invf"] = invf
    kk = np.arange(128)[:, None]
    qq = np.arange(128)[None, :]
    m = np.zeros((128, 3, 128), dtype=np.float32)
    m[:, 0, :] = (kk - qq >= 64)
    m[:, 1, :] = (np.abs(kk - qq) <= 64)
    m[:, 2, :] = (kk - qq <= -64)
    c["amask"] = m.reshape(128, 384)
    c["ltri"] = (np.arange(128)[:, None] < np.arange(128)[None, :]).astype(np.float32)
    return c


def prep_s5(inp):
    L = inp["ssm_a_re"].shape[0]
    o = {}
    def st(a):
        return np.ascontiguousarray(a.reshape(L, 2, 32, 128).transpose(0, 1, 3, 2))
    o["s5_are"] = st(inp["ssm_a_re"])
    o["s5_aim"] = st(inp["ssm_a_im"])
    ldt = np.repeat(inp["ssm_log_dt"][:, :, :, None], 64, axis=3)
    o["s5_ldt"] = st(ldt)
    for nm, src in (("s5_bre", "ssm_b_re"), ("s5_bim", "ssm_b_im")):
        b = inp[src].reshape(L, 2, 32, 2, 64, 16)
        out = np.zeros((L, 2, 32, 32, 128), np.float32)
        for gi in range(2):
            out[:, :, :, gi * 16:(gi + 1) * 16, gi * 64:(gi + 1) * 64] = b[:, :, :, gi].transpose(0, 1, 2, 4, 3)
        o[nm] = out
    for nm, src in (("s5_cre", "ssm_c_re"), ("s5_cim", "ssm_c_im")):
        c = inp[src].reshape(L, 2, 32, 2, 16, 64)
        out = np.zeros((L, 2, 32, 128, 32), np.float32)
        for gi in range(2):
            out[:, :, :, gi * 64:(gi + 1) * 64, gi * 16:(gi + 1) * 16] = c[:, :, :, gi].transpose(0, 1, 2, 4, 3)
        o[nm] = out
    o["s5_d"] = np.ascontiguousarray(inp["ssm_d"].reshape(L, 32, 32, 1))
    return o


class MK:
    def __init__(self, S, L, dbg=False, stop=None):
        self.S, self.L, self.dbg, self.stop = S, L, dbg, stop
        self.TS = min(S, 2048)
        self.nc = nc = bass.Bass("TRN2", target_bir_lowering=False)
        self.stack = contextlib.ExitStack()
        self.P = Prog(nc, self.stack)
        self.din = {}
        self.dscr = {}

    def reg(self, eng, val):
        if not hasattr(self, "_regs"):
            self._regs = {}
        if val not in self._regs:
            self._regs[val] = eng.to_reg(val)
        return self._regs[val]

    def inp(self, name, shape, dt=F32):
        t = self.nc.dram_tensor(name, list(shape), dt, kind="ExternalInput").ap()
        self.din[name] = t
        return t

    def scr(self, name, shape, dt, out=False):
        kind = "ExternalOutput" if (out or self.dbg) else "Internal"
        t = self.nc.dram_tensor(name, list(shape), dt, kind=kind).ap()
        self.dscr[name] = t
        return t

    def sb(self, st, name, shape, dt):
        self._uid = getattr(self, "_uid", 0) + 1
        return st.enter_context(self.nc.sbuf_tensor("%s__%d" % (name, self._uid), list(shape), dt))

    def ps(self, st, name, shape, dt=F32):
        self.P.psum_keys.add(name)
        self._uid = getattr(self, "_uid", 0) + 1
        return st.enter_context(self.nc.psum_tensor("%s__%d" % (name, self._uid), list(shape), dt))

    def declare(self):
        S, L = self.S, self.L
        i = self.inp
        self.x = i("x", [S, D])
        self.pT = i("pT", [L, 256, S])
        self.pos = i("positions", [1, S], I32)
        self.norm_mix = i("norm_mix", [L, D])
        self.w_in = i("w_in", [L, D, NIN])
        self.q_norm = i("q_norm", [L, 128])
        self.k_norm = i("k_norm", [L, 128])
        self.w_attn_br = i("w_attn_br", [L, 512, D])
        self.w_ssm_br = i("w_ssm_br", [L, 1024, 2 * D])
        self.w_out = i("w_out", [L, D, D])
        self.norm_ffn = i("norm_ffn", [L, D])
        self.norm_ple = i("norm_ple", [L, D])
        self.w_ple_gate = i("w_ple_gate", [L, D, D])
        self.w_ple_proj = i("w_ple_proj", [L, 256, D])
        self.w_router = i("w_router", [L, D, 16])
        self.w_exp_gate = i("w_exp_gate", [L, 16, D, 1024])
        self.w_exp_up = i("w_exp_up", [L, 16, D, 1024])
        self.w_exp_down = i("w_exp_down", [L, 16, 1024, D])
        self.c_ltri = i("c_ltri", [128, 128])
        self.s5_are = i("s5_are", [L, 2, 128, 32])
        self.s5_aim = i("s5_aim", [L, 2, 128, 32])
        self.s5_ldt = i("s5_ldt", [L, 2, 128, 32])
        self.s5_bre = i("s5_bre", [L, 2, 32, 32, 128])
        self.s5_bim = i("s5_bim", [L, 2, 32, 32, 128])
        self.s5_cre = i("s5_cre", [L, 2, 32, 128, 32])
        self.s5_cim = i("s5_cim", [L, 2, 32, 128, 32])
        self.s5_d = i("s5_d", [L, 32, 32, 1])
        for n in ["ident", "ones", "rmat"]:
            setattr(self, "c_" + n, i("c_" + n, [128, 128]))
        self.c_invf = i("c_invf", [128, 1])
        self.c_amask = i("c_amask", [128, 384])
        s = self.scr
        self.h = s("h", [S, D], F32, out=True)
        self.cosT = s("cosT", [128, S], F32)
        self.sinT = s("sinT", [128, S], F32)
        self.qkT = s("qkT", [24, 128, S], BF16)
        self.v = s("v", [S, 1536], BF16)
        self.uT = s("uT", [1024, S], BF16)
        self.gT = s("gT", [4096, S], BF16)
        self.attT = s("attT", [512, S], BF16)
        self.ygT = s("ygT", [1024, S], BF16)
        self.mergedT = s("mergedT", [2048, S], BF16)
        self.xrows = s("xrows", [S, 2112], BF16)
        self.xg = [s("xg%d" % e_, [S // 8, 2112], BF16) for e_ in range(16)]
        if self.dbg:
            self.dbg_posi = s("dbg_posi", [128, (S // 128) * 16], I32)
            self.dbg_aff = s("dbg_aff", [128, (S // 128) * 16], F32)
        if self.stop == "s5raw":
            self.dbg_y = s("dbg_y", [1024, S], F32)

    def load_consts(self):
        P, st = self.P, self.stack
        self.ident_b = self.sb(st, "ident_b", [128, 128], BF16)
        self.ones_b = self.sb(st, "ones_b", [128, 128], BF16)
        self.rmat_b = self.sb(st, "rmat_b", [128, 128], BF16)
        self.ident_f = self.sb(st, "ident_f", [128, 128], F32)
        self.ones_f = self.sb(st, "ones_f", [128, 128], F32)
        self.invf = self.sb(st, "invf", [128, 1], F32)
        P.dma("pool", self.ident_b[:], self.c_ident, w=["ident_b"])
        P.dma("pool", self.ones_b[:], self.c_ones, w=["ones_b"])
        P.dma("pool", self.rmat_b[:], self.c_rmat, w=["rmat_b"])
        P.dma("sp", self.ident_f[:], self.c_ident, w=["ident_f"])
        P.dma("sp", self.ones_f[:], self.c_ones, w=["ones_f"])
        P.dma("sp", self.invf[:], self.c_invf, w=["invf"])

    def sin_reduced(self, x, xk, out, ok, ti, tik, tf, tfk, shift):
        P = self.P
        P.op("dve", lambda e: e.tensor_scalar(tf[:], x[:], shift, 1.0 / TWO_PI, ALU.add, ALU.mult), r=[xk], w=[tfk])
        P.op("dve", lambda e: e.tensor_copy(ti[:], tf[:]), r=[tfk], w=[tik])
        P.op("dve", lambda e: e.tensor_copy(tf[:], ti[:]), r=[tik], w=[tfk])
        P.op("dve", lambda e: e.scalar_tensor_tensor(tf[:], tf[:], -TWO_PI, x[:], ALU.mult, ALU.add), r=[tfk, xk], w=[tfk])
        P.op("dve", lambda e: e.tensor_scalar(tf[:], tf[:], shift - math.pi, -2.0 * math.pi + 2 * math.pi, ALU.max, ALU.add) if False else
             e.tensor_scalar(tf[:], tf[:], shift, -math.pi, ALU.add, ALU.max), r=[tfk], w=[tfk])
        P.op("dve", lambda e: e.tensor_scalar(tf[:], tf[:], math.pi, None, ALU.min), r=[tfk], w=[tfk])
        P.op("act", lambda e: e.activation(out=out[:], in_=tf[:], func=AF.Sin), r=[tfk], w=[ok])

    def rope_tables(self):
        P, S = self.P, self.S
        CH = min(S, 2048)
        with contextlib.ExitStack() as st:
            pi_ = self.sb(st, "rp_i", [128, CH], I32)
            pf = self.sb(st, "rp_f", [128, CH], F32)
            m1 = self.sb(st, "rp_m1", [128, CH], F32)
            m2 = self.sb(st, "rp_m2", [128, CH], F32)
            sn = self.sb(st, "rp_sn", [128, CH], F32)
            cs = self.sb(st, "rp_cs", [128, CH], F32)
            for c in range(S // CH):
                sl = slice(c * CH, (c + 1) * CH)
                P.dma("sp", pi_[:], self.pos[0:1, sl].partition_broadcast(128), w=["rp_i"])
                P.op("dve", lambda e: e.tensor_copy(pf[:], pi_[:]), r=["rp_i"], w=["rp_f"])
                P.op("dve", lambda e: e.tensor_scalar(m1[:], pf[:], self.invf[:, 0:1], None, ALU.mult), r=["rp_f", "invf"], w=["rp_m1"])
                self.sin_reduced(m1, "rp_m1", sn, "rp_sn", pi_, "rp_i", m2, "rp_m2", 0.0)
                self.sin_reduced(m1, "rp_m1", cs, "rp_cs", pi_, "rp_i", m2, "rp_m2", math.pi / 2)
                P.dma("sp", self.sinT[:, sl], sn[:], r=["rp_sn"], w=["sinT"])
                P.dma("sp", self.cosT[:, sl], cs[:], r=["rp_cs"], w=["cosT"])
            P.barrier()

    def norm_transpose(self, st, src, T0, ntok, gain_row, hnT, key, pfx):
        P = self.P
        gb = self.sb(st, pfx + "gb", [128, D], F32)
        P.dma("sp", gb[:], gain_row.partition_broadcast(128), w=[pfx + "gb"])
        hb = [self.sb(st, pfx + "hb%d" % i, [128, D], F32) for i in range(2)]
        junk = self.sb(st, pfx + "junk", [128, D], BF16)
        ss = [self.sb(st, pfx + "ss%d" % i, [128, 1], F32) for i in range(2)]
        rs = [self.sb(st, pfx + "rs%d" % i, [128, 1], F32) for i in range(2)]
        xn = [self.sb(st, pfx + "xn%d" % i, [128, D], BF16) for i in range(2)]
        pT = [self.ps(st, pfx + "pT%d" % i, [128, 4, 128], BF16) for i in range(2)]
        for tt in range(ntok // 128):
            b = tt % 2
            hbk, ssk, rsk, xnk = pfx + "hb%d" % b, pfx + "ss%d" % b, pfx + "rs%d" % b, pfx + "xn%d" % b
            t0 = T0 + tt * 128
            P.dma("sp", hb[b][:], src[t0:t0 + 128, :], w=[hbk])
            P.op("act", lambda e, b=b: e.activation(out=junk[:], in_=hb[b][:], func=AF.Square, accum_out=ss[b][:, 0:1]),
                 r=[hbk], w=[pfx + "junk", ssk])
            P.op("act", lambda e, b=b: e.activation(out=rs[b][:], in_=ss[b][:], func=AF.Sqrt, scale=1.0 / D, bias=self.epsb[:, 0:1]), r=[ssk, "epsb"], w=[rsk])
            P.op("dve", lambda e, b=b: e.reciprocal(rs[b][:], rs[b][:]), r=[rsk], w=[rsk])
            P.op("dve", lambda e, b=b: e.scalar_tensor_tensor(xn[b][:], hb[b][:], rs[b][:, 0:1], gb[:], ALU.mult, ALU.mult),
                 r=[hbk, rsk, pfx + "gb"], w=[xnk])
            for g4 in range(4):
                pb = (tt * 4 + g4) % 2
                pk = pfx + "pT%d" % pb
                for j in range(4):
                    kc = g4 * 4 + j
                    P.op("pe", lambda e, b=b, pb=pb, j=j, kc=kc: e.transpose(pT[pb][:, j, :], xn[b][:, kc * 128:(kc + 1) * 128], self.ident_b[:]),
                         r=[xnk, "ident_b"], w=[pk])
                eng = "act" if g4 % 2 == 0 else "dve"
                if eng == "act":
                    P.op("act", lambda e, pb=pb, g4=g4, tt=tt: e.copy(hnT[:, g4 * 4:(g4 + 1) * 4, tt * 128:(tt + 1) * 128], pT[pb][:]),
                         r=[pk], w=[(key, tt)])
                else:
                    P.op("dve", lambda e, pb=pb, g4=g4, tt=tt: e.tensor_copy(hnT[:, g4 * 4:(g4 + 1) * 4, tt * 128:(tt + 1) * 128], pT[pb][:]),
                         r=[pk], w=[(key, tt)])

    def stage_a(self, l):
        P, S, TS = self.P, self.S, self.TS
        nsub = TS // 512
        ntt = TS // 128
        for sup in range(S // TS):
            T0 = sup * TS
            with contextlib.ExitStack() as st:
                hnT = self.sb(st, "a_hnT", [128, 16, TS], BF16)
                with contextlib.ExitStack() as st2:
                    self.norm_transpose(st2, self.h, T0, TS, self.norm_mix[l:l + 1, :], hnT, "a_hnT", "an_")
                    P.barrier()
                if self.stop == "norm":
                    self.dbg_hnT = self.scr("dbg_hnT", [128, 16, TS], BF16)
                    P.dma("sp", self.dbg_hnT, hnT[:], r=[("a_hnT", tt) for tt in range(ntt)], w=["dbg_hnT"])
                    P.barrier()
                    return
                hkeys = [("a_hnT", tt) for tt in range(ntt)]
                cosb = self.sb(st, "a_cos", [128, TS], F32)
                sinb = self.sb(st, "a_sin", [128, TS], F32)
                P.dma("sp", cosb[:], self.cosT[:, T0:T0 + TS], r=["cosT"], w=["a_cos"])
                P.dma("sp", sinb[:], self.sinT[:, T0:T0 + TS], r=["sinT"], w=["a_sin"])
                gq = self.sb(st, "a_gq", [128, 2], F32)
                import os
                DBG = os.environ.get("DBG", "")
                if "nogq" in DBG:
                    P.op("dve", lambda e: e.memset(gq[:], 1.0), w=["a_gq"])
                else:
                    P.dma("sp", gq[:, 0:1], self.q_norm[l:l + 1, :].rearrange("o p -> p o"), w=["a_gq"])
                    P.dma("sp", gq[:, 1:2], self.k_norm[l:l + 1, :].rearrange("o p -> p o"), w=["a_gq"])
                wb = [self.sb(st, "a_wb%d" % i, [128, 16, 256], BF16) for i in range(3)]
                psm = [self.ps(st, "a_ps%d" % i, [128, 512], F32) for i in range(3)]
                ps_ss = self.ps(st, "a_psss", [128, 512], F32)
                ps_rot = self.ps(st, "a_psrot", [128, 512], F32)
                sq = self.sb(st, "a_sq", [128, 512], BF16)
                qg = self.sb(st, "a_qg", [128, 512], BF16)
                rstd = self.sb(st, "a_rstd", [128, 512], F32)
                t1 = self.sb(st, "a_t1", [128, 512], F32)
                t2 = self.sb(st, "a_t2", [128, 512], F32)
                ob = [self.sb(st, "a_ob%d" % i, [128, TS], BF16) for i in range(2)]
                wcount = [0]
                mmcount = [0]
                ocount = [0]

                def load_w(col0, ncols):
                    i = wcount[0] % 3
                    wcount[0] += 1
                    src = self.w_in[l, :, col0:col0 + ncols].rearrange("(kc p) n -> p kc n", p=128)
                    P.dma("pool", wb[i][:, :, 0:ncols], src, w=["a_wb%d" % i])
                    return i

                fm_cols = [(j * 128, "qk", j) for j in range(24)] + \
                          [(4608 + j * 128, "u", j) for j in range(8)] + \
                          [(5632 + j * 128, "g", j) for j in range(32)]
                for pair in range(len(fm_cols) // 2):
                    if self.stop == 'a1' and pair not in (0, 12, 16):
                        continue
                    col0 = fm_cols[2 * pair][0]
                    wi = load_w(col0, 256)
                    for half in range(2):
                        _, kind, j = fm_cols[2 * pair + half]
                        oi = ocount[0] % 2
                        ocount[0] += 1
                        okey = "a_ob%d" % oi
                        for sub in range(nsub):
                            pi = mmcount[0] % 3
                            mmcount[0] += 1
                            pk = "a_ps%d" % pi
                            tsl = slice(sub * 512, (sub + 1) * 512)
                            for kc in range(16):
                                P.op("pe", lambda e, pi=pi, wi=wi, half=half, kc=kc, tsl=tsl: e.matmul(
                                    psm[pi][:], wb[wi][:, kc, half * 128:(half + 1) * 128], hnT[:, kc, tsl],
                                    start=(kc == 0), stop=(kc == 15)),
                                    r=["a_wb%d" % wi] + hkeys[sub * 4:(sub + 1) * 4], w=[pk])
                            if kind == "qk" and "noepi" in DBG:
                                P.op("act", lambda e, pi=pi, oi=oi, tsl=tsl: e.copy(ob[oi][:, tsl], psm[pi][:]), r=[pk], w=[okey])
                            elif kind == "qk":
                                gi = 0 if j < 12 else 1
                                P.op("act", lambda e, pi=pi: e.activation(out=sq[:], in_=psm[pi][:], func=AF.Square), r=[pk], w=["a_sq"])
                                P.op("dve", lambda e, pi=pi, gi=gi: e.tensor_scalar(qg[:], psm[pi][:], gq[:, gi:gi + 1], None, ALU.mult),
                                     r=[pk, "a_gq"], w=["a_qg"])
                                P.op("pe", lambda e: e.matmul(ps_ss[:], self.ones_b[:], sq[:], start=True, stop=True),
                                     r=["ones_b", "a_sq"], w=["a_psss"])
                                P.op("pe", lambda e: e.matmul(ps_rot[:], self.rmat_b[:], qg[:], start=True, stop=True),
                                     r=["rmat_b", "a_qg"], w=["a_psrot"])
                                if "e1" in DBG:
                                    P.op("act", lambda e, oi=oi, tsl=tsl: e.copy(ob[oi][:, tsl], ps_rot[:]), r=["a_psrot"], w=[okey])
                                    P.op("act", lambda e, oi=oi, tsl=tsl: e.copy(t1[:], ps_ss[:]), r=["a_psss"], w=["a_t1"])
                                    continue
                                P.op("act", lambda e: e.activation(out=rstd[:], in_=ps_ss[:], func=AF.Sqrt, scale=1.0 / HD, bias=self.epsb[:, 0:1]),
                                     r=["a_psss", "epsb"], w=["a_rstd"])
                                if "e2" in DBG:
                                    P.op("dve", lambda e: e.reciprocal(t1[:], rstd[:]), r=["a_rstd"], w=["a_t1"])
                                    P.op("act", lambda e, oi=oi, tsl=tsl: e.copy(ob[oi][:, tsl], ps_rot[:]), r=["a_psrot"], w=[okey])
                                    continue
                                P.op("dve", lambda e: e.reciprocal(rstd[:], rstd[:]), r=["a_rstd"], w=["a_rstd"])
                                P.op("dve", lambda e, tsl=tsl: e.tensor_tensor(t1[:], qg[:], cosb[:, tsl], ALU.mult), r=["a_qg", "a_cos"], w=["a_t1"])
                                P.op("dve", lambda e, tsl=tsl: e.tensor_tensor(t2[:], ps_rot[:], sinb[:, tsl], ALU.mult), r=["a_psrot", "a_sin"], w=["a_t2"])
                                P.op("dve", lambda e: e.tensor_tensor(t1[:], t1[:], t2[:], ALU.add), r=["a_t1", "a_t2"], w=["a_t1"])
                                P.op("dve", lambda e, oi=oi, tsl=tsl: e.tensor_tensor(ob[oi][:, tsl], t1[:], rstd[:], ALU.mult),
                                     r=["a_t1", "a_rstd"], w=[okey])
                            elif kind == "u":
                                P.op("act", lambda e, pi=pi, oi=oi, tsl=tsl: e.copy(ob[oi][:, tsl], psm[pi][:]), r=[pk], w=[okey])
                            else:
                                P.op("act", lambda e, pi=pi, oi=oi, tsl=tsl: e.activation(out=ob[oi][:, tsl], in_=psm[pi][:], func=AF.Sigmoid),
                                     r=[pk], w=[okey])
                        if kind == "qk":
                            P.dma("sp", self.qkT[j, :, T0:T0 + TS], ob[oi][:], r=[okey], w=["qkT"])
                        elif kind == "u":
                            P.dma("sp", self.uT[j * 128:(j + 1) * 128, T0:T0 + TS], ob[oi][:], r=[okey], w=["uT"])
                        else:
                            P.dma("sp", self.gT[j * 128:(j + 1) * 128, T0:T0 + TS], ob[oi][:], r=[okey], w=["gT"])
                vo = [self.sb(st, "a_vo%d" % i, [128, 256], BF16) for i in range(2)]
                vcount = 0
                for c in range(6):
                    if self.stop == 'a1':
                        continue
                    wi = load_w(3072 + c * 256, 256)
                    for tt in range(ntt):
                        pi = mmcount[0] % 3
                        mmcount[0] += 1
                        pk = "a_ps%d" % pi
                        for kc in range(16):
                            P.op("pe", lambda e, pi=pi, wi=wi, kc=kc, tt=tt: e.matmul(
                                psm[pi][:, 0:256], hnT[:, kc, tt * 128:(tt + 1) * 128], wb[wi][:, kc, :],
                                start=(kc == 0), stop=(kc == 15)),
                                r=["a_wb%d" % wi, ("a_hnT", tt)], w=[pk])
                        vi = vcount % 2
                        vcount += 1
                        P.op("act", lambda e, pi=pi, vi=vi: e.copy(vo[vi][:], psm[pi][:, 0:256]), r=[pk], w=["a_vo%d" % vi])
                        P.dma("sp", self.v[T0 + tt * 128:T0 + (tt + 1) * 128, c * 256:(c + 1) * 256], vo[vi][:],
                              r=["a_vo%d" % vi], w=["v"])
                P.barrier()
        return

    def stage_attn(self, l):
        P, S = self.P, self.S
        scale = float(HD) ** -0.5
        with contextlib.ExitStack() as st:
            amask = self.sb(st, "t_amask", [128, 3, 128], BF16)
            P.dma("pool", amask[:], self.c_amask.rearrange("p (j q) -> p j q", j=3), w=["t_amask"])
            acc_n = self.sb(st, "t_accn", [128, S], F32)
            acc_d = self.sb(st, "t_accd", [128, S], F32)
            qT = self.sb(st, "t_qT", [128, S], BF16)
            kT = self.sb(st, "t_kT", [128, S], BF16)
            vt = self.sb(st, "t_vt", [128, S // 128, 128], BF16)
            es = [self.sb(st, "t_es%d" % i, [128, 3, 128], BF16) for i in range(2)]
            em = [self.sb(st, "t_em%d" % i, [128, 3, 128], BF16) for i in range(2)]
            ps_s = [self.ps(st, "t_pss%d" % i, [128, 3, 128], F32) for i in range(2)]
            ps_o = [self.ps(st, "t_pso%d" % i, [128, 128], F32) for i in range(2)]
            ps_d = [self.ps(st, "t_psd%d" % i, [128, 128], F32) for i in range(2)]
            ob = self.sb(st, "t_ob", [128, S], BF16)
            cnt = 0
            for slot in range(4):
                for g, d in enumerate((1, 4, 16)):
                    head = 4 * g + slot
                    n = S // d // 128
                    P.dma("sp", qT[:], self.qkT[head, :, :], r=["qkT"], w=["t_qT"])
                    P.dma("sp", kT[:], self.qkT[12 + head, :, :], r=["qkT"], w=["t_kT"])
                    vh = self.v[:, head * 128:(head + 1) * 128].rearrange("(jt jp d) c -> d jp jt c", d=d, jp=128)
                    for r in range(d):
                        P.dma("sp", vt[:, r * n:(r + 1) * n, :], vh[r], r=["v"], w=["t_vt"])
                    qv = qT[:].rearrange("p (j d) -> p d j", d=d)
                    kv = kT[:].rearrange("p (j d) -> p d j", d=d)
                    anv = acc_n[:].rearrange("p (j d) -> p d j", d=d)
                    adv = acc_d[:].rearrange("p (j d) -> p d j", d=d)
                    for r in range(d):
                        for i in range(n):
                            b = cnt % 2
                            cnt += 1
                            kts = [kt for kt in (i - 1, i, i + 1) if 0 <= kt < n]
                            jlo, jhi = kts[0] - i + 1, kts[-1] - i + 2
                            qs = slice(i * 128, (i + 1) * 128)
                            for kt in kts:
                                jj = kt - i + 1
                                P.op("pe", lambda e, b=b, jj=jj, r=r, kt=kt, qs=qs, kv=kv, qv=qv: e.matmul(
                                    ps_s[b][:, jj, :], kv[:, r, kt * 128:(kt + 1) * 128], qv[:, r, qs], start=True, stop=True),
                                    r=["t_kT", "t_qT"], w=["t_pss%d" % b])
                            P.op("act", lambda e, b=b, jlo=jlo, jhi=jhi: e.activation(out=es[b][:, jlo:jhi, :], in_=ps_s[b][:, jlo:jhi, :], func=AF.Exp, scale=scale),
                                 r=["t_pss%d" % b], w=["t_es%d" % b])
                            P.op("dve", lambda e, b=b, jlo=jlo, jhi=jhi: e.tensor_tensor(em[b][:, jlo:jhi, :], es[b][:, jlo:jhi, :], amask[:, jlo:jhi, :], ALU.mult),
                                 r=["t_es%d" % b, "t_amask"], w=["t_em%d" % b])
                            for kt in kts:
                                jj = kt - i + 1
                                P.op("pe", lambda e, b=b, jj=jj, r=r, kt=kt, n=n, kts=kts: e.matmul(
                                    ps_o[b][:], vt[:, r * n + kt, :], em[b][:, jj, :], start=(kt == kts[0]), stop=(kt == kts[-1])),
                                    r=["t_vt", "t_em%d" % b], w=["t_pso%d" % b])
                            for kt in kts:
                                jj = kt - i + 1
                                P.op("pe", lambda e, b=b, jj=jj, kt=kt, kts=kts: e.matmul(
                                    ps_d[b][:], self.ones_b[:], em[b][:, jj, :], start=(kt == kts[0]), stop=(kt == kts[-1])),
                                    r=["ones_b", "t_em%d" % b], w=["t_psd%d" % b])
                            if g == 0:
                                P.op("act", lambda e, b=b, r=r, qs=qs, anv=anv: e.copy(anv[:, r, qs], ps_o[b][:]), r=["t_pso%d" % b], w=["t_accn"])
                                P.op("dve", lambda e, b=b, r=r, qs=qs, adv=adv: e.tensor_copy(adv[:, r, qs], ps_d[b][:]), r=["t_psd%d" % b], w=["t_accd"])
                            else:
                                P.op("dve", lambda e, b=b, r=r, qs=qs, anv=anv: e.tensor_tensor(anv[:, r, qs], anv[:, r, qs], ps_o[b][:], ALU.add),
                                     r=["t_pso%d" % b, "t_accn"], w=["t_accn"])
                                P.op("dve", lambda e, b=b, r=r, qs=qs, adv=adv: e.tensor_tensor(adv[:, r, qs], adv[:, r, qs], ps_d[b][:], ALU.add),
                                     r=["t_psd%d" % b, "t_accd"], w=["t_accd"])
                P.op("dve", lambda e: e.reciprocal(acc_d[:], acc_d[:]), r=["t_accd"], w=["t_accd"])
                P.op("dve", lambda e: e.tensor_tensor(ob[:], acc_n[:], acc_d[:], ALU.mult), r=["t_accn", "t_accd"], w=["t_ob"])
                P.dma("sp", self.attT[slot * 128:(slot + 1) * 128, :], ob[:], r=["t_ob"], w=["attT"])
            P.barrier()

    def stage_s5(self, l):
        P, S = self.P, self.S
        Lc = 512
        nch = S // Lc
        GC = 1.5957691216057308
        with contextlib.ExitStack() as st:
            sb = lambda n, shp, dt: self.sb(st, n, shp, dt)
            ioti = sb("s_ioti", [128, Lc + 1], I32)
            iot = sb("s_iot", [128, Lc + 1], F32)
            P.op("pool", lambda e: e.iota(ioti[:], pattern=[[1, Lc + 1]], base=0, channel_multiplier=0), w=["s_ioti"])
            P.op("dve", lambda e: e.tensor_copy(iot[:], ioti[:]), r=["s_ioti"], w=["s_iot"])
            prm = {}
            pti = sb("s_pti", [128, 32], I32)
            ptf = sb("s_ptf", [128, 32], F32)
            for dr in range(2):
                names = ["are", "aim", "ldt", "dt", "ar", "th", "rho", "sn", "cs", "lbr", "lbi", "den", "nr", "cr", "ci", "nci", "t"]
                T = {n: sb("s_%s%d" % (n, dr), [128, 32], F32) for n in names}
                K = {n: "s_%s%d" % (n, dr) for n in names}
                P.dma("sp", T["are"][:], self.s5_are[l, dr], w=[K["are"]])
                P.dma("sp", T["aim"][:], self.s5_aim[l, dr], w=[K["aim"]])
                P.dma("sp", T["ldt"][:], self.s5_ldt[l, dr], w=[K["ldt"]])
                P.op("act", lambda e, T=T: e.activation(out=T["dt"][:], in_=T["ldt"][:], func=AF.Exp), r=[K["ldt"]], w=[K["dt"]])
                tt = lambda o, a, b, op, T=T, K=K: P.op("dve", lambda e: e.tensor_tensor(T[o][:], T[a][:], T[b][:], op), r=[K[a], K[b]], w=[K[o]])
                tt("ar", "are", "dt", ALU.mult)
                tt("th", "aim", "dt", ALU.mult)
                P.op("act", lambda e, T=T: e.activation(out=T["rho"][:], in_=T["ar"][:], func=AF.Exp), r=[K["ar"]], w=[K["rho"]])
                self.sin_reduced(T["th"], K["th"], T["sn"], K["sn"], pti, "s_pti", ptf, "s_ptf", 0.0)
                self.sin_reduced(T["th"], K["th"], T["cs"], K["cs"], pti, "s_pti", ptf, "s_ptf", math.pi / 2)
                tt("lbr", "rho", "cs", ALU.mult)
                tt("lbi", "rho", "sn", ALU.mult)
                tt("t", "are", "are", ALU.mult)
                tt("den", "aim", "aim", ALU.mult)
                tt("den", "den", "t", ALU.add)
                P.op("dve", lambda e, T=T: e.reciprocal(T["den"][:], T["den"][:]), r=[K["den"]], w=[K["den"]])
                P.op("dve", lambda e, T=T: e.tensor_scalar(T["nr"][:], T["lbr"][:], -1.0, None, ALU.add), r=[K["lbr"]], w=[K["nr"]])
                tt("cr", "nr", "are", ALU.mult)
                tt("t", "lbi", "aim", ALU.mult)
                tt("cr", "cr", "t", ALU.add)
                tt("cr", "cr", "den", ALU.mult)
                tt("ci", "lbi", "are", ALU.mult)
                tt("t", "nr", "aim", ALU.mult)
                tt("ci", "ci", "t", ALU.subtract)
                tt("ci", "ci", "den", ALU.mult)
                P.op("dve", lambda e, T=T: e.tensor_scalar(T["nci"][:], T["ci"][:], -1.0, None, ALU.mult), r=[K["ci"]], w=[K["nci"]])
                prm[dr] = (T, K)
            uP = sb("s_uP", [32, S], BF16)
            yacc = sb("s_yacc", [32, S], F32)
            dcol = sb("s_dcol", [32, 1], F32)
            Bre = sb("s_Bre", [32, 128], BF16)
            Bim = sb("s_Bim", [32, 128], BF16)
            Cre = sb("s_Cre", [128, 32], F32)
            Cim = sb("s_Cim", [128, 32], F32)
            Ct = sb("s_Ct", [128, 32], F32)
            Cpr = sb("s_Cpr", [128, 32], BF16)
            Cni = sb("s_Cni", [128, 32], BF16)
            ang = sb("s_ang", [128, Lc + 1], F32)
            tsn = sb("s_tsn", [128, Lc + 1], F32)
            tcs = sb("s_tcs", [128, Lc + 1], F32)
            tti = sb("s_tti", [128, Lc + 1], I32)
            ttf = sb("s_ttf", [128, Lc + 1], F32)
            rhob = sb("s_rhob", [128, Lc], F32)
            br = [sb("s_br%d" % i, [128, Lc], F32) for i in range(2)]
            bi = [sb("s_bi%d" % i, [128, Lc], F32) for i in range(2)]
            q1 = [sb("s_q1%d" % i, [128, Lc], F32) for i in range(2)]
            q2 = [sb("s_q2%d" % i, [128, Lc], F32) for i in range(2)]
            q3 = [sb("s_q3%d" % i, [128, Lc], F32) for i in range(2)]
            q4 = [sb("s_q4%d" % i, [128, Lc], F32) for i in range(2)]
            btr = sb("s_btr", [128, Lc], F32)
            bti = sb("s_bti", [128, Lc], F32)
            d3 = sb("s_d3", [128, Lc], F32)
            d4 = sb("s_d4", [128, Lc], F32)
            ytmp = sb("s_ytmp", [32, Lc], F32)
            wr = [sb("s_wr%d" % i, [128, Lc], F32) for i in range(2)]
            wi_ = [sb("s_wi%d" % i, [128, Lc], F32) for i in range(2)]
            d1 = sb("s_d1", [128, Lc], F32)
            d2 = sb("s_d2", [128, Lc], F32)
            xr = [sb("s_xr%d" % i, [128, Lc], BF16) for i in range(2)]
            xi = [sb("s_xi%d" % i, [128, Lc], BF16) for i in range(2)]
            ini = [sb("s_ini%d" % i, [128, 2], F32) for i in range(2)]
            it_ = sb("s_it", [128, 1], F32)
            GW = min(S, 2048)
            g1 = sb("s_g1", [32, GW], F32)
            g2 = sb("s_g2", [32, GW], F32)
            yo = sb("s_yo", [32, GW], BF16)
            ps_br = [self.ps(st, "s_psbr%d" % i, [128, Lc], F32) for i in range(2)]
            ps_bi = [self.ps(st, "s_psbi%d" % i, [128, Lc], F32) for i in range(2)]
            ps_y = [self.ps(st, "s_psy%d" % i, [32, Lc], F32) for i in range(2)]
            cnt = 0
            import os
            for gp in range(1 if 's5one' in os.environ.get('DBG', '') else 32):
                P.dma("sp", uP[:], self.uT[gp * 32:(gp + 1) * 32, :], r=["uT"], w=["s_uP"])
                P.dma("sp", dcol[:], self.s5_d[l, gp], w=["s_dcol"])
                for dr in range(2):
                    T, K = prm[dr]
                    col = lambda n, T=T, gp=gp: T[n][:, gp:gp + 1]
                    P.dma("pool", Bre[:], self.s5_bre[l, dr, gp], w=["s_Bre"])
                    P.dma("pool", Bim[:], self.s5_bim[l, dr, gp], w=["s_Bim"])
                    P.dma("sp", Cre[:], self.s5_cre[l, dr, gp], w=["s_Cre"])
                    P.dma("sp", Cim[:], self.s5_cim[l, dr, gp], w=["s_Cim"])
                    P.op("dve", lambda e, col=col: e.tensor_scalar(Ct[:], Cim[:], col("ci"), None, ALU.mult), r=["s_Cim", K["ci"]], w=["s_Ct"])
                    P.op("dve", lambda e, col=col: e.scalar_tensor_tensor(Cpr[:], Cre[:], col("cr"), Ct[:], ALU.mult, ALU.subtract),
                         r=["s_Cre", "s_Ct", K["cr"]], w=["s_Cpr"])
                    P.op("dve", lambda e, col=col: e.tensor_scalar(Ct[:], Cim[:], col("cr"), None, ALU.mult), r=["s_Cim", K["cr"]], w=["s_Ct"])
                    P.op("dve", lambda e, col=col: e.scalar_tensor_tensor(Cni[:], Cre[:], col("nci"), Ct[:], ALU.mult, ALU.subtract),
                         r=["s_Cre", "s_Ct", K["nci"]], w=["s_Cni"])
                    P.op("dve", lambda e, col=col: e.tensor_scalar(ang[:], iot[:], col("th"), None, ALU.mult), r=["s_iot", K["th"]], w=["s_ang"])
                    self.sin_reduced(ang, "s_ang", tsn, "s_tsn", tti, "s_tti", ttf, "s_ttf", 0.0)
                    self.sin_reduced(ang, "s_ang", tcs, "s_tcs", tti, "s_tti", ttf, "s_ttf", math.pi / 2)
                    P.op("dve", lambda e, col=col: e.tensor_scalar(rhob[:], iot[:, 0:Lc], 0.0, col("rho"), ALU.mult, ALU.add),
                         r=["s_iot", K["rho"]], w=["s_rhob"])
                    sn, cs = tsn[:, 0:Lc], tcs[:, 0:Lc]
                    snL, csL = tsn[:, Lc:Lc + 1], tcs[:, Lc:Lc + 1]
                    prev = None
                    rv = (lambda ap: ap) if dr == 0 else (lambda ap: ap[:, ::-1])
                    bufs = []
                    for ci_ in range(nch):
                        bufs.append(cnt % 2)
                        cnt += 1

                    def front(ci_, dr=dr, rv=rv, cs=cs, sn=sn, bufs=bufs):
                        c = ci_ if dr == 0 else nch - 1 - ci_
                        b = bufs[ci_]
                        csl = slice(c * Lc, (c + 1) * Lc)
                        kb = lambda n, b=b: "s_%s%d" % (n, b)
                        P.op("pe", lambda e, b=b, csl=csl: e.matmul(ps_br[b][:], Bre[:], uP[:, csl], start=True, stop=True), r=["s_Bre", "s_uP"], w=[kb("psbr")])
                        P.op("pe", lambda e, b=b, csl=csl: e.matmul(ps_bi[b][:], Bim[:], uP[:, csl], start=True, stop=True), r=["s_Bim", "s_uP"], w=[kb("psbi")])
                        P.op("act", lambda e, b=b, rv=rv: e.copy(br[b][:], rv(ps_br[b][:])), r=[kb("psbr")], w=[kb("br")])
                        P.op("act", lambda e, b=b, rv=rv: e.copy(bi[b][:], rv(ps_bi[b][:])), r=[kb("psbi")], w=[kb("bi")])
                        P.op("pool", lambda e, b=b, cs=cs: e.tensor_tensor(q1[b][:], br[b][:], cs, ALU.mult), r=[kb("br"), "s_tcs"], w=[kb("q1")])
                        P.op("pool", lambda e, b=b, sn=sn: e.tensor_tensor(q2[b][:], bi[b][:], sn, ALU.mult), r=[kb("bi"), "s_tsn"], w=[kb("q2")])
                        P.op("pool", lambda e, b=b, cs=cs: e.tensor_tensor(q3[b][:], bi[b][:], cs, ALU.mult), r=[kb("bi"), "s_tcs"], w=[kb("q3")])
                        P.op("pool", lambda e, b=b, sn=sn: e.tensor_tensor(q4[b][:], br[b][:], sn, ALU.mult), r=[kb("br"), "s_tsn"], w=[kb("q4")])

                    def back(ci_, prev, dr=dr, cs=cs, sn=sn, snL=snL, csL=csL, bufs=bufs):
                        c = ci_ if dr == 0 else nch - 1 - ci_
                        b = bufs[ci_]
                        csl = slice(c * Lc, (c + 1) * Lc)
                        kb = lambda n, b=b: "s_%s%d" % (n, b)
                        P.op("dve", lambda e, b=b: e.tensor_tensor(btr[:], q1[b][:], q2[b][:], ALU.add), r=[kb("q1"), kb("q2")], w=["s_btr"])
                        P.op("dve", lambda e, b=b: e.tensor_tensor(bti[:], q3[b][:], q4[b][:], ALU.subtract), r=[kb("q3"), kb("q4")], w=["s_bti"])
                        if prev is None:
                            P.op("dve", lambda e, b=b: e.memset(ini[b][:], 0.0), w=[kb("ini")])
                        else:
                            pb = prev
                            wre, wie = wr[pb][:, Lc - 1:Lc], wi_[pb][:, Lc - 1:Lc]
                            P.op("dve", lambda e, wie=wie, snL=snL: e.tensor_tensor(it_[:], wie, snL, ALU.mult), r=["s_wi%d" % pb, "s_tsn"], w=["s_it"])
                            P.op("dve", lambda e, b=b, wre=wre, csL=csL: e.tensor_tensor(ini[b][:, 0:1], wre, csL, ALU.mult), r=["s_wr%d" % pb, "s_tcs"], w=[kb("ini")])
                            P.op("dve", lambda e, b=b: e.tensor_tensor(ini[b][:, 0:1], ini[b][:, 0:1], it_[:], ALU.subtract), r=[kb("ini"), "s_it"], w=[kb("ini")])
                            P.op("dve", lambda e, wie=wie, csL=csL: e.tensor_tensor(it_[:], wie, csL, ALU.mult), r=["s_wi%d" % pb, "s_tcs"], w=["s_it"])
                            P.op("dve", lambda e, b=b, wre=wre, snL=snL: e.tensor_tensor(ini[b][:, 1:2], wre, snL, ALU.mult), r=["s_wr%d" % pb, "s_tsn"], w=[kb("ini")])
                            P.op("dve", lambda e, b=b: e.tensor_tensor(ini[b][:, 1:2], ini[b][:, 1:2], it_[:], ALU.add), r=[kb("ini"), "s_it"], w=[kb("ini")])
                        P.op("dve", lambda e, b=b: e.tensor_tensor_scan(wr[b][:], rhob[:], btr[:], ini[b][:, 0:1], ALU.mult, ALU.add),
                             r=["s_rhob", "s_btr", kb("ini")], w=[kb("wr")])
                        P.op("dve", lambda e, b=b: e.tensor_tensor_scan(wi_[b][:], rhob[:], bti[:], ini[b][:, 1:2], ALU.mult, ALU.add),
                             r=["s_rhob", "s_bti", kb("ini")], w=[kb("wi")])
                        P.op("dve", lambda e, b=b, cs=cs: e.tensor_tensor(d1[:], wr[b][:], cs, ALU.mult), r=[kb("wr"), "s_tcs"], w=["s_d1"])
                        P.op("dve", lambda e, b=b, sn=sn: e.tensor_tensor(d2[:], wi_[b][:], sn, ALU.mult), r=[kb("wi"), "s_tsn"], w=["s_d2"])
                        P.op("dve", lambda e, b=b: e.tensor_tensor(xr[b][:], d1[:], d2[:], ALU.subtract), r=["s_d1", "s_d2"], w=[kb("xr")])
                        P.op("dve", lambda e, b=b, sn=sn: e.tensor_tensor(d3[:], wr[b][:], sn, ALU.mult), r=[kb("wr"), "s_tsn"], w=["s_d3"])
                        P.op("dve", lambda e, b=b, cs=cs: e.tensor_tensor(d4[:], wi_[b][:], cs, ALU.mult), r=[kb("wi"), "s_tcs"], w=["s_d4"])
                        P.op("dve", lambda e, b=b: e.tensor_tensor(xi[b][:], d3[:], d4[:], ALU.add), r=["s_d3", "s_d4"], w=[kb("xi")])
                        P.op("pe", lambda e, b=b: e.matmul(ps_y[b][:], Cpr[:], xr[b][:], start=True, stop=False), r=["s_Cpr", kb("xr")], w=[kb("psy")])
                        P.op("pe", lambda e, b=b: e.matmul(ps_y[b][:], Cni[:], xi[b][:], start=False, stop=True), r=["s_Cni", kb("xi")], w=[kb("psy")])
                        if dr == 0:
                            P.op("act", lambda e, b=b, csl=csl: e.copy(yacc[:, csl], ps_y[b][:]), r=[kb("psy")], w=["s_yacc"])
                        else:
                            P.op("act", lambda e, b=b: e.copy(ytmp[:], ps_y[b][:]), r=[kb("psy")], w=["s_ytmp"])
                            P.op("pool", lambda e, csl=csl: e.tensor_tensor(yacc[:, csl][:, ::-1], yacc[:, csl][:, ::-1], ytmp[:], ALU.add),
                                 r=["s_ytmp", "s_yacc"], w=["s_yacc"])
                        return b

                    front(0)
                    prev = None
                    for ci_ in range(nch):
                        if ci_ + 1 < nch:
                            front(ci_ + 1)
                        prev = back(ci_, prev)
                for gc in range(S // GW):
                    gsl = slice(gc * GW, (gc + 1) * GW)
                    P.op("dve", lambda e, gsl=gsl: e.scalar_tensor_tensor(g1[:], uP[:, gsl], dcol[:, 0:1], yacc[:, gsl], ALU.mult, ALU.add), r=["s_uP", "s_dcol", "s_yacc"], w=["s_g1"])
                    if self.stop == "s5raw":
                        P.dma("sp", self.dbg_y[gp * 32:(gp + 1) * 32, gsl], g1[:], r=["s_g1"], w=["dbg_y"])
                    P.op("pool", lambda e: e.tensor_tensor(g2[:], g1[:], g1[:], ALU.mult), r=["s_g1"], w=["s_g2"])
                    P.op("pool", lambda e: e.tensor_scalar(g2[:], g2[:], 0.044715, 1.0, ALU.mult, ALU.add), r=["s_g2"], w=["s_g2"])
                    P.op("pool", lambda e: e.tensor_tensor(g2[:], g2[:], g1[:], ALU.mult), r=["s_g2", "s_g1"], w=["s_g2"])
                    P.op("act", lambda e: e.activation(out=g2[:], in_=g2[:], func=AF.Sigmoid, scale=GC), r=["s_g2"], w=["s_g2"])
                    P.op("pool", lambda e: e.tensor_tensor(yo[:], g2[:], g1[:], ALU.mult), r=["s_g2", "s_g1"], w=["s_yo"])
                    P.dma("sp", self.ygT[gp * 32:(gp + 1) * 32, gsl], yo[:], r=["s_yo"], w=["ygT"])
            P.barrier()

    def stage_c1(self, l):
        P, S, TS = self.P, self.S, self.TS
        nsub = TS // 512
        for sup in range(S // TS):
            T0 = sup * TS
            with contextlib.ExitStack() as st:
                sb = lambda n, shp, dt: self.sb(st, n, shp, dt)
                aT = sb("c_aT", [128, 4, TS], BF16)
                yT = sb("c_yT", [128, 8, TS], BF16)
                P.dma("sp", aT[:], self.attT[:, T0:T0 + TS].rearrange("(kc p) t -> p kc t", p=128), r=["attT"], w=["c_aT"])
                P.dma("sp", yT[:], self.ygT[:, T0:T0 + TS].rearrange("(kc p) t -> p kc t", p=128), r=["ygT"], w=["c_yT"])
                wa = [sb("c_wa%d" % i, [128, 4, 128], BF16) for i in range(2)]
                wv = [sb("c_wv%d" % i, [128, 8, 128], BF16) for i in range(2)]
                wg = [sb("c_wg%d" % i, [128, 8, 128], BF16) for i in range(2)]
                ga = [sb("c_ga%d" % i, [128, TS], BF16) for i in range(2)]
                gs = [sb("c_gs%d" % i, [128, TS], BF16) for i in range(2)]
                mo = [sb("c_mo%d" % i, [128, TS], BF16) for i in range(2)]
                sg = sb("c_sg", [128, 512], F32)
                sbr = sb("c_sbr", [128, 512], F32)
                ta = sb("c_ta", [128, 512], F32)
                psA = [self.ps(st, "c_psA%d" % i, [128, 512], F32) for i in range(2)]
                psV = [self.ps(st, "c_psV%d" % i, [128, 512], F32) for i in range(2)]
                psG = [self.ps(st, "c_psG%d" % i, [128, 512], F32) for i in range(2)]
                cnt = 0
                for m in range(16):
                    wb_ = m % 2
                    cs_ = slice(m * 128, (m + 1) * 128)
                    P.dma("pool", wa[wb_][:], self.w_attn_br[l, :, cs_].rearrange("(kc p) n -> p kc n", p=128), w=["c_wa%d" % wb_])
                    P.dma("pool", wv[wb_][:], self.w_ssm_br[l, :, cs_].rearrange("(kc p) n -> p kc n", p=128), w=["c_wv%d" % wb_])
                    P.dma("pool", wg[wb_][:], self.w_ssm_br[l, :, 2048 + m * 128:2048 + (m + 1) * 128].rearrange("(kc p) n -> p kc n", p=128), w=["c_wg%d" % wb_])
                    P.dma("sp", ga[wb_][:], self.gT[m * 128:(m + 1) * 128, T0:T0 + TS], r=["gT"], w=["c_ga%d" % wb_])
                    P.dma("sp", gs[wb_][:], self.gT[2048 + m * 128:2048 + (m + 1) * 128, T0:T0 + TS], r=["gT"], w=["c_gs%d" % wb_])
                    for sub in range(nsub):
                        b = cnt % 2
                        cnt += 1
                        tsl = slice(sub * 512, (sub + 1) * 512)
                        for kc in range(4):
                            P.op("pe", lambda e, b=b, wb_=wb_, kc=kc, tsl=tsl: e.matmul(psA[b][:], wa[wb_][:, kc, :], aT[:, kc, tsl], start=(kc == 0), stop=(kc == 3)),
                                 r=["c_wa%d" % wb_, "c_aT"], w=["c_psA%d" % b])
                        for kc in range(8):
                            P.op("pe", lambda e, b=b, wb_=wb_, kc=kc, tsl=tsl: e.matmul(psV[b][:], wv[wb_][:, kc, :], yT[:, kc, tsl], start=(kc == 0), stop=(kc == 7)),
                                 r=["c_wv%d" % wb_, "c_yT"], w=["c_psV%d" % b])
                        for kc in range(8):
                            P.op("pe", lambda e, b=b, wb_=wb_, kc=kc, tsl=tsl: e.matmul(psG[b][:], wg[wb_][:, kc, :], yT[:, kc, tsl], start=(kc == 0), stop=(kc == 7)),
                                 r=["c_wg%d" % wb_, "c_yT"], w=["c_psG%d" % b])
                        P.op("act", lambda e, b=b: e.activation(out=sg[:], in_=psG[b][:], func=AF.Sigmoid), r=["c_psG%d" % b], w=["c_sg"])
                        P.op("dve", lambda e, b=b: e.tensor_tensor(sbr[:], psV[b][:], sg[:], ALU.mult), r=["c_psV%d" % b, "c_sg"], w=["c_sbr"])
                        P.op("dve", lambda e, wb_=wb_, tsl=tsl: e.tensor_tensor(sbr[:], sbr[:], gs[wb_][:, tsl], ALU.mult), r=["c_sbr", "c_gs%d" % wb_], w=["c_sbr"])
                        P.op("dve", lambda e, b=b, wb_=wb_, tsl=tsl: e.tensor_tensor(ta[:], psA[b][:], ga[wb_][:, tsl], ALU.mult), r=["c_psA%d" % b, "c_ga%d" % wb_], w=["c_ta"])
                        P.op("dve", lambda e, wb_=wb_, tsl=tsl: e.tensor_tensor(mo[wb_][:, tsl], ta[:], sbr[:], ALU.add), r=["c_ta", "c_sbr"], w=["c_mo%d" % wb_])
                    P.dma("sp", self.mergedT[m * 128:(m + 1) * 128, T0:T0 + TS], mo[wb_][:], r=["c_mo%d" % wb_], w=["mergedT"])
                P.barrier()

    def stage_c2(self, l):
        P, S = self.P, self.S
        with contextlib.ExitStack() as st:
            sb = lambda n, shp, dt: self.sb(st, n, shp, dt)
            W = sb("o_W", [128, 16, 2048], BF16)
            for c in range(8):
                P.dma("pool", W[:, :, c * 256:(c + 1) * 256], self.w_out[l, :, c * 256:(c + 1) * 256].rearrange("(kc p) n -> p kc n", p=128), w=[("o_W", c)])
            wkeys = [("o_W", c) for c in range(8)]
            mT = [sb("o_mT%d" % i, [128, 16, 128], BF16) for i in range(2)]
            hb = [sb("o_hb%d" % i, [128, 2048], F32) for i in range(2)]
            psm = [self.ps(st, "o_ps%d" % i, [128, 512], F32) for i in range(4)]
            for tt in range(S // 128):
                b = tt % 2
                t0 = tt * 128
                P.dma("sp", mT[b][:], self.mergedT[:, t0:t0 + 128].rearrange("(kc p) t -> p kc t", p=128), r=["mergedT"], w=["o_mT%d" % b])
                P.dma("sp", hb[b][:], self.h[t0:t0 + 128, :], r=["h"], w=["o_hb%d" % b])
                for c4 in range(4):
                    for kc in range(16):
                        P.op("pe", lambda e, b=b, c4=c4, kc=kc: e.matmul(psm[c4][:], mT[b][:, kc, :], W[:, kc, c4 * 512:(c4 + 1) * 512], start=(kc == 0), stop=(kc == 15)),
                             r=["o_mT%d" % b] + wkeys[c4 * 2:c4 * 2 + 2], w=["o_ps%d" % c4])
                    P.op("dve", lambda e, b=b, c4=c4: e.tensor_tensor(hb[b][:, c4 * 512:(c4 + 1) * 512], hb[b][:, c4 * 512:(c4 + 1) * 512], psm[c4][:], ALU.add),
                         r=["o_ps%d" % c4, "o_hb%d" % b], w=["o_hb%d" % b])
                P.dma("sp", self.h[t0:t0 + 128, :], hb[b][:], r=["o_hb%d" % b], w=["h"])
            P.barrier()

    def norm_tile(self, hb, hbk, gb, gbk, xn, xnk, ss, ssk, rs, rsk, junk, junkk):
        P = self.P
        P.op("act", lambda e: e.activation(out=junk[:], in_=hb[:], func=AF.Square, accum_out=ss[:, 0:1]), r=[hbk], w=[junkk, ssk])
        P.op("act", lambda e: e.activation(out=rs[:], in_=ss[:], func=AF.Sqrt, scale=1.0 / D, bias=self.epsb[:, 0:1]), r=[ssk, "epsb"], w=[rsk])
        P.op("dve", lambda e: e.reciprocal(rs[:], rs[:]), r=[rsk], w=[rsk])
        P.op("dve", lambda e: e.scalar_tensor_tensor(xn, hb[:], rs[:, 0:1], gb[:], ALU.mult, ALU.mult), r=[hbk, rsk, gbk], w=[xnk])

    def transpose_tile(self, xn, xnk, pT, pTk, dst_fn, dkey, cnt0=0):
        P = self.P
        for g4 in range(4):
            pb = (cnt0 + g4) % 2
            for j in range(4):
                kc = g4 * 4 + j
                P.op("pe", lambda e, pb=pb, j=j, kc=kc: e.transpose(pT[pb][:, j, :], xn[:, kc * 128:(kc + 1) * 128], self.ident_b[:]),
                     r=[xnk, "ident_b"], w=[pTk % pb])
            if g4 % 2 == 0:
                P.op("act", lambda e, pb=pb, g4=g4: e.copy(dst_fn(g4), pT[pb][:]), r=[pTk % pb], w=[dkey])
            else:
                P.op("dve", lambda e, pb=pb, g4=g4: e.tensor_copy(dst_fn(g4), pT[pb][:]), r=[pTk % pb], w=[dkey])

    def stage_moe(self, l):
        P, S = self.P, self.S
        NT = S // 128
        C = S // 8
        NCT = C // 128
        RW = 2112
        BIG = float(1 << 20)
        NIT = 34
        with contextlib.ExitStack() as st0:
            aff = self.sb(st0, "m_aff", [128, NT, 16], F32)
            posi = self.p_posi
            with contextlib.ExitStack() as st:
                sb = lambda n, shp, dt: self.sb(st, n, shp, dt)
                gb = sb("m_gb", [128, D], F32)
                P.dma("sp", gb[:], self.norm_ffn[l:l + 1, :].partition_broadcast(128), w=["m_gb"])
                wr = sb("m_wr", [128, 16, 16], BF16)
                P.dma("pool", wr[:], self.w_router[l].rearrange("(kc p) n -> p kc n", p=128), w=["m_wr"])
                hb = [sb("m_hb%d" % i, [128, D], F32) for i in range(2)]
                junk = sb("m_junk", [128, D], BF16)
                ss = [sb("m_ss%d" % i, [128, 1], F32) for i in range(2)]
                rs = [sb("m_rs%d" % i, [128, 1], F32) for i in range(2)]
                xrow = [sb("m_xrow%d" % i, [128, RW], BF16) for i in range(2)]
                xT = [sb("m_xT%d" % i, [128, 16, 128], BF16) for i in range(2)]
                ex = sb("m_ex", [128, 16], F32)
                sm = sb("m_sm", [128, 1], F32)
                pT = [self.ps(st, "m_pT%d" % i, [128, 4, 128], BF16) for i in range(2)]
                psl = [self.ps(st, "m_psl%d" % i, [128, 16], F32) for i in range(2)]
                for tt in range(NT):
                    b = tt % 2
                    t0 = tt * 128
                    k = lambda n, b=b: "m_%s%d" % (n, b)
                    P.dma("sp", hb[b][:], self.h[t0:t0 + 128, :], r=["h"], w=[k("hb")])
                    self.norm_tile(hb[b], k("hb"), gb, "m_gb", xrow[b][:, 0:D], k("xrow"), ss[b], k("ss"), rs[b], k("rs"), junk, "m_junk")
                    self.transpose_tile(xrow[b][:, 0:D], k("xrow"), pT, "m_pT%d", (lambda g4, b=b: xT[b][:, g4 * 4:(g4 + 1) * 4, :]), k("xT"), cnt0=tt * 4)
                    for kc in range(16):
                        P.op("pe", lambda e, b=b, kc=kc: e.matmul(psl[b][:], xT[b][:, kc, :], wr[:, kc, :], start=(kc == 0), stop=(kc == 15)),
                             r=[k("xT"), "m_wr"], w=[k("psl")])
                    P.op("act", lambda e, b=b: e.activation(out=ex[:], in_=psl[b][:], func=AF.Exp, accum_out=sm[:, 0:1]), r=[k("psl")], w=["m_ex", "m_sm"])
                    P.op("dve", lambda e: e.reciprocal(sm[:], sm[:]), r=["m_sm"], w=["m_sm"])
                    P.op("dve", lambda e, tt=tt: e.tensor_scalar(aff[:, tt, :], ex[:], sm[:, 0:1], None, ALU.mult), r=["m_ex", "m_sm"], w=["m_aff"])
                    P.op("dve", lambda e, b=b, tt=tt: e.tensor_copy(xrow[b][:, D:D + 32].bitcast(F32), aff[:, tt, :]), r=["m_aff"], w=[k("xrow")])
                    P.op("pool", lambda e, b=b, t0=t0: e.iota(xrow[b][:, D + 32:D + 34].bitcast(I32), pattern=[[0, 1]], base=t0, channel_multiplier=1), w=[k("xrow")])
                    P.dma("sp", self.xrows[t0:t0 + 128, :], xrow[b][:], r=[k("xrow")], w=["xrows"])
                P.barrier()
            with contextlib.ExitStack() as st:
                sb = lambda n, shp, dt: self.sb(st, n, shp, dt)
                ltri = sb("m_ltri", [128, 128], BF16)
                P.dma("pool", ltri[:], self.c_ltri, w=["m_ltri"])
                lo = sb("m_lo", [128, 16], F32)
                hi = sb("m_hi", [128, 16], F32)
                mid = sb("m_mid", [128, 16], F32)
                cmpt = sb("m_cmp", [128, NT, 16], F32)
                cntp = sb("m_cntp", [128, 16], BF16)
                cntpf = sb("m_cntpf", [128, 16], F32)
                gei = sb("m_gei", [128, 16], I32)
                lti = sb("m_lti", [128, 16], I32)
                pst = self.ps(st, "m_pst", [128, 16], F32)
                P.op("dve", lambda e: e.memset(lo[:], 0.0), w=["m_lo"])
                P.op("dve", lambda e: e.memset(hi[:], 1.0), w=["m_hi"])
                affv = aff[:].rearrange("p t e -> p e t")
                cmpv = cmpt[:].rearrange("p t e -> p e t")

                def count_ge(thr, thrk):
                    P.op("dve", lambda e: e.tensor_tensor(cmpt[:], aff[:], thr[:].unsqueeze(1).to_broadcast([128, NT, 16]), ALU.is_ge),
                         r=["m_aff", thrk], w=["m_cmp"])
                    P.op("dve", lambda e: e.tensor_reduce(cntpf[:], cmpv, AX.X, ALU.add), r=["m_cmp"], w=["m_cntpf"])
                    P.op("dve", lambda e: e.tensor_copy(cntp[:], cntpf[:]), r=["m_cntpf"], w=["m_cntp"])
                for it in range(NIT):
                    P.op("dve", lambda e: e.tensor_tensor(mid[:], lo[:], hi[:], ALU.add), r=["m_lo", "m_hi"], w=["m_mid"])
                    P.op("dve", lambda e: e.tensor_scalar(mid[:], mid[:], 0.5, None, ALU.mult), r=["m_mid"], w=["m_mid"])
                    count_ge(mid, "m_mid")
                    P.op("pe", lambda e: e.matmul(pst[:], self.ones_b[:], cntp[:], start=True, stop=True), r=["ones_b", "m_cntp"], w=["m_pst"])
                    P.op("dve", lambda e: e.tensor_scalar(gei[:], pst[:], float(C), None, ALU.is_ge), r=["m_pst"], w=["m_gei"])
                    P.op("dve", lambda e: e.tensor_scalar(lti[:], pst[:], float(C), None, ALU.is_lt), r=["m_pst"], w=["m_lti"])
                    P.op("dve", lambda e: e.copy_predicated(lo[:], gei[:], mid[:]), r=["m_gei", "m_mid", "m_lo"], w=["m_lo"])
                    P.op("dve", lambda e: e.copy_predicated(hi[:], lti[:], mid[:]), r=["m_lti", "m_mid", "m_hi"], w=["m_hi"])
                count_ge(lo, "m_lo")
                P.op("pe", lambda e: e.matmul(pst[:], ltri[:], cntp[:], start=True, stop=True), r=["m_ltri", "m_cntp"], w=["m_pst"])
                offs = sb("m_offs", [128, 16], F32)
                P.op("act", lambda e: e.copy(offs[:], pst[:]), r=["m_pst"], w=["m_offs"])
                cum = sb("m_cum", [128, NT, 16], F32)
                cumv = cum[:].rearrange("p t e -> p e t")
                onesr = sb("m_onesr", [128, NT], F32)
                P.op("dve", lambda e: e.memset(onesr[:], 1.0), w=["m_onesr"])
                for ex_ in range(16):
                    P.op("dve", lambda e, ex_=ex_: e.tensor_tensor_scan(cumv[:, ex_, :], onesr[:], cmpv[:, ex_, :], 0.0, ALU.mult, ALU.add),
                         r=["m_cmp", "m_onesr"], w=["m_cum"])
                P.op("dve", lambda e: e.tensor_tensor(cum[:], cum[:], cmpt[:], ALU.subtract), r=["m_cum", "m_cmp"], w=["m_cum"])
                P.op("dve", lambda e: e.tensor_tensor(cum[:], cum[:], offs[:].unsqueeze(1).to_broadcast([128, NT, 16]), ALU.add), r=["m_cum", "m_offs"], w=["m_cum"])
                P.op("dve", lambda e: e.tensor_scalar(cum[:], cum[:], -BIG, None, ALU.add), r=["m_cum"], w=["m_cum"])
                P.op("dve", lambda e: e.tensor_tensor(cum[:], cum[:], cmpt[:], ALU.mult), r=["m_cum", "m_cmp"], w=["m_cum"])
                P.op("dve", lambda e: e.tensor_scalar(cum[:], cum[:], BIG, None, ALU.add), r=["m_cum"], w=["m_cum"])
                P.op("dve", lambda e: e.tensor_copy(posi[:], cum[:].rearrange("p t e -> p (t e)")), r=["m_cum"], w=["m_posi"])
                if self.dbg:
                    P.dma("sp", self.dbg_posi, posi[:], r=["m_posi"], w=["dbg_posi"])
                    P.dma("sp", self.dbg_aff, aff[:].rearrange("p t e -> p (t e)"), r=["m_aff"], w=["dbg_aff"])
                P.barrier()
            with contextlib.ExitStack() as st:
                xr = self.p_xr
                for tt in range(NT):
                    b = tt % 2
                    P.dma("sp", xr[b][:], self.xrows[tt * 128:(tt + 1) * 128, :], r=["xrows"], w=["m_dr%d" % b])
                    for ex_ in range(16):
                        col = tt * 16 + ex_
                        P.dma_fn("pool", lambda e, b=b, ex_=ex_, col=col: e.indirect_dma_start(
                            out=self.xg[ex_][:, :], out_offset=bass.IndirectOffsetOnAxis(ap=posi[:, col:col + 1], axis=0),
                            in_=xr[b][:], in_offset=None, bounds_check=self.reg(e, C - 1), oob_is_err=False),
                            r=["m_dr%d" % b, "m_posi"], w=[("xg", ex_)])
                P.barrier()
        with contextlib.ExitStack() as st:
            sb = lambda n, shp, dt: self.sb(st, n, shp, dt)
            xgT = sb("e_xgT", [128, 16, C], BF16)
            hidT = sb("e_hidT", [128, 8, C], BF16)
            Wd = sb("e_Wd", [128, 8, D], BF16)
            wgb = [sb("e_wg%d" % i, [128, 16, 128], BF16) for i in range(2)]
            wub = [sb("e_wu%d" % i, [128, 16, 128], BF16) for i in range(2)]
            xrw = [sb("e_xr%d" % i, [128, RW], BF16) for i in range(2)]
            gates = sb("e_gates", [128, NCT], F32)
            tid = self.p_tid
            sg = sb("e_sg", [128, 512], F32)
            yrow = self.p_yrow
            pT = [self.ps(st, "e_pT%d" % i, [128, 4, 128], BF16) for i in range(2)]
            psG = self.ps(st, "e_psG", [128, 512], F32)
            psU = self.ps(st, "e_psU", [128, 512], F32)
            psY = [self.ps(st, "e_psY%d" % i, [128, 512], F32) for i in range(2)]
            nsubc = max(1, C // 512)
            subw = min(C, 512)
            tcnt = 0
            ycnt = 0
            for ex_ in range(16):
                for c in range(8):
                    P.dma("pool", Wd[:, :, c * 256:(c + 1) * 256], self.w_exp_down[l, ex_, :, c * 256:(c + 1) * 256].rearrange("(kc p) n -> p kc n", p=128), w=[("e_Wd", c)])
                for ct in range(NCT):
                    b = ct % 2
                    P.dma("sp", xrw[b][:], self.xg[ex_][ct * 128:(ct + 1) * 128, :], r=[("xg", ex_)], w=["e_xr%d" % b])
                    self.transpose_tile(xrw[b][:, 0:D], "e_xr%d" % b, pT, "e_pT%d", (lambda g4, ct=ct: xgT[:, g4 * 4:(g4 + 1) * 4, ct * 128:(ct + 1) * 128]), ("e_xgT", ct), cnt0=tcnt)
                    tcnt += 4
                    P.op("dve", lambda e, b=b, ct=ct, ex_=ex_: e.tensor_copy(gates[:, ct:ct + 1], xrw[b][:, D + 2 * ex_:D + 2 * ex_ + 2].bitcast(F32)), r=["e_xr%d" % b], w=["e_gates"])
                    P.op("dve", lambda e, b=b, ct=ct: e.tensor_copy(tid[:, ct:ct + 1], xrw[b][:, D + 32:D + 34].bitcast(I32)), r=["e_xr%d" % b], w=["e_tid"])
                xkeys = [("e_xgT", ct) for ct in range(NCT)]
                for fb in range(8):
                    wb_ = fb % 2
                    P.dma("pool", wgb[wb_][:], self.w_exp_gate[l, ex_, :, fb * 128:(fb + 1) * 128].rearrange("(kc p) n -> p kc n", p=128), w=["e_wg%d" % wb_])
                    P.dma("pool", wub[wb_][:], self.w_exp_up[l, ex_, :, fb * 128:(fb + 1) * 128].rearrange("(kc p) n -> p kc n", p=128), w=["e_wu%d" % wb_])
                    for sub in range(nsubc):
                        tsl = slice(sub * subw, (sub + 1) * subw)
                        for kc in range(16):
                            P.op("pe", lambda e, wb_=wb_, kc=kc, tsl=tsl: e.matmul(psG[:, 0:subw], wgb[wb_][:, kc, :], xgT[:, kc, tsl], start=(kc == 0), stop=(kc == 15)),
                                 r=["e_wg%d" % wb_] + xkeys, w=["e_psG"])
                        for kc in range(16):
                            P.op("pe", lambda e, wb_=wb_, kc=kc, tsl=tsl: e.matmul(psU[:, 0:subw], wub[wb_][:, kc, :], xgT[:, kc, tsl], start=(kc == 0), stop=(kc == 15)),
                                 r=["e_wu%d" % wb_] + xkeys, w=["e_psU"])
                        P.op("act", lambda e: e.activation(out=sg[:, 0:subw], in_=psG[:, 0:subw], func=AF.Silu), r=["e_psG"], w=["e_sg"])
                        P.op("dve", lambda e, fb=fb, tsl=tsl: e.tensor_tensor(hidT[:, fb, tsl], sg[:, 0:subw], psU[:, 0:subw], ALU.mult), r=["e_sg", "e_psU"], w=[("e_hidT", fb)])
                hkeys = [("e_hidT", fb) for fb in range(8)]
                for ct in range(NCT):
                    yb = ycnt % 2
                    ycnt += 1
                    for c4 in range(4):
                        pb = c4 % 2
                        for fc in range(8):
                            P.op("pe", lambda e, pb=pb, fc=fc, ct=ct, c4=c4: e.matmul(psY[pb][:], hidT[:, fc, ct * 128:(ct + 1) * 128], Wd[:, fc, c4 * 512:(c4 + 1) * 512], start=(fc == 0), stop=(fc == 7)),
                                 r=hkeys + [("e_Wd", 2 * c4), ("e_Wd", 2 * c4 + 1)], w=["e_psY%d" % pb])
                        P.op("dve" if c4 % 2 == 0 else "act",
                             (lambda e, pb=pb, yb=yb, c4=c4, ct=ct: e.tensor_scalar(yrow[yb][:, c4 * 512:(c4 + 1) * 512], psY[pb][:], gates[:, ct:ct + 1], None, ALU.mult)) if c4 % 2 == 0 else
                             (lambda e, pb=pb, yb=yb, c4=c4, ct=ct: e.activation(out=yrow[yb][:, c4 * 512:(c4 + 1) * 512], in_=psY[pb][:], func=AF.Copy, scale=gates[:, ct:ct + 1])),
                             r=["e_psY%d" % pb, "e_gates"], w=["e_yrow%d" % yb])
                    P.dma_fn("pool", lambda e, yb=yb, ct=ct: e.indirect_dma_start(
                        out=self.h[:, :], out_offset=bass.IndirectOffsetOnAxis(ap=tid[:, ct:ct + 1], axis=0),
                        in_=yrow[yb][:], in_offset=None, bounds_check=self.reg(e, S - 1), oob_is_err=True, compute_op=ALU.add),
                        r=["e_yrow%d" % yb, "e_tid"], w=["h"])
            P.barrier()

    def stage_ple(self, l):
        P, S = self.P, self.S
        with contextlib.ExitStack() as st:
            sb = lambda n, shp, dt: self.sb(st, n, shp, dt)
            Wg = sb("l_Wg", [128, 16, D], BF16)
            for c in range(8):
                P.dma("pool", Wg[:, :, c * 256:(c + 1) * 256], self.w_ple_gate[l, :, c * 256:(c + 1) * 256].rearrange("(kc p) n -> p kc n", p=128), w=[("l_Wg", c)])
            Wp = sb("l_Wp", [128, 2, D], BF16)
            for c in range(2):
                P.dma("pool", Wp[:, :, c * 1024:(c + 1) * 1024], self.w_ple_proj[l, :, c * 1024:(c + 1) * 1024].rearrange("(kc p) n -> p kc n", p=128), w=[("l_Wp", c)])
            gb = sb("l_gb", [128, D], F32)
            P.dma("sp", gb[:], self.norm_ple[l:l + 1, :].partition_broadcast(128), w=["l_gb"])
            hb = [sb("l_hb%d" % i, [128, D], F32) for i in range(2)]
            junk = sb("l_junk", [128, D], BF16)
            ss = [sb("l_ss%d" % i, [128, 1], F32) for i in range(2)]
            rs = [sb("l_rs%d" % i, [128, 1], F32) for i in range(2)]
            xn = [sb("l_xn%d" % i, [128, D], BF16) for i in range(2)]
            hT = [sb("l_hT%d" % i, [128, 16, 128], BF16) for i in range(2)]
            pTt = [sb("l_pTt%d" % i, [128, 2, 128], BF16) for i in range(2)]
            sg = sb("l_sg", [128, 512], F32)
            pT = [self.ps(st, "l_pT%d" % i, [128, 4, 128], BF16) for i in range(2)]
            psG = [self.ps(st, "l_psG%d" % i, [128, 512], F32) for i in range(2)]
            psP = [self.ps(st, "l_psP%d" % i, [128, 512], F32) for i in range(2)]
            for tt in range(S // 128):
                b = tt % 2
                t0 = tt * 128
                k = lambda n, b=b: "l_%s%d" % (n, b)
                P.dma("sp", hb[b][:], self.h[t0:t0 + 128, :], r=["h"], w=[k("hb")])
                P.dma("pool", pTt[b][:], self.pT[l, :, t0:t0 + 128].rearrange("(kc p) t -> p kc t", p=128), w=[k("pTt")])
                self.norm_tile(hb[b], k("hb"), gb, "l_gb", xn[b][:], k("xn"), ss[b], k("ss"), rs[b], k("rs"), junk, "l_junk")
                self.transpose_tile(xn[b][:], k("xn"), pT, "l_pT%d", (lambda g4, b=b: hT[b][:, g4 * 4:(g4 + 1) * 4, :]), k("hT"), cnt0=tt * 4)
                for c4 in range(4):
                    pb = c4 % 2
                    cs_ = slice(c4 * 512, (c4 + 1) * 512)
                    for kc in range(16):
                        P.op("pe", lambda e, b=b, pb=pb, kc=kc, cs_=cs_: e.matmul(psG[pb][:], hT[b][:, kc, :], Wg[:, kc, cs_], start=(kc == 0), stop=(kc == 15)),
                             r=[k("hT"), ("l_Wg", 2 * c4), ("l_Wg", 2 * c4 + 1)], w=["l_psG%d" % pb])
                    for kc in range(2):
                        P.op("pe", lambda e, b=b, pb=pb, kc=kc, cs_=cs_: e.matmul(psP[pb][:], pTt[b][:, kc, :], Wp[:, kc, cs_], start=(kc == 0), stop=(kc == 1)),
                             r=[k("pTt"), ("l_Wp", c4 // 2)], w=["l_psP%d" % pb])
                    P.op("act", lambda e, pb=pb: e.activation(out=sg[:], in_=psG[pb][:], func=AF.Sigmoid), r=["l_psG%d" % pb], w=["l_sg"])
                    P.op("dve", lambda e, pb=pb: e.tensor_tensor(sg[:], sg[:], psP[pb][:], ALU.mult), r=["l_sg", "l_psP%d" % pb], w=["l_sg"])
                    P.op("dve", lambda e, b=b, cs_=cs_: e.tensor_tensor(hb[b][:, cs_], hb[b][:, cs_], sg[:], ALU.add), r=["l_sg", k("hb")], w=[k("hb")])
                P.dma("sp", self.h[t0:t0 + 128, :], hb[b][:], r=[k("hb")], w=["h"])
            P.barrier()

    def build(self):
        self.declare()
        P = self.P
        self.load_consts()
        self.pib = self.sb(self.stack, "pib", [128, 1], F32)
        P.op("pool", lambda e: e.memset(self.pib[:], math.pi), w=["pib"])
        self.p_posi = self.sb(self.stack, "m_posi", [128, (self.S // 128) * 16], I32)
        self.p_xr = [self.sb(self.stack, "m_dr%d" % i, [128, 2112], BF16) for i in range(2)]
        self.p_tid = self.sb(self.stack, "e_tid", [128, max(1, self.S // 1024)], I32)
        self.p_yrow = [self.sb(self.stack, "e_yrow%d" % i, [128, D], F32) for i in range(2)]
        self.epsb = self.sb(self.stack, "epsb", [128, 1], F32)
        P.op("pool", lambda e: e.memset(self.epsb[:], EPS), w=["epsb"])
        P.dma("sp", self.h[:, :], self.x[:, :], w=["h"])
        if self.stop != "init":
            self.rope_tables()
        for l in range(self.L):
            if self.stop in ("init", "rope"):
                break
            self.stage_a(l)
            if self.stop in ("a", "a1"):
                break
            self.stage_attn(l)
            if self.stop == "attn":
                break
            self.stage_s5(l)
            if self.stop in ("s5", "s5raw"):
                break
            self.stage_c1(l)
            self.stage_c2(l)
            if self.stop == "c":
                break
            self.stage_moe(l)
            if self.stop == "moe":
                break
            self.stage_ple(l)
        P.finish()
        P.emit()
        self.stack.close()
        return self.nc


SEQ = 8192
DEPTH = 4
N_CORES = 8


def kernel(**inputs):
    inp = {k: np.asarray(v) for k, v in inputs.items()}
    B = inp["x"].shape[0]
    m = MK(SEQ, DEPTH, dbg=False)
    nc = m.build()
    consts = make_consts()
    s5 = prep_s5(inp)
    shared = {}
    for name in ["norm_mix", "w_in", "q_norm", "k_norm", "w_attn_br", "w_ssm_br", "w_out", "norm_ffn", "w_router",
                 "w_exp_gate", "w_exp_up", "w_exp_down", "norm_ple", "w_ple_gate", "w_ple_proj"]:
        shared[name] = np.ascontiguousarray(inp[name], dtype=np.float32)
    shared.update(s5)
    for k, v in consts.items():
        shared["c_" + k] = v
    per_b = []
    for b in range(B):
        per_b.append({
            "x": np.ascontiguousarray(inp["x"][b], dtype=np.float32),
            "pT": np.ascontiguousarray(inp["p"][:, b].transpose(0, 2, 1), dtype=np.float32),
            "positions": np.ascontiguousarray(inp["positions"][b:b + 1], dtype=np.int32),
        })
    in_maps = []
    for c in range(N_CORES):
        b = (c * B) // N_CORES
        d = dict(shared)
        d.update(per_b[b])
        in_maps.append({k: v for k, v in d.items() if k in m.din})
    res = run_bass_kernel_spmd(nc, in_maps, core_ids=list(range(N_CORES)))
    outs = []
    for b in range(B):
        c = (b * N_CORES) // B
        outs.append(np.asarray(res.results[c]["h"], dtype=np.float32))
    return np.stack(outs, axis=0)
```

```python
import math
import contextlib
import numpy as np
import concourse.bass as bass
import concourse.mybir as mybir
from concourse.bass_utils import run_bass_kernel_spmd


F32 = mybir.dt.float32
BF16 = mybir.dt.bfloat16
I32 = mybir.dt.int32
U32 = mybir.dt.uint32
AF = mybir.ActivationFunctionType
ALU = mybir.AluOpType
AX = mybir.AxisListType


class Prog:
    ENGS = ["pe", "dve", "act", "pool", "sp"]

    def __init__(self, nc, stack, ndma=16):
        self.nc = nc
        self.ins = {e: [] for e in self.ENGS}
        self.sems = {e: stack.enter_context(nc.semaphore("cs_" + e)) for e in self.ENGS}
        self.qslots = {"sp": list(range(0, 10)), "pool": list(range(10, 18)), "act": list(range(18, 20))}
        self.ndma = ndma = 20
        self.dsems = [stack.enter_context(nc.semaphore("ds%d" % i)) for i in range(ndma)]
        self.dcount = [0] * ndma
        self.qnext = {q: 0 for q in self.qslots}
        self.last_w = {}
        self.readers = {}
        self.psum_keys = set()

    def _deps(self, reads, writes):
        deps = []
        for b in reads:
            ev = self.last_w.get(b)
            if ev is not None:
                deps.append(ev)
            if b in self.psum_keys:
                deps.extend(self.readers.get(b, ()))
        for b in writes:
            ev = self.last_w.get(b)
            if ev is not None:
                deps.append(ev)
            deps.extend(self.readers.get(b, ()))
        return deps

    def _update(self, ev, reads, writes):
        for b in reads:
            lst = self.readers.setdefault(b, [])
            if ev[0] == "c":
                lst[:] = [x for x in lst if not (x[0] == "c" and x[1] == ev[1])]
            lst.append(ev)
        for b in writes:
            self.last_w[b] = ev
            self.readers[b] = []

    def op(self, eng, fn, r=(), w=()):
        deps = self._deps(r, w)
        idx = len(self.ins[eng])
        self.ins[eng].append(dict(fn=fn, deps=deps, dma=None, sig=False))
        self._update(("c", eng, idx), r, w)

    def dma(self, q, out, in_, r=(), w=(), **kw):
        fn = lambda e: e.dma_start(out=out, in_=in_, **kw)
        self.dma_fn(q, fn, r, w)

    def dma_fn(self, q, fn, r=(), w=()):
        deps = self._deps(r, w)
        sl = self.qslots[q]
        s = sl[self.qnext[q] % len(sl)]
        self.qnext[q] += 1
        if self.dcount[s] > 0:
            deps.append(("d", s, 16 * self.dcount[s]))
        self.dcount[s] += 1
        tgt = 16 * self.dcount[s]
        self.ins[q].append(dict(fn=fn, deps=deps, dma=(s, tgt), sig=False))
        self._update(("d", s, tgt), r, w)

    def barrier(self):
        evs = []
        for e in self.ENGS:
            n = len(self.ins[e])
            for i in range(n - 1, -1, -1):
                if self.ins[e][i]["dma"] is None and self.ins[e][i]["fn"] is not None:
                    evs.append(("c", e, i))
                    break
        for s in range(self.ndma):
            if self.dcount[s] > 0:
                evs.append(("d", s, 16 * self.dcount[s]))
        for e in self.ENGS:
            self.ins[e].append(dict(fn=None, deps=list(evs), dma=None, sig=False))
        self.last_w = {}
        self.readers = {}

    def emit(self):
        nc = self.nc
        plans = {}
        for e in self.ENGS:
            wc = {x: -1 for x in self.ENGS}
            wd = [0] * self.ndma
            plan = []
            for idx, it in enumerate(self.ins[e]):
                waits = []
                for ev in it["deps"]:
                    if ev[0] == "c":
                        _, e2, i2 = ev
                        if e2 == e and e == "pe":
                            continue
                        if e2 == e and i2 >= idx:
                            continue
                        if wc[e2] >= i2:
                            continue
                        wc[e2] = i2
                        self.ins[e2][i2]["sig"] = True
                        waits.append(("c", e2, i2))
                    else:
                        _, s, tgt = ev
                        if wd[s] >= tgt:
                            continue
                        wd[s] = tgt
                        waits.append(ev)
                plan.append(waits)
            plans[e] = plan
        sigval = {}
        for e in self.ENGS:
            c = 0
            for idx, it in enumerate(self.ins[e]):
                if it["sig"]:
                    c += 1
                    sigval[(e, idx)] = c
        self.sig_totals = {e: sum(1 for it in self.ins[e] if it["sig"]) for e in self.ENGS}

        def run(e, eng):
            for idx, it in enumerate(self.ins[e]):
                for wv in plans[e][idx]:
                    if wv[0] == "c":
                        eng.wait_ge(self.sems[wv[1]], sigval[(wv[1], wv[2])])
                    else:
                        eng.wait_ge(self.dsems[wv[1]], wv[2])
                if it["fn"] is None:
                    continue
                ins = it["fn"](eng)
                if it["dma"] is not None:
                    ins.then_inc(self.dsems[it["dma"][0]], 16)
                elif it["sig"]:
                    ins.then_inc(self.sems[e], 1)

        with nc.Block() as block:
            @block.tensor
            def _(eng):
                run("pe", eng)

            @block.vector
            def _(eng):
                run("dve", eng)

            @block.scalar
            def _(eng):
                run("act", eng)

            @block.gpsimd
            def _(eng):
                run("pool", eng)

            @block.sync
            def _(eng):
                run("sp", eng)

    def finish(self):
        evs = [("d", s, 16 * self.dcount[s]) for s in range(self.ndma) if self.dcount[s] > 0]
        self.ins["sp"].append(dict(fn=None, deps=evs, dma=None, sig=False))


D = 2048
NIN = 9728
HD = 128
NH = 12
EPS = 1e-6
TWO_PI = 2.0 * math.pi


def make_consts():
    c = {}
    c["ident"] = np.eye(128, dtype=np.float32)
    c["ones"] = np.ones((128, 128), dtype=np.float32)
    rm = np.zeros((128, 128), dtype=np.float32)
    for i in range(16):
        rm[i + 16, i] = -1.0
        rm[i, i + 16] = 1.0
    c["rmat"] = rm
    invf = np.zeros((128, 1), dtype=np.float32)
    fr = np.power(np.float32(500000.0), -np.arange(16, dtype=np.float32) * np.float32(2.0) / np.float32(32.0)).astype(np.float32)
    invf[0:16, 0] = fr
    invf[16:32, 0] = fr
    c["invf"] = invf
    kk = np.arange(128)[:, None]
    qq = np.arange(128)[None, :]
    m = np.zeros((128, 3, 128), dtype=np.float32)
    m[:, 0, :] = (kk - qq >= 64)
    m[:, 1, :] = (np.abs(kk - qq) <= 64)
    m[:, 2, :] = (kk - qq <= -64)
    c["amask"] = m.reshape(128, 384)
    c["ltri"] = (np.arange(128)[:, None] < np.arange(128)[None, :]).astype(np.float32)
    return c


def prep_s5(inp):
    L = inp["ssm_a_re"].shape[0]
    o = {}
    def st(a):
        return np.ascontiguousarray(a.reshape(L, 2, 32, 128).transpose(0, 1, 3, 2))
    o["s5_are"] = st(inp["ssm_a_re"])
    o["s5_aim"] = st(inp["ssm_a_im"])
    ldt = np.repeat(inp["ssm_log_dt"][:, :, :, None], 64, axis=3)
    o["s5_ldt"] = st(ldt)
    for nm, src in (("s5_bre", "ssm_b_re"), ("s5_bim", "ssm_b_im")):
        b = inp[src].reshape(L, 2, 32, 2, 64, 16)
        out = np.zeros((L, 2, 32, 32, 128), np.float32)
        for gi in range(2):
            out[:, :, :, gi * 16:(gi + 1) * 16, gi * 64:(gi + 1) * 64] = b[:, :, :, gi].transpose(0, 1, 2, 4, 3)
        o[nm] = out
    for nm, src in (("s5_cre", "ssm_c_re"), ("s5_cim", "ssm_c_im")):
        c = inp[src].reshape(L, 2, 32, 2, 16, 64)
        out = np.zeros((L, 2, 32, 128, 32), np.float32)
        for gi in range(2):
            out[:, :, :, gi * 64:(gi + 1) * 64, gi * 16:(gi + 1) * 16] = c[:, :, :, gi].transpose(0, 1, 2, 4, 3)
        o[nm] = out
    o["s5_d"] = np.ascontiguousarray(inp["ssm_d"].reshape(L, 32, 32, 1))
    return o


class MK:
    def __init__(self, S, L, dbg=False, stop=None):
        self.S, self.L, self.dbg, self.stop = S, L, dbg, stop
        self.TS = min(S, 2048)
        self.nc = nc = bass.Bass("TRN2", target_bir_lowering=False)
        self.stack = contextlib.ExitStack()
        self.P = Prog(nc, self.stack)
        self.din = {}
        self.dscr = {}

    def reg(self, eng, val):
        if not hasattr(self, "_regs"):
            self._regs = {}
        if val not in self._regs:
            self._regs[val] = eng.to_reg(val)
        return self._regs[val]

    def inp(self, name, shape, dt=F32):
        t = self.nc.dram_tensor(name, list(shape), dt, kind="ExternalInput").ap()
        self.din[name] = t
        return t

    def scr(self, name, shape, dt, out=False):
        kind = "ExternalOutput" if (out or self.dbg) else "Internal"
        t = self.nc.dram_tensor(name, list(shape), dt, kind=kind).ap()
        self.dscr[name] = t
        return t

    def sb(self, st, name, shape, dt):
        self._uid = getattr(self, "_uid", 0) + 1
        return st.enter_context(self.nc.sbuf_tensor("%s__%d" % (name, self._uid), list(shape), dt))

    def ps(self, st, name, shape, dt=F32):
        self.P.psum_keys.add(name)
        self._uid = getattr(self, "_uid", 0) + 1
        return st.enter_context(self.nc.psum_tensor("%s__%d" % (name, self._uid), list(shape), dt))

    def declare(self):
        S, L = self.S, self.L
        i = self.inp
        self.x = i("x", [S, D])
        self.pT = i("pT", [L, 256, S])
        self.pos = i("positions", [1, S], I32)
        self.norm_mix = i("norm_mix", [L, D])
        self.w_in = i("w_in", [L, D, NIN])
        self.q_norm = i("q_norm", [L, 128])
        self.k_norm = i("k_norm", [L, 128])
        self.w_attn_br = i("w_attn_br", [L, 512, D])
        self.w_ssm_br = i("w_ssm_br", [L, 1024, 2 * D])
        self.w_out = i("w_out", [L, D, D])
        self.norm_ffn = i("norm_ffn", [L, D])
        self.norm_ple = i("norm_ple", [L, D])
        self.w_ple_gate = i("w_ple_gate", [L, D, D])
        self.w_ple_proj = i("w_ple_proj", [L, 256, D])
        self.w_router = i("w_router", [L, D, 16])
        self.w_exp_gate = i("w_exp_gate", [L, 16, D, 1024])
        self.w_exp_up = i("w_exp_up", [L, 16, D, 1024])
        self.w_exp_down = i("w_exp_down", [L, 16, 1024, D])
        self.c_ltri = i("c_ltri", [128, 128])
        self.s5_are = i("s5_are", [L, 2, 128, 32])
        self.s5_aim = i("s5_aim", [L, 2, 128, 32])
        self.s5_ldt = i("s5_ldt", [L, 2, 128, 32])
        self.s5_bre = i("s5_bre", [L, 2, 32, 32, 128])
        self.s5_bim = i("s5_bim", [L, 2, 32, 32, 128])
        self.s5_cre = i("s5_cre", [L, 2, 32, 128, 32])
        self.s5_cim = i("s5_cim", [L, 2, 32, 128, 32])
        self.s5_d = i("s5_d", [L, 32, 32, 1])
        for n in ["ident", "ones", "rmat"]:
            setattr(self, "c_" + n, i("c_" + n, [128, 128]))
        self.c_invf = i("c_invf", [128, 1])
        self.c_amask = i("c_amask", [128, 384])
        s = self.scr
        self.h = s("h", [S, D], F32, out=True)
        self.cosT = s("cosT", [128, S], F32)
        self.sinT = s("sinT", [128, S], F32)
        self.qkT = s("qkT", [24, 128, S], BF16)
        self.v = s("v", [S, 1536], BF16)
        self.uT = s("uT", [1024, S], BF16)
        self.gT = s("gT", [4096, S], BF16)
        self.attT = s("attT", [512, S], BF16)
        self.ygT = s("ygT", [1024, S], BF16)
        self.mergedT = s("mergedT", [2048, S], BF16)
        self.xrows = s("xrows", [S, 2112], BF16)
        self.xg = [s("xg%d" % e_, [S // 8, 2112], BF16) for e_ in range(16)]
        if self.dbg:
            self.dbg_posi = s("dbg_posi", [128, (S // 128) * 16], I32)
            self.dbg_aff = s("dbg_aff", [128, (S // 128) * 16], F32)
        if self.stop == "s5raw":
            self.dbg_y = s("dbg_y", [1024, S], F32)

    def load_consts(self):
        P, st = self.P, self.stack
        self.ident_b = self.sb(st, "ident_b", [128, 128], BF16)
        self.ones_b = self.sb(st, "ones_b", [128, 128], BF16)
        self.rmat_b = self.sb(st, "rmat_b", [128, 128], BF16)
        self.ident_f = self.sb(st, "ident_f", [128, 128], F32)
        self.ones_f = self.sb(st, "ones_f", [128, 128], F32)
        self.invf = self.sb(st, "invf", [128, 1], F32)
        P.dma("pool", self.ident_b[:], self.c_ident, w=["ident_b"])
        P.dma("pool", self.ones_b[:], self.c_ones, w=["ones_b"])
        P.dma("pool", self.rmat_b[:], self.c_rmat, w=["rmat_b"])
        P.dma("sp", self.ident_f[:], self.c_ident, w=["ident_f"])
        P.dma("sp", self.ones_f[:], self.c_ones, w=["ones_f"])
        P.dma("sp", self.invf[:], self.c_invf, w=["invf"])

    def sin_reduced(self, x, xk, out, ok, ti, tik, tf, tfk, shift):
        P = self.P
        P.op("dve", lambda e: e.tensor_scalar(tf[:], x[:], shift, 1.0 / TWO_PI, ALU.add, ALU.mult), r=[xk], w=[tfk])
        P.op("dve", lambda e: e.tensor_copy(ti[:], tf[:]), r=[tfk], w=[tik])
        P.op("dve", lambda e: e.tensor_copy(tf[:], ti[:]), r=[tik], w=[tfk])
        P.op("dve", lambda e: e.scalar_tensor_tensor(tf[:], tf[:], -TWO_PI, x[:], ALU.mult, ALU.add), r=[tfk, xk], w=[tfk])
        P.op("dve", lambda e: e.tensor_scalar(tf[:], tf[:], shift - math.pi, -2.0 * math.pi + 2 * math.pi, ALU.max, ALU.add) if False else
             e.tensor_scalar(tf[:], tf[:], shift, -math.pi, ALU.add, ALU.max), r=[tfk], w=[tfk])
        P.op("dve", lambda e: e.tensor_scalar(tf[:], tf[:], math.pi, None, ALU.min), r=[tfk], w=[tfk])
        P.op("act", lambda e: e.activation(out=out[:], in_=tf[:], func=AF.Sin), r=[tfk], w=[ok])

    def rope_tables(self):
        P, S = self.P, self.S
        CH = min(S, 2048)
        with contextlib.ExitStack() as st:
            pi_ = self.sb(st, "rp_i", [128, CH], I32)
            pf = self.sb(st, "rp_f", [128, CH], F32)
            m1 = self.sb(st, "rp_m1", [128, CH], F32)
            m2 = self.sb(st, "rp_m2", [128, CH], F32)
            sn = self.sb(st, "rp_sn", [128, CH], F32)
            cs = self.sb(st, "rp_cs", [128, CH], F32)
            for c in range(S // CH):
                sl = slice(c * CH, (c + 1) * CH)
                P.dma("sp", pi_[:], self.pos[0:1, sl].partition_broadcast(128), w=["rp_i"])
                P.op("dve", lambda e: e.tensor_copy(pf[:], pi_[:]), r=["rp_i"], w=["rp_f"])
                P.op("dve", lambda e: e.tensor_scalar(m1[:], pf[:], self.invf[:, 0:1], None, ALU.mult), r=["rp_f", "invf"], w=["rp_m1"])
                self.sin_reduced(m1, "rp_m1", sn, "rp_sn", pi_, "rp_i", m2, "rp_m2", 0.0)
                self.sin_reduced(m1, "rp_m1", cs, "rp_cs", pi_, "rp_i", m2, "rp_m2", math.pi / 2)
                P.dma("sp", self.sinT[:, sl], sn[:], r=["rp_sn"], w=["sinT"])
                P.dma("sp", self.cosT[:, sl], cs[:], r=["rp_cs"], w=["cosT"])
            P.barrier()

    def norm_transpose(self, st, src, T0, ntok, gain_row, hnT, key, pfx):
        P = self.P
        gb = self.sb(st, pfx + "gb", [128, D], F32)
        P.dma("sp", gb[:], gain_row.partition_broadcast(128), w=[pfx + "gb"])
        hb = [self.sb(st, pfx + "hb%d" % i, [128, D], F32) for i in range(2)]
        junk = self.sb(st, pfx + "junk", [128, D], BF16)
        ss = [self.sb(st, pfx + "ss%d" % i, [128, 1], F32) for i in range(2)]
        rs = [self.sb(st, pfx + "rs%d" % i, [128, 1], F32) for i in range(2)]
        xn = [self.sb(st, pfx + "xn%d" % i, [128, D], BF16) for i in range(2)]
        pT = [self.ps(st, pfx + "pT%d" % i, [128, 4, 128], BF16) for i in range(2)]
        for tt in range(ntok // 128):
            b = tt % 2
            hbk, ssk, rsk, xnk = pfx + "hb%d" % b, pfx + "ss%d" % b, pfx + "rs%d" % b, pfx + "xn%d" % b
            t0 = T0 + tt * 128
            P.dma("sp", hb[b][:], src[t0:t0 + 128, :], w=[hbk])
            P.op("act", lambda e, b=b: e.activation(out=junk[:], in_=hb[b][:], func=AF.Square, accum_out=ss[b][:, 0:1]),
                 r=[hbk], w=[pfx + "junk", ssk])
            P.op("act", lambda e, b=b: e.activation(out=rs[b][:], in_=ss[b][:], func=AF.Sqrt, scale=1.0 / D, bias=self.epsb[:, 0:1]), r=[ssk, "epsb"], w=[rsk])
            P.op("dve", lambda e, b=b: e.reciprocal(rs[b][:], rs[b][:]), r=[rsk], w=[rsk])
            P.op("dve", lambda e, b=b: e.scalar_tensor_tensor(xn[b][:], hb[b][:], rs[b][:, 0:1], gb[:], ALU.mult, ALU.mult),
                 r=[hbk, rsk, pfx + "gb"], w=[xnk])
            for g4 in range(4):
                pb = (tt * 4 + g4) % 2
                pk = pfx + "pT%d" % pb
                for j in range(4):
                    kc = g4 * 4 + j
                    P.op("pe", lambda e, b=b, pb=pb, j=j, kc=kc: e.transpose(pT[pb][:, j, :], xn[b][:, kc * 128:(kc + 1) * 128], self.ident_b[:]),
                         r=[xnk, "ident_b"], w=[pk])
                eng = "act" if g4 % 2 == 0 else "dve"
                if eng == "act":
                    P.op("act", lambda e, pb=pb, g4=g4, tt=tt: e.copy(hnT[:, g4 * 4:(g4 + 1) * 4, tt * 128:(tt + 1) * 128], pT[pb][:]),
                         r=[pk], w=[(key, tt)])
                else:
                    P.op("dve", lambda e, pb=pb, g4=g4, tt=tt: e.tensor_copy(hnT[:, g4 * 4:(g4 + 1) * 4, tt * 128:(tt + 1) * 128], pT[pb][:]),
                         r=[pk], w=[(key, tt)])

    def stage_a(self, l):
        P, S, TS = self.P, self.S, self.TS
        nsub = TS // 512
        ntt = TS // 128
        for sup in range(S // TS):
            T0 = sup * TS
            with contextlib.ExitStack() as st:
                hnT = self.sb(st, "a_hnT", [128, 16, TS], BF16)
                with contextlib.ExitStack() as st2:
                    self.norm_transpose(st2, self.h, T0, TS, self.norm_mix[l:l + 1, :], hnT, "a_hnT", "an_")
                    P.barrier()
                if self.stop == "norm":
                    self.dbg_hnT = self.scr("dbg_hnT", [128, 16, TS], BF16)
                    P.dma("sp", self.dbg_hnT, hnT[:], r=[("a_hnT", tt) for tt in range(ntt)], w=["dbg_hnT"])
                    P.barrier()
                    return
                hkeys = [("a_hnT", tt) for tt in range(ntt)]
                cosb = self.sb(st, "a_cos", [128, TS], F32)
                sinb = self.sb(st, "a_sin", [128, TS], F32)
                P.dma("sp", cosb[:], self.cosT[:, T0:T0 + TS], r=["cosT"], w=["a_cos"])
                P.dma("sp", sinb[:], self.sinT[:, T0:T0 + TS], r=["sinT"], w=["a_sin"])
                gq = self.sb(st, "a_gq", [128, 2], F32)
                import os
                DBG = os.environ.get("DBG", "")
                if "nogq" in DBG:
                    P.op("dve", lambda e: e.memset(gq[:], 1.0), w=["a_gq"])
                else:
                    P.dma("sp", gq[:, 0:1], self.q_norm[l:l + 1, :].rearrange("o p -> p o"), w=["a_gq"])
                    P.dma("sp", gq[:, 1:2], self.k_norm[l:l + 1, :].rearrange("o p -> p o"), w=["a_gq"])
                wb = [self.sb(st, "a_wb%d" % i, [128, 16, 256], BF16) for i in range(3)]
                psm = [self.ps(st, "a_ps%d" % i, [128, 512], F32) for i in range(3)]
                ps_ss = self.ps(st, "a_psss", [128, 512], F32)
                ps_rot = self.ps(st, "a_psrot", [128, 512], F32)
                sq = self.sb(st, "a_sq", [128, 512], BF16)
                qg = self.sb(st, "a_qg", [128, 512], BF16)
                rstd = self.sb(st, "a_rstd", [128, 512], F32)
                t1 = self.sb(st, "a_t1", [128, 512], F32)
                t2 = self.sb(st, "a_t2", [128, 512], F32)
                ob = [self.sb(st, "a_ob%d" % i, [128, TS], BF16) for i in range(2)]
                wcount = [0]
                mmcount = [0]
                ocount = [0]

                def load_w(col0, ncols):
                    i = wcount[0] % 3
                    wcount[0] += 1
                    src = self.w_in[l, :, col0:col0 + ncols].rearrange("(kc p) n -> p kc n", p=128)
                    P.dma("pool", wb[i][:, :, 0:ncols], src, w=["a_wb%d" % i])
                    return i

                fm_cols = [(j * 128, "qk", j) for j in range(24)] + \
                          [(4608 + j * 128, "u", j) for j in range(8)] + \
                          [(5632 + j * 128, "g", j) for j in range(32)]
                for pair in range(len(fm_cols) // 2):
                    if self.stop == 'a1' and pair not in (0, 12, 16):
                        continue
                    col0 = fm_cols[2 * pair][0]
                    wi = load_w(col0, 256)
                    for half in range(2):
                        _, kind, j = fm_cols[2 * pair + half]
                        oi = ocount[0] % 2
                        ocount[0] += 1
                        okey = "a_ob%d" % oi
                        for sub in range(nsub):
                            pi = mmcount[0] % 3
                            mmcount[0] += 1
                            pk = "a_ps%d" % pi
                            tsl = slice(sub * 512, (sub + 1) * 512)
                            for kc in range(16):
                                P.op("pe", lambda e, pi=pi, wi=wi, half=half, kc=kc, tsl=tsl: e.matmul(
                                    psm[pi][:], wb[wi][:, kc, half * 128:(half + 1) * 128], hnT[:, kc, tsl],
                                    start=(kc == 0), stop=(kc == 15)),
                                    r=["a_wb%d" % wi] + hkeys[sub * 4:(sub + 1) * 4], w=[pk])
                            if kind == "qk" and "noepi" in DBG:
                                P.op("act", lambda e, pi=pi, oi=oi, tsl=tsl: e.copy(ob[oi][:, tsl], psm[pi][:]), r=[pk], w=[okey])
                            elif kind == "qk":
                                gi = 0 if j < 12 else 1
                                P.op("act", lambda e, pi=pi: e.activation(out=sq[:], in_=psm[pi][:], func=AF.Square), r=[pk], w=["a_sq"])
                                P.op("dve", lambda e, pi=pi, gi=gi: e.tensor_scalar(qg[:], psm[pi][:], gq[:, gi:gi + 1], None, ALU.mult),
                                     r=[pk, "a_gq"], w=["a_qg"])
                                P.op("pe", lambda e: e.matmul(ps_ss[:], self.ones_b[:], sq[:], start=True, stop=True),
                                     r=["ones_b", "a_sq"], w=["a_psss"])
                                P.op("pe", lambda e: e.matmul(ps_rot[:], self.rmat_b[:], qg[:], start=True, stop=True),
                                     r=["rmat_b", "a_qg"], w=["a_psrot"])
                                if "e1" in DBG:
                                    P.op("act", lambda e, oi=oi, tsl=tsl: e.copy(ob[oi][:, tsl], ps_rot[:]), r=["a_psrot"], w=[okey])
                                    P.op("act", lambda e, oi=oi, tsl=tsl: e.copy(t1[:], ps_ss[:]), r=["a_psss"], w=["a_t1"])
                                    continue
                                P.op("act", lambda e: e.activation(out=rstd[:], in_=ps_ss[:], func=AF.Sqrt, scale=1.0 / HD, bias=self.epsb[:, 0:1]),
                                     r=["a_psss", "epsb"], w=["a_rstd"])
                                if "e2" in DBG:
                                    P.op("dve", lambda e: e.reciprocal(t1[:], rstd[:]), r=["a_rstd"], w=["a_t1"])
                                    P.op("act", lambda e, oi=oi, tsl=tsl: e.copy(ob[oi][:, tsl], ps_rot[:]), r=["a_psrot"], w=[okey])
                                    continue
                                P.op("dve", lambda e: e.reciprocal(rstd[:], rstd[:]), r=["a_rstd"], w=["a_rstd"])
                                P.op("dve", lambda e, tsl=tsl: e.tensor_tensor(t1[:], qg[:], cosb[:, tsl], ALU.mult), r=["a_qg", "a_cos"], w=["a_t1"])
                                P.op("dve", lambda e, tsl=tsl: e.tensor_tensor(t2[:], ps_rot[:], sinb[:, tsl], ALU.mult), r=["a_psrot", "a_sin"], w=["a_t2"])
                                P.op("dve", lambda e: e.tensor_tensor(t1[:], t1[:], t2[:], ALU.add), r=["a_t1", "a_t2"], w=["a_t1"])
                                P.op("dve", lambda e, oi=oi, tsl=tsl: e.tensor_tensor(ob[oi][:, tsl], t1[:], rstd[:], ALU.mult),
                                     r=["a_t1", "a_rstd"], w=[okey])
                            elif kind == "u":
                                P.op("act", lambda e, pi=pi, oi=oi, tsl=tsl: e.copy(ob[oi][:, tsl], psm[pi][:]), r=[pk], w=[okey])
                            else:
                                P.op("act", lambda e, pi=pi, oi=oi, tsl=tsl: e.activation(out=ob[oi][:, tsl], in_=psm[pi][:], func=AF.Sigmoid),
                                     r=[pk], w=[okey])
                        if kind == "qk":
                            P.dma("sp", self.qkT[j, :, T0:T0 + TS], ob[oi][:], r=[okey], w=["qkT"])
                        elif kind == "u":
                            P.dma("sp", self.uT[j * 128:(j + 1) * 128, T0:T0 + TS], ob[oi][:], r=[okey], w=["uT"])
                        else:
                            P.dma("sp", self.gT[j * 128:(j + 1) * 128, T0:T0 + TS], ob[oi][:], r=[okey], w=["gT"])
                vo = [self.sb(st, "a_vo%d" % i, [128, 256], BF16) for i in range(2)]
                vcount = 0
                for c in range(6):
                    if self.stop == 'a1':
                        continue
                    wi = load_w(3072 + c * 256, 256)
                    for tt in range(ntt):
                        pi = mmcount[0] % 3
                        mmcount[0] += 1
                        pk = "a_ps%d" % pi
                        for kc in range(16):
                            P.op("pe", lambda e, pi=pi, wi=wi, kc=kc, tt=tt: e.matmul(
                                psm[pi][:, 0:256], hnT[:, kc, tt * 128:(tt + 1) * 128], wb[wi][:, kc, :],
                                start=(kc == 0), stop=(kc == 15)),
                                r=["a_wb%d" % wi, ("a_hnT", tt)], w=[pk])
                        vi = vcount % 2
                        vcount += 1
                        P.op("act", lambda e, pi=pi, vi=vi: e.copy(vo[vi][:], psm[pi][:, 0:256]), r=[pk], w=["a_vo%d" % vi])
                        P.dma("sp", self.v[T0 + tt * 128:T0 + (tt + 1) * 128, c * 256:(c + 1) * 256], vo[vi][:],
                              r=["a_vo%d" % vi], w=["v"])
                P.barrier()
        return

    def stage_attn(self, l):
        P, S = self.P, self.S
        scale = float(HD) ** -0.5
        with contextlib.ExitStack() as st:
            amask = self.sb(st, "t_amask", [128, 3, 128], BF16)
            P.dma("pool", amask[:], self.c_amask.rearrange("p (j q) -> p j q", j=3), w=["t_amask"])
            acc_n = self.sb(st, "t_accn", [128, S], F32)
            acc_d = self.sb(st, "t_accd", [128, S], F32)
            qT = self.sb(st, "t_qT", [128, S], BF16)
            kT = self.sb(st, "t_kT", [128, S], BF16)
            vt = self.sb(st, "t_vt", [128, S // 128, 128], BF16)
            es = [self.sb(st, "t_es%d" % i, [128, 3, 128], BF16) for i in range(2)]
            em = [self.sb(st, "t_em%d" % i, [128, 3, 128], BF16) for i in range(2)]
            ps_s = [self.ps(st, "t_pss%d" % i, [128, 3, 128], F32) for i in range(2)]
            ps_o = [self.ps(st, "t_pso%d" % i, [128, 128], F32) for i in range(2)]
            ps_d = [self.ps(st, "t_psd%d" % i, [128, 128], F32) for i in range(2)]
            ob = self.sb(st, "t_ob", [128, S], BF16)
            cnt = 0
            for slot in range(4):
                for g, d in enumerate((1, 4, 16)):
                    head = 4 * g + slot
                    n = S // d // 128
                    P.dma("sp", qT[:], self.qkT[head, :, :], r=["qkT"], w=["t_qT"])
                    P.dma("sp", kT[:], self.qkT[12 + head, :, :], r=["qkT"], w=["t_kT"])
                    vh = self.v[:, head * 128:(head + 1) * 128].rearrange("(jt jp d) c -> d jp jt c", d=d, jp=128)
                    for r in range(d):
                        P.dma("sp", vt[:, r * n:(r + 1) * n, :], vh[r], r=["v"], w=["t_vt"])
                    qv = qT[:].rearrange("p (j d) -> p d j", d=d)
                    kv = kT[:].rearrange("p (j d) -> p d j", d=d)
                    anv = acc_n[:].rearrange("p (j d) -> p d j", d=d)
                    adv = acc_d[:].rearrange("p (j d) -> p d j", d=d)
                    for r in range(d):
                        for i in range(n):
                            b = cnt % 2
                            cnt += 1
                            kts = [kt for kt in (i - 1, i, i + 1) if 0 <= kt < n]
                            jlo, jhi = kts[0] - i + 1, kts[-1] - i + 2
                            qs = slice(i * 128, (i + 1) * 128)
                            for kt in kts:
                                jj = kt - i + 1
                                P.op("pe", lambda e, b=b, jj=jj, r=r, kt=kt, qs=qs, kv=kv, qv=qv: e.matmul(
                                    ps_s[b][:, jj, :], kv[:, r, kt * 128:(kt + 1) * 128], qv[:, r, qs], start=True, stop=True),
                                    r=["t_kT", "t_qT"], w=["t_pss%d" % b])
                            P.op("act", lambda e, b=b, jlo=jlo, jhi=jhi: e.activation(out=es[b][:, jlo:jhi, :], in_=ps_s[b][:, jlo:jhi, :], func=AF.Exp, scale=scale),
                                 r=["t_pss%d" % b], w=["t_es%d" % b])
                            P.op("dve", lambda e, b=b, jlo=jlo, jhi=jhi: e.tensor_tensor(em[b][:, jlo:jhi, :], es[b][:, jlo:jhi, :], amask[:, jlo:jhi, :], ALU.mult),
                                 r=["t_es%d" % b, "t_amask"], w=["t_em%d" % b])
                            for kt in kts:
                                jj = kt - i + 1
                                P.op("pe", lambda e, b=b, jj=jj, r=r, kt=kt, n=n, kts=kts: e.matmul(
                                    ps_o[b][:], vt[:, r * n + kt, :], em[b][:, jj, :], start=(kt == kts[0]), stop=(kt == kts[-1])),
                                    r=["t_vt", "t_em%d" % b], w=["t_pso%d" % b])
                            for kt in kts:
                                jj = kt - i + 1
                                P.op("pe", lambda e, b=b, jj=jj, kt=kt, kts=kts: e.matmul(
                                    ps_d[b][:], self.ones_b[:], em[b][:, jj, :], start=(kt == kts[0]), stop=(kt == kts[-1])),
                                    r=["ones_b", "t_em%d" % b], w=["t_psd%d" % b])
                            if g == 0:
                                P.op("act", lambda e, b=b, r=r, qs=qs, anv=anv: e.copy(anv[:, r, qs], ps_o[b][:]), r=["t_pso%d" % b], w=["t_accn"])
                                P.op("dve", lambda e, b=b, r=r, qs=qs, adv=adv: e.tensor_copy(adv[:, r, qs], ps_d[b][:]), r=["t_psd%d" % b], w=["t_accd"])
                            else:
                                P.op("dve", lambda e, b=b, r=r, qs=qs, anv=anv: e.tensor_tensor(anv[:, r, qs], anv[:, r, qs], ps_o[b][:], ALU.add),
                                     r=["t_pso%d" % b, "t_accn"], w=["t_accn"])
                                P.op("dve", lambda e, b=b, r=r, qs=qs, adv=adv: e.tensor_tensor(adv[:, r, qs], adv[:, r, qs], ps_d[b][:], ALU.add),
                                     r=["t_psd%d" % b, "t_accd"], w=["t_accd"])
                P.op("dve", lambda e: e.reciprocal(acc_d[:], acc_d[:]), r=["t_accd"], w=["t_accd"])
                P.op("dve", lambda e: e.tensor_tensor(ob[:], acc_n[:], acc_d[:], ALU.mult), r=["t_accn", "t_accd"], w=["t_ob"])
                P.dma("sp", self.attT[slot * 128:(slot + 1) * 128, :], ob[:], r=["t_ob"], w=["attT"])
            P.barrier()

    def stage_s5(self, l):
        P, S = self.P, self.S
        Lc = 512
        nch = S // Lc
        GC = 1.5957691216057308
        with contextlib.ExitStack() as st:
            sb = lambda n, shp, dt: self.sb(st, n, shp, dt)
            ioti = sb("s_ioti", [128, Lc + 1], I32)
            iot = sb("s_iot", [128, Lc + 1], F32)
            P.op("pool", lambda e: e.iota(ioti[:], pattern=[[1, Lc + 1]], base=0, channel_multiplier=0), w=["s_ioti"])
            P.op("dve", lambda e: e.tensor_copy(iot[:], ioti[:]), r=["s_ioti"], w=["s_iot"])
            prm = {}
            pti = sb("s_pti", [128, 32], I32)
            ptf = sb("s_ptf", [128, 32], F32)
            for dr in range(2):
                names = ["are", "aim", "ldt", "dt", "ar", "th", "rho", "sn", "cs", "lbr", "lbi", "den", "nr", "cr", "ci", "nci", "t"]
                T = {n: sb("s_%s%d" % (n, dr), [128, 32], F32) for n in names}
                K = {n: "s_%s%d" % (n, dr) for n in names}
                P.dma("sp", T["are"][:], self.s5_are[l, dr], w=[K["are"]])
                P.dma("sp", T["aim"][:], self.s5_aim[l, dr], w=[K["aim"]])
                P.dma("sp", T["ldt"][:], self.s5_ldt[l, dr], w=[K["ldt"]])
                P.op("act", lambda e, T=T: e.activation(out=T["dt"][:], in_=T["ldt"][:], func=AF.Exp), r=[K["ldt"]], w=[K["dt"]])
                tt = lambda o, a, b, op, T=T, K=K: P.op("dve", lambda e: e.tensor_tensor(T[o][:], T[a][:], T[b][:], op), r=[K[a], K[b]], w=[K[o]])
                tt("ar", "are", "dt", ALU.mult)
                tt("th", "aim", "dt", ALU.mult)
                P.op("act", lambda e, T=T: e.activation(out=T["rho"][:], in_=T["ar"][:], func=AF.Exp), r=[K["ar"]], w=[K["rho"]])
                self.sin_reduced(T["th"], K["th"], T["sn"], K["sn"], pti, "s_pti", ptf, "s_ptf", 0.0)
                self.sin_reduced(T["th"], K["th"], T["cs"], K["cs"], pti, "s_pti", ptf, "s_ptf", math.pi / 2)
                tt("lbr", "rho", "cs", ALU.mult)
                tt("lbi", "rho", "sn", ALU.mult)
                tt("t", "are", "are", ALU.mult)
                tt("den", "aim", "aim", ALU.mult)
                tt("den", "den", "t", ALU.add)
                P.op("dve", lambda e, T=T: e.reciprocal(T["den"][:], T["den"][:]), r=[K["den"]], w=[K["den"]])
                P.op("dve", lambda e, T=T: e.tensor_scalar(T["nr"][:], T["lbr"][:], -1.0, None, ALU.add), r=[K["lbr"]], w=[K["nr"]])
                tt("cr", "nr", "are", ALU.mult)
                tt("t", "lbi", "aim", ALU.mult)
                tt("cr", "cr", "t", ALU.add)
                tt("cr", "cr", "den", ALU.mult)
                tt("ci", "lbi", "are", ALU.mult)
                tt("t", "nr", "aim", ALU.mult)
                tt("ci", "ci", "t", ALU.subtract)
                tt("ci", "ci", "den", ALU.mult)
                P.op("dve", lambda e, T=T: e.tensor_scalar(T["nci"][:], T["ci"][:], -1.0, None, ALU.mult), r=[K["ci"]], w=[K["nci"]])
                prm[dr] = (T, K)
            uP = sb("s_uP", [32, S], BF16)
            yacc = sb("s_yacc", [32, S], F32)
            dcol = sb("s_dcol", [32, 1], F32)
            Bre = sb("s_Bre", [32, 128], BF16)
            Bim = sb("s_Bim", [32, 128], BF16)
            Cre = sb("s_Cre", [128, 32], F32)
            Cim = sb("s_Cim", [128, 32], F32)
            Ct = sb("s_Ct", [128, 32], F32)
            Cpr = sb("s_Cpr", [128, 32], BF16)
            Cni = sb("s_Cni", [128, 32], BF16)
            ang = sb("s_ang", [128, Lc + 1], F32)
            tsn = sb("s_tsn", [128, Lc + 1], F32)
            tcs = sb("s_tcs", [128, Lc + 1], F32)
            tti = sb("s_tti", [128, Lc + 1], I32)
            ttf = sb("s_ttf", [128, Lc + 1], F32)
            rhob = sb("s_rhob", [128, Lc], F32)
            nsnt = sb("s_nsn", [128, Lc], F32)
            q1 = [sb("s_q1%d" % i, [128, Lc], F32) for i in range(2)]
            q2 = [sb("s_q2%d" % i, [128, Lc], F32) for i in range(2)]
            q3 = [sb("s_q3%d" % i, [128, Lc], F32) for i in range(2)]
            q4 = [sb("s_q4%d" % i, [128, Lc], F32) for i in range(2)]
            ytmp = sb("s_ytmp", [32, Lc], F32)
            wr = [sb("s_wr%d" % i, [128, Lc], F32) for i in range(2)]
            wi_ = [sb("s_wi%d" % i, [128, Lc], F32) for i in range(2)]
            m1 = [sb("s_m1%d" % i, [128, Lc], BF16) for i in range(2)]
            m2 = [sb("s_m2%d" % i, [128, Lc], BF16) for i in range(2)]
            m3 = [sb("s_m3%d" % i, [128, Lc], BF16) for i in range(2)]
            m4 = [sb("s_m4%d" % i, [128, Lc], BF16) for i in range(2)]
            ini = [sb("s_ini%d" % i, [128, 2], F32) for i in range(2)]
            it_ = sb("s_it", [128, 1], F32)
            GW = min(S, 2048)
            g1 = sb("s_g1", [32, GW], F32)
            g2 = sb("s_g2", [32, GW], F32)
            yo = sb("s_yo", [32, GW], BF16)
            ps_br = [self.ps(st, "s_psbr%d" % i, [128, Lc], F32) for i in range(2)]
            ps_bi = [self.ps(st, "s_psbi%d" % i, [128, Lc], F32) for i in range(2)]
            ps_btr = self.ps(st, "s_psbtr", [128, Lc], F32)
            ps_bti = self.ps(st, "s_psbti", [128, Lc], F32)
            ps_y = self.ps(st, "s_psy", [32, Lc], F32)
            cnt = 0
            import os
            for gp in range(1 if 's5one' in os.environ.get('DBG', '') else 32):
                P.dma("sp", uP[:], self.uT[gp * 32:(gp + 1) * 32, :], r=["uT"], w=["s_uP"])
                P.dma("sp", dcol[:], self.s5_d[l, gp], w=["s_dcol"])
                for dr in range(2):
                    T, K = prm[dr]
                    col = lambda n, T=T, gp=gp: T[n][:, gp:gp + 1]
                    P.dma("pool", Bre[:], self.s5_bre[l, dr, gp], w=["s_Bre"])
                    P.dma("pool", Bim[:], self.s5_bim[l, dr, gp], w=["s_Bim"])
                    P.dma("sp", Cre[:], self.s5_cre[l, dr, gp], w=["s_Cre"])
                    P.dma("sp", Cim[:], self.s5_cim[l, dr, gp], w=["s_Cim"])
                    P.op("dve", lambda e, col=col: e.tensor_scalar(Ct[:], Cim[:], col("ci"), None, ALU.mult), r=["s_Cim", K["ci"]], w=["s_Ct"])
                    P.op("dve", lambda e, col=col: e.scalar_tensor_tensor(Cpr[:], Cre[:], col("cr"), Ct[:], ALU.mult, ALU.subtract),
                         r=["s_Cre", "s_Ct", K["cr"]], w=["s_Cpr"])
                    P.op("dve", lambda e, col=col: e.tensor_scalar(Ct[:], Cim[:], col("cr"), None, ALU.mult), r=["s_Cim", K["cr"]], w=["s_Ct"])
                    P.op("dve", lambda e, col=col: e.scalar_tensor_tensor(Cni[:], Cre[:], col("nci"), Ct[:], ALU.mult, ALU.subtract),
                         r=["s_Cre", "s_Ct", K["nci"]], w=["s_Cni"])
                    P.op("dve", lambda e, col=col: e.tensor_scalar(ang[:], iot[:], col("th"), None, ALU.mult), r=["s_iot", K["th"]], w=["s_ang"])
                    self.sin_reduced(ang, "s_ang", tsn, "s_tsn", tti, "s_tti", ttf, "s_ttf", 0.0)
                    self.sin_reduced(ang, "s_ang", tcs, "s_tcs", tti, "s_tti", ttf, "s_ttf", math.pi / 2)
                    P.op("dve", lambda e, col=col: e.tensor_scalar(rhob[:], iot[:, 0:Lc], 0.0, col("rho"), ALU.mult, ALU.add),
                         r=["s_iot", K["rho"]], w=["s_rhob"])
                    P.op("dve", lambda e: e.tensor_scalar(nsnt[:], tsn[:, 0:Lc], -1.0, None, ALU.mult), r=["s_tsn"], w=["s_nsn"])
                    sn, cs = tsn[:, 0:Lc], tcs[:, 0:Lc]
                    snL, csL = tsn[:, Lc:Lc + 1], tcs[:, Lc:Lc + 1]
                    prev = None
                    rv = (lambda ap: ap) if dr == 0 else (lambda ap: ap[:, ::-1])
                    bufs = []
                    for ci_ in range(nch):
                        bufs.append(cnt % 2)
                        cnt += 1

                    nsn = nsnt[:, 0:Lc]

                    def front(ci_, dr=dr, rv=rv, cs=cs, sn=sn, nsn=nsn, bufs=bufs):
                        c = ci_ if dr == 0 else nch - 1 - ci_
                        b = bufs[ci_]
                        csl = slice(c * Lc, (c + 1) * Lc)
                        kb = lambda n, b=b: "s_%s%d" % (n, b)
                        P.op("pe", lambda e, b=b, csl=csl: e.matmul(ps_br[b][:], Bre[:], uP[:, csl], start=True, stop=True), r=["s_Bre", "s_uP"], w=[kb("psbr")])
                        P.op("pe", lambda e, b=b, csl=csl: e.matmul(ps_bi[b][:], Bim[:], uP[:, csl], start=True, stop=True), r=["s_Bim", "s_uP"], w=[kb("psbi")])
                        P.op("dve", lambda e, b=b, rv=rv, cs=cs: e.tensor_tensor(q1[b][:], rv(ps_br[b][:]), cs, ALU.mult), r=[kb("psbr"), "s_tcs"], w=[kb("q1")])
                        P.op("dve", lambda e, b=b, rv=rv, sn=sn: e.tensor_tensor(q2[b][:], rv(ps_bi[b][:]), sn, ALU.mult), r=[kb("psbi"), "s_tsn"], w=[kb("q2")])
                        P.op("dve", lambda e, b=b, rv=rv, cs=cs: e.tensor_tensor(q3[b][:], rv(ps_bi[b][:]), cs, ALU.mult), r=[kb("psbi"), "s_tcs"], w=[kb("q3")])
                        P.op("dve", lambda e, b=b, rv=rv, nsn=nsn: e.tensor_tensor(q4[b][:], rv(ps_br[b][:]), nsn, ALU.mult), r=[kb("psbr"), "s_nsn"], w=[kb("q4")])

                    def back(ci_, prev, dr=dr, cs=cs, sn=sn, nsn=nsn, snL=snL, csL=csL, bufs=bufs):
                        c = ci_ if dr == 0 else nch - 1 - ci_
                        b = bufs[ci_]
                        csl = slice(c * Lc, (c + 1) * Lc)
                        kb = lambda n, b=b: "s_%s%d" % (n, b)
                        P.op("pe", lambda e, b=b: e.matmul(ps_btr[:], self.ident_f[:], q1[b][:], start=True, stop=False), r=["ident_f", kb("q1")], w=["s_psbtr"])
                        P.op("pe", lambda e, b=b: e.matmul(ps_btr[:], self.ident_f[:], q2[b][:], start=False, stop=True), r=["ident_f", kb("q2")], w=["s_psbtr"])
                        P.op("pe", lambda e, b=b: e.matmul(ps_bti[:], self.ident_f[:], q3[b][:], start=True, stop=False), r=["ident_f", kb("q3")], w=["s_psbti"])
                        P.op("pe", lambda e, b=b: e.matmul(ps_bti[:], self.ident_f[:], q4[b][:], start=False, stop=True), r=["ident_f", kb("q4")], w=["s_psbti"])
                        if prev is None:
                            P.op("dve", lambda e, b=b: e.memset(ini[b][:], 0.0), w=[kb("ini")])
                        else:
                            pb = prev
                            wre, wie = wr[pb][:, Lc - 1:Lc], wi_[pb][:, Lc - 1:Lc]
                            P.op("dve", lambda e, wie=wie, snL=snL: e.tensor_tensor(it_[:], wie, snL, ALU.mult), r=["s_wi%d" % pb, "s_tsn"], w=["s_it"])
                            P.op("dve", lambda e, b=b, wre=wre, csL=csL: e.tensor_tensor(ini[b][:, 0:1], wre, csL, ALU.mult), r=["s_wr%d" % pb, "s_tcs"], w=[kb("ini")])
                            P.op("dve", lambda e, b=b: e.tensor_tensor(ini[b][:, 0:1], ini[b][:, 0:1], it_[:], ALU.subtract), r=[kb("ini"), "s_it"], w=[kb("ini")])
                            P.op("dve", lambda e, wie=wie, csL=csL: e.tensor_tensor(it_[:], wie, csL, ALU.mult), r=["s_wi%d" % pb, "s_tcs"], w=["s_it"])
                            P.op("dve", lambda e, b=b, wre=wre, snL=snL: e.tensor_tensor(ini[b][:, 1:2], wre, snL, ALU.mult), r=["s_wr%d" % pb, "s_tsn"], w=[kb("ini")])
                            P.op("dve", lambda e, b=b: e.tensor_tensor(ini[b][:, 1:2], ini[b][:, 1:2], it_[:], ALU.add), r=[kb("ini"), "s_it"], w=[kb("ini")])
                        P.op("dve", lambda e, b=b: e.tensor_tensor_scan(wr[b][:], rhob[:], ps_btr[:], ini[b][:, 0:1], ALU.mult, ALU.add),
                             r=["s_rhob", "s_psbtr", kb("ini")], w=[kb("wr")])
                        P.op("dve", lambda e, b=b: e.tensor_tensor_scan(wi_[b][:], rhob[:], ps_bti[:], ini[b][:, 1:2], ALU.mult, ALU.add),
                             r=["s_rhob", "s_psbti", kb("ini")], w=[kb("wi")])
                        P.op("dve", lambda e, b=b, cs=cs: e.tensor_tensor(m1[b][:], wr[b][:], cs, ALU.mult), r=[kb("wr"), "s_tcs"], w=[kb("m1")])
                        P.op("dve", lambda e, b=b, nsn=nsn: e.tensor_tensor(m2[b][:], wi_[b][:], nsn, ALU.mult), r=[kb("wi"), "s_nsn"], w=[kb("m2")])
                        P.op("dve", lambda e, b=b, sn=sn: e.tensor_tensor(m3[b][:], wr[b][:], sn, ALU.mult), r=[kb("wr"), "s_tsn"], w=[kb("m3")])
                        P.op("dve", lambda e, b=b, cs=cs: e.tensor_tensor(m4[b][:], wi_[b][:], cs, ALU.mult), r=[kb("wi"), "s_tcs"], w=[kb("m4")])
                        P.op("pe", lambda e, b=b: e.matmul(ps_y[:], Cpr[:], m1[b][:], start=True, stop=False), r=["s_Cpr", kb("m1")], w=["s_psy"])
                        P.op("pe", lambda e, b=b: e.matmul(ps_y[:], Cpr[:], m2[b][:], start=False, stop=False), r=["s_Cpr", kb("m2")], w=["s_psy"])
                        P.op("pe", lambda e, b=b: e.matmul(ps_y[:], Cni[:], m3[b][:], start=False, stop=False), r=["s_Cni", kb("m3")], w=["s_psy"])
                        P.op("pe", lambda e, b=b: e.matmul(ps_y[:], Cni[:], m4[b][:], start=False, stop=True), r=["s_Cni", kb("m4")], w=["s_psy"])
                        if dr == 0:
                            P.op("act", lambda e, csl=csl: e.copy(yacc[:, csl], ps_y[:]), r=["s_psy"], w=["s_yacc"])
                        else:
                            P.op("act", lambda e: e.copy(ytmp[:], ps_y[:]), r=["s_psy"], w=["s_ytmp"])
                            P.op("dve", lambda e, csl=csl: e.tensor_tensor(yacc[:, csl][:, ::-1], yacc[:, csl][:, ::-1], ytmp[:], ALU.add),
                                 r=["s_ytmp", "s_yacc"], w=["s_yacc"])
                        return b

                    front(0)
                    prev = None
                    for ci_ in range(nch):
                        if ci_ + 1 < nch:
                            front(ci_ + 1)
                        prev = back(ci_, prev)
                for gc in range(S // GW):
                    gsl = slice(gc * GW, (gc + 1) * GW)
                    P.op("dve", lambda e, gsl=gsl: e.scalar_tensor_tensor(g1[:], uP[:, gsl], dcol[:, 0:1], yacc[:, gsl], ALU.mult, ALU.add), r=["s_uP", "s_dcol", "s_yacc"], w=["s_g1"])
                    if self.stop == "s5raw":
                        P.dma("sp", self.dbg_y[gp * 32:(gp + 1) * 32, gsl], g1[:], r=["s_g1"], w=["dbg_y"])
                    P.op("pool", lambda e: e.tensor_tensor(g2[:], g1[:], g1[:], ALU.mult), r=["s_g1"], w=["s_g2"])
                    P.op("pool", lambda e: e.tensor_scalar(g2[:], g2[:], 0.044715, 1.0, ALU.mult, ALU.add), r=["s_g2"], w=["s_g2"])
                    P.op("pool", lambda e: e.tensor_tensor(g2[:], g2[:], g1[:], ALU.mult), r=["s_g2", "s_g1"], w=["s_g2"])
                    P.op("act", lambda e: e.activation(out=g2[:], in_=g2[:], func=AF.Sigmoid, scale=GC), r=["s_g2"], w=["s_g2"])
                    P.op("pool", lambda e: e.tensor_tensor(yo[:], g2[:], g1[:], ALU.mult), r=["s_g2", "s_g1"], w=["s_yo"])
                    P.dma("sp", self.ygT[gp * 32:(gp + 1) * 32, gsl], yo[:], r=["s_yo"], w=["ygT"])
            P.barrier()

    def stage_c1(self, l):
        P, S, TS = self.P, self.S, self.TS
        nsub = TS // 512
        for sup in range(S // TS):
            T0 = sup * TS
            with contextlib.ExitStack() as st:
                sb = lambda n, shp, dt: self.sb(st, n, shp, dt)
                aT = sb("c_aT", [128, 4, TS], BF16)
                yT = sb("c_yT", [128, 8, TS], BF16)
                P.dma("sp", aT[:], self.attT[:, T0:T0 + TS].rearrange("(kc p) t -> p kc t", p=128), r=["attT"], w=["c_aT"])
                P.dma("sp", yT[:], self.ygT[:, T0:T0 + TS].rearrange("(kc p) t -> p kc t", p=128), r=["ygT"], w=["c_yT"])
                wa = [sb("c_wa%d" % i, [128, 4, 128], BF16) for i in range(2)]
                wv = [sb("c_wv%d" % i, [128, 8, 128], BF16) for i in range(2)]
                wg = [sb("c_wg%d" % i, [128, 8, 128], BF16) for i in range(2)]
                ga = [sb("c_ga%d" % i, [128, TS], BF16) for i in range(2)]
                gs = [sb("c_gs%d" % i, [128, TS], BF16) for i in range(2)]
                mo = [sb("c_mo%d" % i, [128, TS], BF16) for i in range(2)]
                sg = sb("c_sg", [128, 512], F32)
                sbr = sb("c_sbr", [128, 512], F32)
                ta = sb("c_ta", [128, 512], F32)
                psA = [self.ps(st, "c_psA%d" % i, [128, 512], F32) for i in range(2)]
                psV = [self.ps(st, "c_psV%d" % i, [128, 512], F32) for i in range(2)]
                psG = [self.ps(st, "c_psG%d" % i, [128, 512], F32) for i in range(2)]
                cnt = 0
                for m in range(16):
                    wb_ = m % 2
                    cs_ = slice(m * 128, (m + 1) * 128)
                    P.dma("pool", wa[wb_][:], self.w_attn_br[l, :, cs_].rearrange("(kc p) n -> p kc n", p=128), w=["c_wa%d" % wb_])
                    P.dma("pool", wv[wb_][:], self.w_ssm_br[l, :, cs_].rearrange("(kc p) n -> p kc n", p=128), w=["c_wv%d" % wb_])
                    P.dma("pool", wg[wb_][:], self.w_ssm_br[l, :, 2048 + m * 128:2048 + (m + 1) * 128].rearrange("(kc p) n -> p kc n", p=128), w=["c_wg%d" % wb_])
                    P.dma("sp", ga[wb_][:], self.gT[m * 128:(m + 1) * 128, T0:T0 + TS], r=["gT"], w=["c_ga%d" % wb_])
                    P.dma("sp", gs[wb_][:], self.gT[2048 + m * 128:2048 + (m + 1) * 128, T0:T0 + TS], r=["gT"], w=["c_gs%d" % wb_])
                    for sub in range(nsub):
                        b = cnt % 2
                        cnt += 1
                        tsl = slice(sub * 512, (sub + 1) * 512)
                        for kc in range(4):
                            P.op("pe", lambda e, b=b, wb_=wb_, kc=kc, tsl=tsl: e.matmul(psA[b][:], wa[wb_][:, kc, :], aT[:, kc, tsl], start=(kc == 0), stop=(kc == 3)),
                                 r=["c_wa%d" % wb_, "c_aT"], w=["c_psA%d" % b])
                        for kc in range(8):
                            P.op("pe", lambda e, b=b, wb_=wb_, kc=kc, tsl=tsl: e.matmul(psV[b][:], wv[wb_][:, kc, :], yT[:, kc, tsl], start=(kc == 0), stop=(kc == 7)),
                                 r=["c_wv%d" % wb_, "c_yT"], w=["c_psV%d" % b])
                        for kc in range(8):
                            P.op("pe", lambda e, b=b, wb_=wb_, kc=kc, tsl=tsl: e.matmul(psG[b][:], wg[wb_][:, kc, :], yT[:, kc, tsl], start=(kc == 0), stop=(kc == 7)),
                                 r=["c_wg%d" % wb_, "c_yT"], w=["c_psG%d" % b])
                        P.op("act", lambda e, b=b: e.activation(out=sg[:], in_=psG[b][:], func=AF.Sigmoid), r=["c_psG%d" % b], w=["c_sg"])
                        P.op("dve", lambda e, b=b: e.tensor_tensor(sbr[:], psV[b][:], sg[:], ALU.mult), r=["c_psV%d" % b, "c_sg"], w=["c_sbr"])
                        P.op("dve", lambda e, wb_=wb_, tsl=tsl: e.tensor_tensor(sbr[:], sbr[:], gs[wb_][:, tsl], ALU.mult), r=["c_sbr", "c_gs%d" % wb_], w=["c_sbr"])
                        P.op("dve", lambda e, b=b, wb_=wb_, tsl=tsl: e.tensor_tensor(ta[:], psA[b][:], ga[wb_][:, tsl], ALU.mult), r=["c_psA%d" % b, "c_ga%d" % wb_], w=["c_ta"])
                        P.op("dve", lambda e, wb_=wb_, tsl=tsl: e.tensor_tensor(mo[wb_][:, tsl], ta[:], sbr[:], ALU.add), r=["c_ta", "c_sbr"], w=["c_mo%d" % wb_])
                    P.dma("sp", self.mergedT[m * 128:(m + 1) * 128, T0:T0 + TS], mo[wb_][:], r=["c_mo%d" % wb_], w=["mergedT"])
                P.barrier()

    def stage_c2(self, l):
        P, S = self.P, self.S
        with contextlib.ExitStack() as st:
            sb = lambda n, shp, dt: self.sb(st, n, shp, dt)
            W = sb("o_W", [128, 16, 2048], BF16)
            for c in range(8):
                P.dma("pool", W[:, :, c * 256:(c + 1) * 256], self.w_out[l, :, c * 256:(c + 1) * 256].rearrange("(kc p) n -> p kc n", p=128), w=[("o_W", c)])
            wkeys = [("o_W", c) for c in range(8)]
            mT = [sb("o_mT%d" % i, [128, 16, 128], BF16) for i in range(2)]
            hb = [sb("o_hb%d" % i, [128, 2048], F32) for i in range(2)]
            psm = [self.ps(st, "o_ps%d" % i, [128, 512], F32) for i in range(4)]
            for tt in range(S // 128):
                b = tt % 2
                t0 = tt * 128
                P.dma("sp", mT[b][:], self.mergedT[:, t0:t0 + 128].rearrange("(kc p) t -> p kc t", p=128), r=["mergedT"], w=["o_mT%d" % b])
                P.dma("sp", hb[b][:], self.h[t0:t0 + 128, :], r=["h"], w=["o_hb%d" % b])
                for c4 in range(4):
                    for kc in range(16):
                        P.op("pe", lambda e, b=b, c4=c4, kc=kc: e.matmul(psm[c4][:], mT[b][:, kc, :], W[:, kc, c4 * 512:(c4 + 1) * 512], start=(kc == 0), stop=(kc == 15)),
                             r=["o_mT%d" % b] + wkeys[c4 * 2:c4 * 2 + 2], w=["o_ps%d" % c4])
                    P.op("dve", lambda e, b=b, c4=c4: e.tensor_tensor(hb[b][:, c4 * 512:(c4 + 1) * 512], hb[b][:, c4 * 512:(c4 + 1) * 512], psm[c4][:], ALU.add),
                         r=["o_ps%d" % c4, "o_hb%d" % b], w=["o_hb%d" % b])
                P.dma("sp", self.h[t0:t0 + 128, :], hb[b][:], r=["o_hb%d" % b], w=["h"])
            P.barrier()

    def norm_tile(self, hb, hbk, gb, gbk, xn, xnk, ss, ssk, rs, rsk, junk, junkk):
        P = self.P
        P.op("act", lambda e: e.activation(out=junk[:], in_=hb[:], func=AF.Square, accum_out=ss[:, 0:1]), r=[hbk], w=[junkk, ssk])
        P.op("act", lambda e: e.activation(out=rs[:], in_=ss[:], func=AF.Sqrt, scale=1.0 / D, bias=self.epsb[:, 0:1]), r=[ssk, "epsb"], w=[rsk])
        P.op("dve", lambda e: e.reciprocal(rs[:], rs[:]), r=[rsk], w=[rsk])
        P.op("dve", lambda e: e.scalar_tensor_tensor(xn, hb[:], rs[:, 0:1], gb[:], ALU.mult, ALU.mult), r=[hbk, rsk, gbk], w=[xnk])

    def transpose_tile(self, xn, xnk, pT, pTk, dst_fn, dkey, cnt0=0):
        P = self.P
        for g4 in range(4):
            pb = (cnt0 + g4) % 2
            for j in range(4):
                kc = g4 * 4 + j
                P.op("pe", lambda e, pb=pb, j=j, kc=kc: e.transpose(pT[pb][:, j, :], xn[:, kc * 128:(kc + 1) * 128], self.ident_b[:]),
                     r=[xnk, "ident_b"], w=[pTk % pb])
            if g4 % 2 == 0:
                P.op("act", lambda e, pb=pb, g4=g4: e.copy(dst_fn(g4), pT[pb][:]), r=[pTk % pb], w=[dkey])
            else:
                P.op("dve", lambda e, pb=pb, g4=g4: e.tensor_copy(dst_fn(g4), pT[pb][:]), r=[pTk % pb], w=[dkey])

    def stage_moe(self, l):
        P, S = self.P, self.S
        NT = S // 128
        C = S // 8
        NCT = C // 128
        RW = 2112
        BIG = float(1 << 20)
        NIT = 34
        with contextlib.ExitStack() as st0:
            aff = self.sb(st0, "m_aff", [128, NT, 16], F32)
            posi = self.p_posi
            with contextlib.ExitStack() as st:
                sb = lambda n, shp, dt: self.sb(st, n, shp, dt)
                gb = sb("m_gb", [128, D], F32)
                P.dma("sp", gb[:], self.norm_ffn[l:l + 1, :].partition_broadcast(128), w=["m_gb"])
                wr = sb("m_wr", [128, 16, 16], BF16)
                P.dma("pool", wr[:], self.w_router[l].rearrange("(kc p) n -> p kc n", p=128), w=["m_wr"])
                hb = [sb("m_hb%d" % i, [128, D], F32) for i in range(2)]
                junk = sb("m_junk", [128, D], BF16)
                ss = [sb("m_ss%d" % i, [128, 1], F32) for i in range(2)]
                rs = [sb("m_rs%d" % i, [128, 1], F32) for i in range(2)]
                xrow = [sb("m_xrow%d" % i, [128, RW], BF16) for i in range(2)]
                xT = [sb("m_xT%d" % i, [128, 16, 128], BF16) for i in range(2)]
                ex = sb("m_ex", [128, 16], F32)
                sm = sb("m_sm", [128, 1], F32)
                pT = [self.ps(st, "m_pT%d" % i, [128, 4, 128], BF16) for i in range(2)]
                psl = [self.ps(st, "m_psl%d" % i, [128, 16], F32) for i in range(2)]
                for tt in range(NT):
                    b = tt % 2
                    t0 = tt * 128
                    k = lambda n, b=b: "m_%s%d" % (n, b)
                    P.dma("sp", hb[b][:], self.h[t0:t0 + 128, :], r=["h"], w=[k("hb")])
                    self.norm_tile(hb[b], k("hb"), gb, "m_gb", xrow[b][:, 0:D], k("xrow"), ss[b], k("ss"), rs[b], k("rs"), junk, "m_junk")
                    self.transpose_tile(xrow[b][:, 0:D], k("xrow"), pT, "m_pT%d", (lambda g4, b=b: xT[b][:, g4 * 4:(g4 + 1) * 4, :]), k("xT"), cnt0=tt * 4)
                    for kc in range(16):
                        P.op("pe", lambda e, b=b, kc=kc: e.matmul(psl[b][:], xT[b][:, kc, :], wr[:, kc, :], start=(kc == 0), stop=(kc == 15)),
                             r=[k("xT"), "m_wr"], w=[k("psl")])
                    P.op("act", lambda e, b=b: e.activation(out=ex[:], in_=psl[b][:], func=AF.Exp, accum_out=sm[:, 0:1]), r=[k("psl")], w=["m_ex", "m_sm"])
                    P.op("dve", lambda e: e.reciprocal(sm[:], sm[:]), r=["m_sm"], w=["m_sm"])
                    P.op("dve", lambda e, tt=tt: e.tensor_scalar(aff[:, tt, :], ex[:], sm[:, 0:1], None, ALU.mult), r=["m_ex", "m_sm"], w=["m_aff"])
                    P.op("dve", lambda e, b=b, tt=tt: e.tensor_copy(xrow[b][:, D:D + 32].bitcast(F32), aff[:, tt, :]), r=["m_aff"], w=[k("xrow")])
                    P.op("pool", lambda e, b=b, t0=t0: e.iota(xrow[b][:, D + 32:D + 34].bitcast(I32), pattern=[[0, 1]], base=t0, channel_multiplier=1), w=[k("xrow")])
                    P.dma("sp", self.xrows[t0:t0 + 128, :], xrow[b][:], r=[k("xrow")], w=["xrows"])
                P.barrier()
            with contextlib.ExitStack() as st:
                sb = lambda n, shp, dt: self.sb(st, n, shp, dt)
                ltri = sb("m_ltri", [128, 128], BF16)
                P.dma("pool", ltri[:], self.c_ltri, w=["m_ltri"])
                lo = sb("m_lo", [128, 16], F32)
                hi = sb("m_hi", [128, 16], F32)
                mid = sb("m_mid", [128, 16], F32)
                cmpt = sb("m_cmp", [128, NT, 16], F32)
                cntp = sb("m_cntp", [128, 16], BF16)
                cntpf = sb("m_cntpf", [128, 16], F32)
                gei = sb("m_gei", [128, 16], I32)
                lti = sb("m_lti", [128, 16], I32)
                pst = self.ps(st, "m_pst", [128, 16], F32)
                P.op("dve", lambda e: e.memset(lo[:], 0.0), w=["m_lo"])
                P.op("dve", lambda e: e.memset(hi[:], 1.0), w=["m_hi"])
                affv = aff[:].rearrange("p t e -> p e t")
                cmpv = cmpt[:].rearrange("p t e -> p e t")

                def count_ge(thr, thrk):
                    P.op("dve", lambda e: e.tensor_tensor(cmpt[:], aff[:], thr[:].unsqueeze(1).to_broadcast([128, NT, 16]), ALU.is_ge),
                         r=["m_aff", thrk], w=["m_cmp"])
                    P.op("dve", lambda e: e.tensor_reduce(cntpf[:], cmpv, AX.X, ALU.add), r=["m_cmp"], w=["m_cntpf"])
                    P.op("dve", lambda e: e.tensor_copy(cntp[:], cntpf[:]), r=["m_cntpf"], w=["m_cntp"])
                for it in range(NIT):
                    P.op("dve", lambda e: e.tensor_tensor(mid[:], lo[:], hi[:], ALU.add), r=["m_lo", "m_hi"], w=["m_mid"])
                    P.op("dve", lambda e: e.tensor_scalar(mid[:], mid[:], 0.5, None, ALU.mult), r=["m_mid"], w=["m_mid"])
                    count_ge(mid, "m_mid")
                    P.op("pe", lambda e: e.matmul(pst[:], self.ones_b[:], cntp[:], start=True, stop=True), r=["ones_b", "m_cntp"], w=["m_pst"])
                    P.op("dve", lambda e: e.tensor_scalar(gei[:], pst[:], float(C), None, ALU.is_ge), r=["m_pst"], w=["m_gei"])
                    P.op("dve", lambda e: e.tensor_scalar(lti[:], pst[:], float(C), None, ALU.is_lt), r=["m_pst"], w=["m_lti"])
                    P.op("dve", lambda e: e.copy_predicated(lo[:], gei[:], mid[:]), r=["m_gei", "m_mid", "m_lo"], w=["m_lo"])
                    P.op("dve", lambda e: e.copy_predicated(hi[:], lti[:], mid[:]), r=["m_lti", "m_mid", "m_hi"], w=["m_hi"])
                count_ge(lo, "m_lo")
                P.op("pe", lambda e: e.matmul(pst[:], ltri[:], cntp[:], start=True, stop=True), r=["m_ltri", "m_cntp"], w=["m_pst"])
                offs = sb("m_offs", [128, 16], F32)
                P.op("act", lambda e: e.copy(offs[:], pst[:]), r=["m_pst"], w=["m_offs"])
                cum = sb("m_cum", [128, NT, 16], F32)
                cumv = cum[:].rearrange("p t e -> p e t")
                onesr = sb("m_onesr", [128, NT], F32)
                P.op("dve", lambda e: e.memset(onesr[:], 1.0), w=["m_onesr"])
                for ex_ in range(16):
                    P.op("dve", lambda e, ex_=ex_: e.tensor_tensor_scan(cumv[:, ex_, :], onesr[:], cmpv[:, ex_, :], 0.0, ALU.mult, ALU.add),
                         r=["m_cmp", "m_onesr"], w=["m_cum"])
                P.op("dve", lambda e: e.tensor_tensor(cum[:], cum[:], cmpt[:], ALU.subtract), r=["m_cum", "m_cmp"], w=["m_cum"])
                P.op("dve", lambda e: e.tensor_tensor(cum[:], cum[:], offs[:].unsqueeze(1).to_broadcast([128, NT, 16]), ALU.add), r=["m_cum", "m_offs"], w=["m_cum"])
                P.op("dve", lambda e: e.tensor_scalar(cum[:], cum[:], -BIG, None, ALU.add), r=["m_cum"], w=["m_cum"])
                P.op("dve", lambda e: e.tensor_tensor(cum[:], cum[:], cmpt[:], ALU.mult), r=["m_cum", "m_cmp"], w=["m_cum"])
                P.op("dve", lambda e: e.tensor_scalar(cum[:], cum[:], BIG, None, ALU.add), r=["m_cum"], w=["m_cum"])
                P.op("dve", lambda e: e.tensor_copy(posi[:], cum[:].rearrange("p t e -> p (t e)")), r=["m_cum"], w=["m_posi"])
                if self.dbg:
                    P.dma("sp", self.dbg_posi, posi[:], r=["m_posi"], w=["dbg_posi"])
                    P.dma("sp", self.dbg_aff, aff[:].rearrange("p t e -> p (t e)"), r=["m_aff"], w=["dbg_aff"])
                P.barrier()
            with contextlib.ExitStack() as st:
                xr = self.p_xr
                for tt in range(NT):
                    b = tt % 2
                    P.dma("sp", xr[b][:], self.xrows[tt * 128:(tt + 1) * 128, :], r=["xrows"], w=["m_dr%d" % b])
                    for ex_ in range(16):
                        col = tt * 16 + ex_
                        P.dma_fn("pool", lambda e, b=b, ex_=ex_, col=col: e.indirect_dma_start(
                            out=self.xg[ex_][:, :], out_offset=bass.IndirectOffsetOnAxis(ap=posi[:, col:col + 1], axis=0),
                            in_=xr[b][:], in_offset=None, bounds_check=self.reg(e, C - 1), oob_is_err=False),
                            r=["m_dr%d" % b, "m_posi"], w=[("xg", ex_)])
                P.barrier()
        with contextlib.ExitStack() as st:
            sb = lambda n, shp, dt: self.sb(st, n, shp, dt)
            xgT = sb("e_xgT", [128, 16, C], BF16)
            hidT = sb("e_hidT", [128, 8, C], BF16)
            Wd = sb("e_Wd", [128, 8, D], BF16)
            wgb = [sb("e_wg%d" % i, [128, 16, 128], BF16) for i in range(2)]
            wub = [sb("e_wu%d" % i, [128, 16, 128], BF16) for i in range(2)]
            xrw = [sb("e_xr%d" % i, [128, RW], BF16) for i in range(2)]
            gates = sb("e_gates", [128, NCT], F32)
            tid = self.p_tid
            sg = sb("e_sg", [128, 512], F32)
            yrow = self.p_yrow
            pT = [self.ps(st, "e_pT%d" % i, [128, 4, 128], BF16) for i in range(2)]
            psG = self.ps(st, "e_psG", [128, 512], F32)
            psU = self.ps(st, "e_psU", [128, 512], F32)
            psY = [self.ps(st, "e_psY%d" % i, [128, 512], F32) for i in range(2)]
            nsubc = max(1, C // 512)
            subw = min(C, 512)
            tcnt = 0
            ycnt = 0
            for ex_ in range(16):
                for c in range(8):
                    P.dma("pool", Wd[:, :, c * 256:(c + 1) * 256], self.w_exp_down[l, ex_, :, c * 256:(c + 1) * 256].rearrange("(kc p) n -> p kc n", p=128), w=[("e_Wd", c)])
                for ct in range(NCT):
                    b = ct % 2
                    P.dma("sp", xrw[b][:], self.xg[ex_][ct * 128:(ct + 1) * 128, :], r=[("xg", ex_)], w=["e_xr%d" % b])
                    self.transpose_tile(xrw[b][:, 0:D], "e_xr%d" % b, pT, "e_pT%d", (lambda g4, ct=ct: xgT[:, g4 * 4:(g4 + 1) * 4, ct * 128:(ct + 1) * 128]), ("e_xgT", ct), cnt0=tcnt)
                    tcnt += 4
                    P.op("dve", lambda e, b=b, ct=ct, ex_=ex_: e.tensor_copy(gates[:, ct:ct + 1], xrw[b][:, D + 2 * ex_:D + 2 * ex_ + 2].bitcast(F32)), r=["e_xr%d" % b], w=["e_gates"])
                    P.op("dve", lambda e, b=b, ct=ct: e.tensor_copy(tid[:, ct:ct + 1], xrw[b][:, D + 32:D + 34].bitcast(I32)), r=["e_xr%d" % b], w=["e_tid"])
                xkeys = [("e_xgT", ct) for ct in range(NCT)]
                for fb in range(8):
                    wb_ = fb % 2
                    P.dma("pool", wgb[wb_][:], self.w_exp_gate[l, ex_, :, fb * 128:(fb + 1) * 128].rearrange("(kc p) n -> p kc n", p=128), w=["e_wg%d" % wb_])
                    P.dma("pool", wub[wb_][:], self.w_exp_up[l, ex_, :, fb * 128:(fb + 1) * 128].rearrange("(kc p) n -> p kc n", p=128), w=["e_wu%d" % wb_])
                    for sub in range(nsubc):
                        tsl = slice(sub * subw, (sub + 1) * subw)
                        for kc in range(16):
                            P.op("pe", lambda e, wb_=wb_, kc=kc, tsl=tsl: e.matmul(psG[:, 0:subw], wgb[wb_][:, kc, :], xgT[:, kc, tsl], start=(kc == 0), stop=(kc == 15)),
                                 r=["e_wg%d" % wb_] + xkeys, w=["e_psG"])
                        for kc in range(16):
                            P.op("pe", lambda e, wb_=wb_, kc=kc, tsl=tsl: e.matmul(psU[:, 0:subw], wub[wb_][:, kc, :], xgT[:, kc, tsl], start=(kc == 0), stop=(kc == 15)),
                                 r=["e_wu%d" % wb_] + xkeys, w=["e_psU"])
                        P.op("act", lambda e: e.activation(out=sg[:, 0:subw], in_=psG[:, 0:subw], func=AF.Silu), r=["e_psG"], w=["e_sg"])
                        P.op("dve", lambda e, fb=fb, tsl=tsl: e.tensor_tensor(hidT[:, fb, tsl], sg[:, 0:subw], psU[:, 0:subw], ALU.mult), r=["e_sg", "e_psU"], w=[("e_hidT", fb)])
                hkeys = [("e_hidT", fb) for fb in range(8)]
                for ct in range(NCT):
                    yb = ycnt % 2
                    ycnt += 1
                    for c4 in range(4):
                        pb = c4 % 2
                        for fc in range(8):
                            P.op("pe", lambda e, pb=pb, fc=fc, ct=ct, c4=c4: e.matmul(psY[pb][:], hidT[:, fc, ct * 128:(ct + 1) * 128], Wd[:, fc, c4 * 512:(c4 + 1) * 512], start=(fc == 0), stop=(fc == 7)),
                                 r=hkeys + [("e_Wd", 2 * c4), ("e_Wd", 2 * c4 + 1)], w=["e_psY%d" % pb])
                        P.op("dve" if c4 % 2 == 0 else "act",
                             (lambda e, pb=pb, yb=yb, c4=c4, ct=ct: e.tensor_scalar(yrow[yb][:, c4 * 512:(c4 + 1) * 512], psY[pb][:], gates[:, ct:ct + 1], None, ALU.mult)) if c4 % 2 == 0 else
                             (lambda e, pb=pb, yb=yb, c4=c4, ct=ct: e.activation(out=yrow[yb][:, c4 * 512:(c4 + 1) * 512], in_=psY[pb][:], func=AF.Copy, scale=gates[:, ct:ct + 1])),
                             r=["e_psY%d" % pb, "e_gates"], w=["e_yrow%d" % yb])
                    P.dma_fn("pool", lambda e, yb=yb, ct=ct: e.indirect_dma_start(
                        out=self.h[:, :], out_offset=bass.IndirectOffsetOnAxis(ap=tid[:, ct:ct + 1], axis=0),
                        in_=yrow[yb][:], in_offset=None, bounds_check=self.reg(e, S - 1), oob_is_err=True, compute_op=ALU.add),
                        r=["e_yrow%d" % yb, "e_tid"], w=["h"])
            P.barrier()

    def stage_ple(self, l):
        P, S = self.P, self.S
        with contextlib.ExitStack() as st:
            sb = lambda n, shp, dt: self.sb(st, n, shp, dt)
            Wg = sb("l_Wg", [128, 16, D], BF16)
            for c in range(8):
                P.dma("pool", Wg[:, :, c * 256:(c + 1) * 256], self.w_ple_gate[l, :, c * 256:(c + 1) * 256].rearrange("(kc p) n -> p kc n", p=128), w=[("l_Wg", c)])
            Wp = sb("l_Wp", [128, 2, D], BF16)
            for c in range(2):
                P.dma("pool", Wp[:, :, c * 1024:(c + 1) * 1024], self.w_ple_proj[l, :, c * 1024:(c + 1) * 1024].rearrange("(kc p) n -> p kc n", p=128), w=[("l_Wp", c)])
            gb = sb("l_gb", [128, D], F32)
            P.dma("sp", gb[:], self.norm_ple[l:l + 1, :].partition_broadcast(128), w=["l_gb"])
            hb = [sb("l_hb%d" % i, [128, D], F32) for i in range(2)]
            junk = sb("l_junk", [128, D], BF16)
            ss = [sb("l_ss%d" % i, [128, 1], F32) for i in range(2)]
            rs = [sb("l_rs%d" % i, [128, 1], F32) for i in range(2)]
            xn = [sb("l_xn%d" % i, [128, D], BF16) for i in range(2)]
            hT = [sb("l_hT%d" % i, [128, 16, 128], BF16) for i in range(2)]
            pTt = [sb("l_pTt%d" % i, [128, 2, 128], BF16) for i in range(2)]
            sg = sb("l_sg", [128, 512], F32)
            pT = [self.ps(st, "l_pT%d" % i, [128, 4, 128], BF16) for i in range(2)]
            psG = [self.ps(st, "l_psG%d" % i, [128, 512], F32) for i in range(2)]
            psP = [self.ps(st, "l_psP%d" % i, [128, 512], F32) for i in range(2)]
            for tt in range(S // 128):
                b = tt % 2
                t0 = tt * 128
                k = lambda n, b=b: "l_%s%d" % (n, b)
                P.dma("sp", hb[b][:], self.h[t0:t0 + 128, :], r=["h"], w=[k("hb")])
                P.dma("pool", pTt[b][:], self.pT[l, :, t0:t0 + 128].rearrange("(kc p) t -> p kc t", p=128), w=[k("pTt")])
                self.norm_tile(hb[b], k("hb"), gb, "l_gb", xn[b][:], k("xn"), ss[b], k("ss"), rs[b], k("rs"), junk, "l_junk")
                self.transpose_tile(xn[b][:], k("xn"), pT, "l_pT%d", (lambda g4, b=b: hT[b][:, g4 * 4:(g4 + 1) * 4, :]), k("hT"), cnt0=tt * 4)
                for c4 in range(4):
                    pb = c4 % 2
                    cs_ = slice(c4 * 512, (c4 + 1) * 512)
                    for kc in range(16):
                        P.op("pe", lambda e, b=b, pb=pb, kc=kc, cs_=cs_: e.matmul(psG[pb][:], hT[b][:, kc, :], Wg[:, kc, cs_], start=(kc == 0), stop=(kc == 15)),
                             r=[k("hT"), ("l_Wg", 2 * c4), ("l_Wg", 2 * c4 + 1)], w=["l_psG%d" % pb])
                    for kc in range(2):
                        P.op("pe", lambda e, b=b, pb=pb, kc=kc, cs_=cs_: e.matmul(psP[pb][:], pTt[b][:, kc, :], Wp[:, kc, cs_], start=(kc == 0), stop=(kc == 1)),
                             r=[k("pTt"), ("l_Wp", c4 // 2)], w=["l_psP%d" % pb])
                    P.op("act", lambda e, pb=pb: e.activation(out=sg[:], in_=psG[pb][:], func=AF.Sigmoid), r=["l_psG%d" % pb], w=["l_sg"])
                    P.op("dve", lambda e, pb=pb: e.tensor_tensor(sg[:], sg[:], psP[pb][:], ALU.mult), r=["l_sg", "l_psP%d" % pb], w=["l_sg"])
                    P.op("dve", lambda e, b=b, cs_=cs_: e.tensor_tensor(hb[b][:, cs_], hb[b][:, cs_], sg[:], ALU.add), r=["l_sg", k("hb")], w=[k("hb")])
                P.dma("sp", self.h[t0:t0 + 128, :], hb[b][:], r=[k("hb")], w=["h"])
            P.barrier()

    def build(self):
        self.declare()
        P = self.P
        self.load_consts()
        self.pib = self.sb(self.stack, "pib", [128, 1], F32)
        P.op("pool", lambda e: e.memset(self.pib[:], math.pi), w=["pib"])
        self.p_posi = self.sb(self.stack, "m_posi", [128, (self.S // 128) * 16], I32)
        self.p_xr = [self.sb(self.stack, "m_dr%d" % i, [128, 2112], BF16) for i in range(2)]
        self.p_tid = self.sb(self.stack, "e_tid", [128, max(1, self.S // 1024)], I32)
        self.p_yrow = [self.sb(self.stack, "e_yrow%d" % i, [128, D], F32) for i in range(2)]
        self.epsb = self.sb(self.stack, "epsb", [128, 1], F32)
        P.op("pool", lambda e: e.memset(self.epsb[:], EPS), w=["epsb"])
        P.dma("sp", self.h[:, :], self.x[:, :], w=["h"])
        if self.stop != "init":
            self.rope_tables()
        for l in range(self.L):
            if self.stop in ("init", "rope"):
                break
            self.stage_a(l)
            if self.stop in ("a", "a1"):
                break
            self.stage_attn(l)
            if self.stop == "attn":
                break
            self.stage_s5(l)
            if self.stop in ("s5", "s5raw"):
                break
            self.stage_c1(l)
            self.stage_c2(l)
            if self.stop == "c":
                break
            self.stage_moe(l)
            if self.stop == "moe":
                break
            self.stage_ple(l)
        P.finish()
        P.emit()
        self.stack.close()
        return self.nc


SEQ = 8192
DEPTH = 4
N_CORES = 8


def kernel(**inputs):
    inp = {k: np.asarray(v) for k, v in inputs.items()}
    B = inp["x"].shape[0]
    m = MK(SEQ, DEPTH, dbg=False)
    nc = m.build()
    consts = make_consts()
    s5 = prep_s5(inp)
    shared = {}
    for name in ["norm_mix", "w_in", "q_norm", "k_norm", "w_attn_br", "w_ssm_br", "w_out", "norm_ffn", "w_router",
                 "w_exp_gate", "w_exp_up", "w_exp_down", "norm_ple", "w_ple_gate", "w_ple_proj"]:
        shared[name] = np.ascontiguousarray(inp[name], dtype=np.float32)
    shared.update(s5)
    for k, v in consts.items():
        shared["c_" + k] = v
    per_b = []
    for b in range(B):
        per_b.append({
            "x": np.ascontiguousarray(inp["x"][b], dtype=np.float32),
            "pT": np.ascontiguousarray(inp["p"][:, b].transpose(0, 2, 1), dtype=np.float32),
            "positions": np.ascontiguousarray(inp["positions"][b:b + 1], dtype=np.int32),
        })
    in_maps = []
    for c in range(N_CORES):
        b = (c * B) // N_CORES
        d = dict(shared)
        d.update(per_b[b])
        in_maps.append({k: v for k, v in d.items() if k in m.din})
    res = run_bass_kernel_spmd(nc, in_maps, core_ids=list(range(N_CORES)))
    outs = []
    for b in range(B):
        c = (b * N_CORES) // B
        outs.append(np.asarray(res.results[c]["h"], dtype=np.float32))
    return np.stack(outs, axis=0)
```

```python
import math
import contextlib
import numpy as np
import concourse.bass as bass
import concourse.mybir as mybir
from concourse.bass_utils import run_bass_kernel_spmd


F32 = mybir.dt.float32
BF16 = mybir.dt.bfloat16
I32 = mybir.dt.int32
U32 = mybir.dt.uint32
AF = mybir.ActivationFunctionType
ALU = mybir.AluOpType
AX = mybir.AxisListType


class Prog:
    ENGS = ["pe", "dve", "act", "pool", "sp"]

    def __init__(self, nc, stack, ndma=16):
        self.nc = nc
        self.ins = {e: [] for e in self.ENGS}
        self.sems = {e: stack.enter_context(nc.semaphore("cs_" + e)) for e in self.ENGS}
        self.qslots = {"sp": list(range(0, 10)), "pool": list(range(10, 18)), "act": list(range(18, 20))}
        self.ndma = ndma = 20
        self.dsems = [stack.enter_context(nc.semaphore("ds%d" % i)) for i in range(ndma)]
        self.dcount = [0] * ndma
        self.qnext = {q: 0 for q in self.qslots}
        self.last_w = {}
        self.readers = {}
        self.psum_keys = set()

    def _deps(self, reads, writes):
        deps = []
        for b in reads:
            ev = self.last_w.get(b)
            if ev is not None:
                deps.append(ev)
            if b in self.psum_keys:
                deps.extend(self.readers.get(b, ()))
        for b in writes:
            ev = self.last_w.get(b)
            if ev is not None:
                deps.append(ev)
            deps.extend(self.readers.get(b, ()))
        return deps

    def _update(self, ev, reads, writes):
        for b in reads:
            lst = self.readers.setdefault(b, [])
            if ev[0] == "c":
                lst[:] = [x for x in lst if not (x[0] == "c" and x[1] == ev[1])]
            lst.append(ev)
        for b in writes:
            self.last_w[b] = ev
            self.readers[b] = []

    def op(self, eng, fn, r=(), w=()):
        deps = self._deps(r, w)
        idx = len(self.ins[eng])
        self.ins[eng].append(dict(fn=fn, deps=deps, dma=None, sig=False))
        self._update(("c", eng, idx), r, w)

    def dma(self, q, out, in_, r=(), w=(), **kw):
        fn = lambda e: e.dma_start(out=out, in_=in_, **kw)
        self.dma_fn(q, fn, r, w)

    def dma_fn(self, q, fn, r=(), w=()):
        deps = self._deps(r, w)
        sl = self.qslots[q]
        s = sl[self.qnext[q] % len(sl)]
        self.qnext[q] += 1
        if self.dcount[s] > 0:
            deps.append(("d", s, 16 * self.dcount[s]))
        self.dcount[s] += 1
        tgt = 16 * self.dcount[s]
        self.ins[q].append(dict(fn=fn, deps=deps, dma=(s, tgt), sig=False))
        self._update(("d", s, tgt), r, w)

    def barrier(self):
        evs = []
        for e in self.ENGS:
            n = len(self.ins[e])
            for i in range(n - 1, -1, -1):
                if self.ins[e][i]["dma"] is None and self.ins[e][i]["fn"] is not None:
                    evs.append(("c", e, i))
                    break
        for s in range(self.ndma):
            if self.dcount[s] > 0:
                evs.append(("d", s, 16 * self.dcount[s]))
        for e in self.ENGS:
            self.ins[e].append(dict(fn=None, deps=list(evs), dma=None, sig=False))
        self.last_w = {}
        self.readers = {}

    def emit(self):
        nc = self.nc
        plans = {}
        for e in self.ENGS:
            wc = {x: -1 for x in self.ENGS}
            wd = [0] * self.ndma
            plan = []
            for idx, it in enumerate(self.ins[e]):
                waits = []
                for ev in it["deps"]:
                    if ev[0] == "c":
                        _, e2, i2 = ev
                        if e2 == e and e == "pe":
                            continue
                        if e2 == e and i2 >= idx:
                            continue
                        if wc[e2] >= i2:
                            continue
                        wc[e2] = i2
                        self.ins[e2][i2]["sig"] = True
                        waits.append(("c", e2, i2))
                    else:
                        _, s, tgt = ev
                        if wd[s] >= tgt:
                            continue
                        wd[s] = tgt
                        waits.append(ev)
                plan.append(waits)
            plans[e] = plan
        sigval = {}
        for e in self.ENGS:
            c = 0
            for idx, it in enumerate(self.ins[e]):
                if it["sig"]:
                    c += 1
                    sigval[(e, idx)] = c
        self.sig_totals = {e: sum(1 for it in self.ins[e] if it["sig"]) for e in self.ENGS}

        def run(e, eng):
            for idx, it in enumerate(self.ins[e]):
                for wv in plans[e][idx]:
                    if wv[0] == "c":
                        eng.wait_ge(self.sems[wv[1]], sigval[(wv[1], wv[2])])
                    else:
                        eng.wait_ge(self.dsems[wv[1]], wv[2])
                if it["fn"] is None:
                    continue
                ins = it["fn"](eng)
                if it["dma"] is not None:
                    ins.then_inc(self.dsems[it["dma"][0]], 16)
                elif it["sig"]:
                    ins.then_inc(self.sems[e], 1)

        with nc.Block() as block:
            @block.tensor
            def _(eng):
                run("pe", eng)

            @block.vector
            def _(eng):
                run("dve", eng)

            @block.scalar
            def _(eng):
                run("act", eng)

            @block.gpsimd
            def _(eng):
                run("pool", eng)

            @block.sync
            def _(eng):
                run("sp", eng)

    def finish(self):
        evs = [("d", s, 16 * self.dcount[s]) for s in range(self.ndma) if self.dcount[s] > 0]
        self.ins["sp"].append(dict(fn=None, deps=evs, dma=None, sig=False))


D = 2048
NIN = 9728
HD = 128
NH = 12
EPS = 1e-6
TWO_PI = 2.0 * math.pi


def make_consts():
    c = {}
    c["ident"] = np.eye(128, dtype=np.float32)
    c["ones"] = np.ones((128, 128), dtype=np.float32)
    rm = np.zeros((128, 128), dtype=np.float32)
    for i in range(16):
        rm[i + 16, i] = -1.0
        rm[i, i + 16] = 1.0
    c["rmat"] = rm
    invf = np.zeros((128, 1), dtype=np.float32)
    fr = np.power(np.float32(500000.0), -np.arange(16, dtype=np.float32) * np.float32(2.0) / np.float32(32.0)).astype(np.float32)
    invf[0:16, 0] = fr
    invf[16:32, 0] = fr
    c["invf"] = invf
    kk = np.arange(128)[:, None]
    qq = np.arange(128)[None, :]
    m = np.zeros((128, 3, 128), dtype=np.float32)
    m[:, 0, :] = (kk - qq >= 64)
    m[:, 1, :] = (np.abs(kk - qq) <= 64)
    m[:, 2, :] = (kk - qq <= -64)
    c["amask"] = m.reshape(128, 384)
    c["ltri"] = (np.arange(128)[:, None] < np.arange(128)[None, :]).astype(np.float32)
    return c


def prep_s5(inp):
    L = inp["ssm_a_re"].shape[0]
    o = {}
    def st(a):
        return np.ascontiguousarray(a.reshape(L, 2, 32, 128).transpose(0, 1, 3, 2))
    o["s5_are"] = st(inp["ssm_a_re"])
    o["s5_aim"] = st(inp["ssm_a_im"])
    ldt = np.repeat(inp["ssm_log_dt"][:, :, :, None], 64, axis=3)
    o["s5_ldt"] = st(ldt)
    for nm, src in (("s5_bre", "ssm_b_re"), ("s5_bim", "ssm_b_im")):
        b = inp[src].reshape(L, 2, 32, 2, 64, 16)
        out = np.zeros((L, 2, 32, 32, 128), np.float32)
        for gi in range(2):
            out[:, :, :, gi * 16:(gi + 1) * 16, gi * 64:(gi + 1) * 64] = b[:, :, :, gi].transpose(0, 1, 2, 4, 3)
        o[nm] = out
    for nm, src in (("s5_cre", "ssm_c_re"), ("s5_cim", "ssm_c_im")):
        c = inp[src].reshape(L, 2, 32, 2, 16, 64)
        out = np.zeros((L, 2, 32, 128, 32), np.float32)
        for gi in range(2):
            out[:, :, :, gi * 64:(gi + 1) * 64, gi * 16:(gi + 1) * 16] = c[:, :, :, gi].transpose(0, 1, 2, 4, 3)
        o[nm] = out
    o["s5_d"] = np.ascontiguousarray(inp["ssm_d"].reshape(L, 32, 32, 1))
    return o


class MK:
    def __init__(self, S, L, dbg=False, stop=None):
        self.S, self.L, self.dbg, self.stop = S, L, dbg, stop
        self.TS = min(S, 2048)
        self.nc = nc = bass.Bass("TRN2", target_bir_lowering=False)
        self.stack = contextlib.ExitStack()
        self.P = Prog(nc, self.stack)
        self.din = {}
        self.dscr = {}

    def reg(self, eng, val):
        if not hasattr(self, "_regs"):
            self._regs = {}
        if val not in self._regs:
            self._regs[val] = eng.to_reg(val)
        return self._regs[val]

    def inp(self, name, shape, dt=F32):
        t = self.nc.dram_tensor(name, list(shape), dt, kind="ExternalInput").ap()
        self.din[name] = t
        return t

    def scr(self, name, shape, dt, out=False):
        kind = "ExternalOutput" if (out or self.dbg) else "Internal"
        t = self.nc.dram_tensor(name, list(shape), dt, kind=kind).ap()
        self.dscr[name] = t
        return t

    def sb(self, st, name, shape, dt):
        self._uid = getattr(self, "_uid", 0) + 1
        return st.enter_context(self.nc.sbuf_tensor("%s__%d" % (name, self._uid), list(shape), dt))

    def ps(self, st, name, shape, dt=F32):
        self.P.psum_keys.add(name)
        self._uid = getattr(self, "_uid", 0) + 1
        return st.enter_context(self.nc.psum_tensor("%s__%d" % (name, self._uid), list(shape), dt))

    def declare(self):
        S, L = self.S, self.L
        i = self.inp
        self.x = i("x", [S, D])
        self.pT = i("pT", [L, 256, S])
        self.pos = i("positions", [1, S], I32)
        self.norm_mix = i("norm_mix", [L, D])
        self.w_in = i("w_in", [L, D, NIN])
        self.q_norm = i("q_norm", [L, 128])
        self.k_norm = i("k_norm", [L, 128])
        self.w_attn_br = i("w_attn_br", [L, 512, D])
        self.w_ssm_br = i("w_ssm_br", [L, 1024, 2 * D])
        self.w_out = i("w_out", [L, D, D])
        self.norm_ffn = i("norm_ffn", [L, D])
        self.norm_ple = i("norm_ple", [L, D])
        self.w_ple_gate = i("w_ple_gate", [L, D, D])
        self.w_ple_proj = i("w_ple_proj", [L, 256, D])
        self.w_router = i("w_router", [L, D, 16])
        self.w_exp_gate = i("w_exp_gate", [L, 16, D, 1024])
        self.w_exp_up = i("w_exp_up", [L, 16, D, 1024])
        self.w_exp_down = i("w_exp_down", [L, 16, 1024, D])
        self.c_ltri = i("c_ltri", [128, 128])
        self.s5_are = i("s5_are", [L, 2, 128, 32])
        self.s5_aim = i("s5_aim", [L, 2, 128, 32])
        self.s5_ldt = i("s5_ldt", [L, 2, 128, 32])
        self.s5_bre = i("s5_bre", [L, 2, 32, 32, 128])
        self.s5_bim = i("s5_bim", [L, 2, 32, 32, 128])
        self.s5_cre = i("s5_cre", [L, 2, 32, 128, 32])
        self.s5_cim = i("s5_cim", [L, 2, 32, 128, 32])
        self.s5_d = i("s5_d", [L, 32, 32, 1])
        for n in ["ident", "ones", "rmat"]:
            setattr(self, "c_" + n, i("c_" + n, [128, 128]))
        self.c_invf = i("c_invf", [128, 1])
        self.c_amask = i("c_amask", [128, 384])
        s = self.scr
        self.h = s("h", [S, D], F32, out=True)
        self.cosT = s("cosT", [128, S], F32)
        self.sinT = s("sinT", [128, S], F32)
        self.qkT = s("qkT", [24, 128, S], BF16)
        self.v = s("v", [S, 1536], BF16)
        self.uT = s("uT", [1024, S], BF16)
        self.gT = s("gT", [4096, S], BF16)
        self.attT = s("attT", [512, S], BF16)
        self.ygT = s("ygT", [1024, S], BF16)
        self.mergedT = s("mergedT", [2048, S], BF16)
        self.xrows = s("xrows", [S, 2112], BF16)
        self.xg = [s("xg%d" % e_, [S // 8, 2112], BF16) for e_ in range(16)]
        if self.dbg:
            self.dbg_posi = s("dbg_posi", [128, (S // 128) * 16], I32)
            self.dbg_aff = s("dbg_aff", [128, (S // 128) * 16], F32)
        if self.stop == "s5raw":
            self.dbg_y = s("dbg_y", [1024, S], F32)

    def load_consts(self):
        P, st = self.P, self.stack
        self.ident_b = self.sb(st, "ident_b", [128, 128], BF16)
        self.ones_b = self.sb(st, "ones_b", [128, 128], BF16)
        self.rmat_b = self.sb(st, "rmat_b", [128, 128], BF16)
        self.ident_f = self.sb(st, "ident_f", [128, 128], F32)
        self.ones_f = self.sb(st, "ones_f", [128, 128], F32)
        self.invf = self.sb(st, "invf", [128, 1], F32)
        P.dma("pool", self.ident_b[:], self.c_ident, w=["ident_b"])
        P.dma("pool", self.ones_b[:], self.c_ones, w=["ones_b"])
        P.dma("pool", self.rmat_b[:], self.c_rmat, w=["rmat_b"])
        P.dma("sp", self.ident_f[:], self.c_ident, w=["ident_f"])
        P.dma("sp", self.ones_f[:], self.c_ones, w=["ones_f"])
        P.dma("sp", self.invf[:], self.c_invf, w=["invf"])

    def sin_reduced(self, x, xk, out, ok, ti, tik, tf, tfk, shift):
        P = self.P
        P.op("dve", lambda e: e.tensor_scalar(tf[:], x[:], shift, 1.0 / TWO_PI, ALU.add, ALU.mult), r=[xk], w=[tfk])
        P.op("dve", lambda e: e.tensor_copy(ti[:], tf[:]), r=[tfk], w=[tik])
        P.op("dve", lambda e: e.tensor_copy(tf[:], ti[:]), r=[tik], w=[tfk])
        P.op("dve", lambda e: e.scalar_tensor_tensor(tf[:], tf[:], -TWO_PI, x[:], ALU.mult, ALU.add), r=[tfk, xk], w=[tfk])
        P.op("dve", lambda e: e.tensor_scalar(tf[:], tf[:], shift - math.pi, -2.0 * math.pi + 2 * math.pi, ALU.max, ALU.add) if False else
             e.tensor_scalar(tf[:], tf[:], shift, -math.pi, ALU.add, ALU.max), r=[tfk], w=[tfk])
        P.op("dve", lambda e: e.tensor_scalar(tf[:], tf[:], math.pi, None, ALU.min), r=[tfk], w=[tfk])
        P.op("act", lambda e: e.activation(out=out[:], in_=tf[:], func=AF.Sin), r=[tfk], w=[ok])

    def rope_tables(self):
        P, S = self.P, self.S
        CH = min(S, 2048)
        with contextlib.ExitStack() as st:
            pi_ = self.sb(st, "rp_i", [128, CH], I32)
            pf = self.sb(st, "rp_f", [128, CH], F32)
            m1 = self.sb(st, "rp_m1", [128, CH], F32)
            m2 = self.sb(st, "rp_m2", [128, CH], F32)
            sn = self.sb(st, "rp_sn", [128, CH], F32)
            cs = self.sb(st, "rp_cs", [128, CH], F32)
            for c in range(S // CH):
                sl = slice(c * CH, (c + 1) * CH)
                P.dma("sp", pi_[:], self.pos[0:1, sl].partition_broadcast(128), w=["rp_i"])
                P.op("dve", lambda e: e.tensor_copy(pf[:], pi_[:]), r=["rp_i"], w=["rp_f"])
                P.op("dve", lambda e: e.tensor_scalar(m1[:], pf[:], self.invf[:, 0:1], None, ALU.mult), r=["rp_f", "invf"], w=["rp_m1"])
                self.sin_reduced(m1, "rp_m1", sn, "rp_sn", pi_, "rp_i", m2, "rp_m2", 0.0)
                self.sin_reduced(m1, "rp_m1", cs, "rp_cs", pi_, "rp_i", m2, "rp_m2", math.pi / 2)
                P.dma("sp", self.sinT[:, sl], sn[:], r=["rp_sn"], w=["sinT"])
                P.dma("sp", self.cosT[:, sl], cs[:], r=["rp_cs"], w=["cosT"])
            P.barrier()

    def norm_transpose(self, st, src, T0, ntok, gain_row, hnT, key, pfx):
        P = self.P
        gb = self.sb(st, pfx + "gb", [128, D], F32)
        P.dma("sp", gb[:], gain_row.partition_broadcast(128), w=[pfx + "gb"])
        hb = [self.sb(st, pfx + "hb%d" % i, [128, D], F32) for i in range(2)]
        junk = self.sb(st, pfx + "junk", [128, D], BF16)
        ss = [self.sb(st, pfx + "ss%d" % i, [128, 1], F32) for i in range(2)]
        rs = [self.sb(st, pfx + "rs%d" % i, [128, 1], F32) for i in range(2)]
        xn = [self.sb(st, pfx + "xn%d" % i, [128, D], BF16) for i in range(2)]
        pT = [self.ps(st, pfx + "pT%d" % i, [128, 4, 128], BF16) for i in range(2)]
        for tt in range(ntok // 128):
            b = tt % 2
            hbk, ssk, rsk, xnk = pfx + "hb%d" % b, pfx + "ss%d" % b, pfx + "rs%d" % b, pfx + "xn%d" % b
            t0 = T0 + tt * 128
            P.dma("sp", hb[b][:], src[t0:t0 + 128, :], w=[hbk])
            P.op("act", lambda e, b=b: e.activation(out=junk[:], in_=hb[b][:], func=AF.Square, accum_out=ss[b][:, 0:1]),
                 r=[hbk], w=[pfx + "junk", ssk])
            P.op("act", lambda e, b=b: e.activation(out=rs[b][:], in_=ss[b][:], func=AF.Sqrt, scale=1.0 / D, bias=self.epsb[:, 0:1]), r=[ssk, "epsb"], w=[rsk])
            P.op("dve", lambda e, b=b: e.reciprocal(rs[b][:], rs[b][:]), r=[rsk], w=[rsk])
            P.op("dve", lambda e, b=b: e.scalar_tensor_tensor(xn[b][:], hb[b][:], rs[b][:, 0:1], gb[:], ALU.mult, ALU.mult),
                 r=[hbk, rsk, pfx + "gb"], w=[xnk])
            for g4 in range(4):
                pb = (tt * 4 + g4) % 2
                pk = pfx + "pT%d" % pb
                for j in range(4):
                    kc = g4 * 4 + j
                    P.op("pe", lambda e, b=b, pb=pb, j=j, kc=kc: e.transpose(pT[pb][:, j, :], xn[b][:, kc * 128:(kc + 1) * 128], self.ident_b[:]),
                         r=[xnk, "ident_b"], w=[pk])
                eng = "act" if g4 % 2 == 0 else "dve"
                if eng == "act":
                    P.op("act", lambda e, pb=pb, g4=g4, tt=tt: e.copy(hnT[:, g4 * 4:(g4 + 1) * 4, tt * 128:(tt + 1) * 128], pT[pb][:]),
                         r=[pk], w=[(key, tt)])
                else:
                    P.op("dve", lambda e, pb=pb, g4=g4, tt=tt: e.tensor_copy(hnT[:, g4 * 4:(g4 + 1) * 4, tt * 128:(tt + 1) * 128], pT[pb][:]),
                         r=[pk], w=[(key, tt)])

    def stage_a(self, l):
        P, S, TS = self.P, self.S, self.TS
        nsub = TS // 512
        ntt = TS // 128
        for sup in range(S // TS):
            T0 = sup * TS
            with contextlib.ExitStack() as st:
                hnT = self.sb(st, "a_hnT", [128, 16, TS], BF16)
                with contextlib.ExitStack() as st2:
                    self.norm_transpose(st2, self.h, T0, TS, self.norm_mix[l:l + 1, :], hnT, "a_hnT", "an_")
                    P.barrier()
                if self.stop == "norm":
                    self.dbg_hnT = self.scr("dbg_hnT", [128, 16, TS], BF16)
                    P.dma("sp", self.dbg_hnT, hnT[:], r=[("a_hnT", tt) for tt in range(ntt)], w=["dbg_hnT"])
                    P.barrier()
                    return
                hkeys = [("a_hnT", tt) for tt in range(ntt)]
                cosb = self.sb(st, "a_cos", [128, TS], F32)
                sinb = self.sb(st, "a_sin", [128, TS], F32)
                P.dma("sp", cosb[:], self.cosT[:, T0:T0 + TS], r=["cosT"], w=["a_cos"])
                P.dma("sp", sinb[:], self.sinT[:, T0:T0 + TS], r=["sinT"], w=["a_sin"])
                gq = self.sb(st, "a_gq", [128, 2], F32)
                import os
                DBG = os.environ.get("DBG", "")
                if "nogq" in DBG:
                    P.op("dve", lambda e: e.memset(gq[:], 1.0), w=["a_gq"])
                else:
                    P.dma("sp", gq[:, 0:1], self.q_norm[l:l + 1, :].rearrange("o p -> p o"), w=["a_gq"])
                    P.dma("sp", gq[:, 1:2], self.k_norm[l:l + 1, :].rearrange("o p -> p o"), w=["a_gq"])
                wb = [self.sb(st, "a_wb%d" % i, [128, 16, 256], BF16) for i in range(3)]
                psm = [self.ps(st, "a_ps%d" % i, [128, 512], F32) for i in range(3)]
                ps_ss = self.ps(st, "a_psss", [128, 512], F32)
                ps_rot = self.ps(st, "a_psrot", [128, 512], F32)
                sq = self.sb(st, "a_sq", [128, 512], BF16)
                qg = self.sb(st, "a_qg", [128, 512], BF16)
                rstd = self.sb(st, "a_rstd", [128, 512], F32)
                t1 = self.sb(st, "a_t1", [128, 512], F32)
                t2 = self.sb(st, "a_t2", [128, 512], F32)
                ob = [self.sb(st, "a_ob%d" % i, [128, TS], BF16) for i in range(2)]
                wcount = [0]
                mmcount = [0]
                ocount = [0]

                def load_w(col0, ncols):
                    i = wcount[0] % 3
                    wcount[0] += 1
                    src = self.w_in[l, :, col0:col0 + ncols].rearrange("(kc p) n -> p kc n", p=128)
                    P.dma("pool", wb[i][:, :, 0:ncols], src, w=["a_wb%d" % i])
                    return i

                fm_cols = [(j * 128, "qk", j) for j in range(24)] + \
                          [(4608 + j * 128, "u", j) for j in range(8)] + \
                          [(5632 + j * 128, "g", j) for j in range(32)]
                for pair in range(len(fm_cols) // 2):
                    if self.stop == 'a1' and pair not in (0, 12, 16):
                        continue
                    col0 = fm_cols[2 * pair][0]
                    wi = load_w(col0, 256)
                    for half in range(2):
                        _, kind, j = fm_cols[2 * pair + half]
                        oi = ocount[0] % 2
                        ocount[0] += 1
                        okey = "a_ob%d" % oi
                        for sub in range(nsub):
                            pi = mmcount[0] % 3
                            mmcount[0] += 1
                            pk = "a_ps%d" % pi
                            tsl = slice(sub * 512, (sub + 1) * 512)
                            for kc in range(16):
                                P.op("pe", lambda e, pi=pi, wi=wi, half=half, kc=kc, tsl=tsl: e.matmul(
                                    psm[pi][:], wb[wi][:, kc, half * 128:(half + 1) * 128], hnT[:, kc, tsl],
                                    start=(kc == 0), stop=(kc == 15)),
                                    r=["a_wb%d" % wi] + hkeys[sub * 4:(sub + 1) * 4], w=[pk])
                            if kind == "qk" and "noepi" in DBG:
                                P.op("act", lambda e, pi=pi, oi=oi, tsl=tsl: e.copy(ob[oi][:, tsl], psm[pi][:]), r=[pk], w=[okey])
                            elif kind == "qk":
                                gi = 0 if j < 12 else 1
                                P.op("act", lambda e, pi=pi: e.activation(out=sq[:], in_=psm[pi][:], func=AF.Square), r=[pk], w=["a_sq"])
                                P.op("dve", lambda e, pi=pi, gi=gi: e.tensor_scalar(qg[:], psm[pi][:], gq[:, gi:gi + 1], None, ALU.mult),
                                     r=[pk, "a_gq"], w=["a_qg"])
                                P.op("pe", lambda e: e.matmul(ps_ss[:], self.ones_b[:], sq[:], start=True, stop=True),
                                     r=["ones_b", "a_sq"], w=["a_psss"])
                                P.op("pe", lambda e: e.matmul(ps_rot[:], self.rmat_b[:], qg[:], start=True, stop=True),
                                     r=["rmat_b", "a_qg"], w=["a_psrot"])
                                if "e1" in DBG:
                                    P.op("act", lambda e, oi=oi, tsl=tsl: e.copy(ob[oi][:, tsl], ps_rot[:]), r=["a_psrot"], w=[okey])
                                    P.op("act", lambda e, oi=oi, tsl=tsl: e.copy(t1[:], ps_ss[:]), r=["a_psss"], w=["a_t1"])
                                    continue
                                P.op("act", lambda e: e.activation(out=rstd[:], in_=ps_ss[:], func=AF.Sqrt, scale=1.0 / HD, bias=self.epsb[:, 0:1]),
                                     r=["a_psss", "epsb"], w=["a_rstd"])
                                if "e2" in DBG:
                                    P.op("dve", lambda e: e.reciprocal(t1[:], rstd[:]), r=["a_rstd"], w=["a_t1"])
                                    P.op("act", lambda e, oi=oi, tsl=tsl: e.copy(ob[oi][:, tsl], ps_rot[:]), r=["a_psrot"], w=[okey])
                                    continue
                                P.op("dve", lambda e: e.reciprocal(rstd[:], rstd[:]), r=["a_rstd"], w=["a_rstd"])
                                P.op("dve", lambda e, tsl=tsl: e.tensor_tensor(t1[:], qg[:], cosb[:, tsl], ALU.mult), r=["a_qg", "a_cos"], w=["a_t1"])
                                P.op("dve", lambda e, tsl=tsl: e.tensor_tensor(t2[:], ps_rot[:], sinb[:, tsl], ALU.mult), r=["a_psrot", "a_sin"], w=["a_t2"])
                                P.op("dve", lambda e: e.tensor_tensor(t1[:], t1[:], t2[:], ALU.add), r=["a_t1", "a_t2"], w=["a_t1"])
                                P.op("dve", lambda e, oi=oi, tsl=tsl: e.tensor_tensor(ob[oi][:, tsl], t1[:], rstd[:], ALU.mult),
                                     r=["a_t1", "a_rstd"], w=[okey])
                            elif kind == "u":
                                P.op("act", lambda e, pi=pi, oi=oi, tsl=tsl: e.copy(ob[oi][:, tsl], psm[pi][:]), r=[pk], w=[okey])
                            else:
                                P.op("act", lambda e, pi=pi, oi=oi, tsl=tsl: e.activation(out=ob[oi][:, tsl], in_=psm[pi][:], func=AF.Sigmoid),
                                     r=[pk], w=[okey])
                        if kind == "qk":
                            P.dma("sp", self.qkT[j, :, T0:T0 + TS], ob[oi][:], r=[okey], w=["qkT"])
                        elif kind == "u":
                            P.dma("sp", self.uT[j * 128:(j + 1) * 128, T0:T0 + TS], ob[oi][:], r=[okey], w=["uT"])
                        else:
                            P.dma("sp", self.gT[j * 128:(j + 1) * 128, T0:T0 + TS], ob[oi][:], r=[okey], w=["gT"])
                vo = [self.sb(st, "a_vo%d" % i, [128, 256], BF16) for i in range(2)]
                vcount = 0
                for c in range(6):
                    if self.stop == 'a1':
                        continue
                    wi = load_w(3072 + c * 256, 256)
                    for tt in range(ntt):
                        pi = mmcount[0] % 3
                        mmcount[0] += 1
                        pk = "a_ps%d" % pi
                        for kc in range(16):
                            P.op("pe", lambda e, pi=pi, wi=wi, kc=kc, tt=tt: e.matmul(
                                psm[pi][:, 0:256], hnT[:, kc, tt * 128:(tt + 1) * 128], wb[wi][:, kc, :],
                                start=(kc == 0), stop=(kc == 15)),
                                r=["a_wb%d" % wi, ("a_hnT", tt)], w=[pk])
                        vi = vcount % 2
                        vcount += 1
                        P.op("act", lambda e, pi=pi, vi=vi: e.copy(vo[vi][:], psm[pi][:, 0:256]), r=[pk], w=["a_vo%d" % vi])
                        P.dma("sp", self.v[T0 + tt * 128:T0 + (tt + 1) * 128, c * 256:(c + 1) * 256], vo[vi][:],
                              r=["a_vo%d" % vi], w=["v"])
                P.barrier()
        return

    def stage_attn(self, l):
        P, S = self.P, self.S
        scale = float(HD) ** -0.5
        with contextlib.ExitStack() as st:
            amask = self.sb(st, "t_amask", [128, 3, 128], BF16)
            P.dma("pool", amask[:], self.c_amask.rearrange("p (j q) -> p j q", j=3), w=["t_amask"])
            acc_n = self.sb(st, "t_accn", [128, S], F32)
            acc_d = self.sb(st, "t_accd", [128, S], F32)
            qT = self.sb(st, "t_qT", [128, S], BF16)
            kT = self.sb(st, "t_kT", [128, S], BF16)
            vt = self.sb(st, "t_vt", [128, S // 128, 128], BF16)
            es = [self.sb(st, "t_es%d" % i, [128, 3, 128], BF16) for i in range(2)]
            em = [self.sb(st, "t_em%d" % i, [128, 3, 128], BF16) for i in range(2)]
            ps_s = [self.ps(st, "t_pss%d" % i, [128, 3, 128], F32) for i in range(2)]
            ps_o = [self.ps(st, "t_pso%d" % i, [128, 128], F32) for i in range(2)]
            ps_d = [self.ps(st, "t_psd%d" % i, [128, 128], F32) for i in range(2)]
            ob = self.sb(st, "t_ob", [128, S], BF16)
            cnt = 0
            for slot in range(4):
                for g, d in enumerate((1, 4, 16)):
                    head = 4 * g + slot
                    n = S // d // 128
                    P.dma("sp", qT[:], self.qkT[head, :, :], r=["qkT"], w=["t_qT"])
                    P.dma("sp", kT[:], self.qkT[12 + head, :, :], r=["qkT"], w=["t_kT"])
                    vh = self.v[:, head * 128:(head + 1) * 128].rearrange("(jt jp d) c -> d jp jt c", d=d, jp=128)
                    for r in range(d):
                        P.dma("sp", vt[:, r * n:(r + 1) * n, :], vh[r], r=["v"], w=["t_vt"])
                    qv = qT[:].rearrange("p (j d) -> p d j", d=d)
                    kv = kT[:].rearrange("p (j d) -> p d j", d=d)
                    anv = acc_n[:].rearrange("p (j d) -> p d j", d=d)
                    adv = acc_d[:].rearrange("p (j d) -> p d j", d=d)
                    for r in range(d):
                        for i in range(n):
                            b = cnt % 2
                            cnt += 1
                            kts = [kt for kt in (i - 1, i, i + 1) if 0 <= kt < n]
                            jlo, jhi = kts[0] - i + 1, kts[-1] - i + 2
                            qs = slice(i * 128, (i + 1) * 128)
                            for kt in kts:
                                jj = kt - i + 1
                                P.op("pe", lambda e, b=b, jj=jj, r=r, kt=kt, qs=qs, kv=kv, qv=qv: e.matmul(
                                    ps_s[b][:, jj, :], kv[:, r, kt * 128:(kt + 1) * 128], qv[:, r, qs], start=True, stop=True),
                                    r=["t_kT", "t_qT"], w=["t_pss%d" % b])
                            P.op("act", lambda e, b=b, jlo=jlo, jhi=jhi: e.activation(out=es[b][:, jlo:jhi, :], in_=ps_s[b][:, jlo:jhi, :], func=AF.Exp, scale=scale),
                                 r=["t_pss%d" % b], w=["t_es%d" % b])
                            P.op("dve", lambda e, b=b, jlo=jlo, jhi=jhi: e.tensor_tensor(em[b][:, jlo:jhi, :], es[b][:, jlo:jhi, :], amask[:, jlo:jhi, :], ALU.mult),
                                 r=["t_es%d" % b, "t_amask"], w=["t_em%d" % b])
                            for kt in kts:
                                jj = kt - i + 1
                                P.op("pe", lambda e, b=b, jj=jj, r=r, kt=kt, n=n, kts=kts: e.matmul(
                                    ps_o[b][:], vt[:, r * n + kt, :], em[b][:, jj, :], start=(kt == kts[0]), stop=(kt == kts[-1])),
                                    r=["t_vt", "t_em%d" % b], w=["t_pso%d" % b])
                            for kt in kts:
                                jj = kt - i + 1
                                P.op("pe", lambda e, b=b, jj=jj, kt=kt, kts=kts: e.matmul(
                                    ps_d[b][:], self.ones_b[:], em[b][:, jj, :], start=(kt == kts[0]), stop=(kt == kts[-1])),
                                    r=["ones_b", "t_em%d" % b], w=["t_psd%d" % b])
                            if g == 0:
                                P.op("act", lambda e, b=b, r=r, qs=qs, anv=anv: e.copy(anv[:, r, qs], ps_o[b][:]), r=["t_pso%d" % b], w=["t_accn"])
                                P.op("dve", lambda e, b=b, r=r, qs=qs, adv=adv: e.tensor_copy(adv[:, r, qs], ps_d[b][:]), r=["t_psd%d" % b], w=["t_accd"])
                            else:
                                P.op("dve", lambda e, b=b, r=r, qs=qs, anv=anv: e.tensor_tensor(anv[:, r, qs], anv[:, r, qs], ps_o[b][:], ALU.add),
                                     r=["t_pso%d" % b, "t_accn"], w=["t_accn"])
                                P.op("dve", lambda e, b=b, r=r, qs=qs, adv=adv: e.tensor_tensor(adv[:, r, qs], adv[:, r, qs], ps_d[b][:], ALU.add),
                                     r=["t_psd%d" % b, "t_accd"], w=["t_accd"])
                P.op("dve", lambda e: e.reciprocal(acc_d[:], acc_d[:]), r=["t_accd"], w=["t_accd"])
                P.op("dve", lambda e: e.tensor_tensor(ob[:], acc_n[:], acc_d[:], ALU.mult), r=["t_accn", "t_accd"], w=["t_ob"])
                P.dma("sp", self.attT[slot * 128:(slot + 1) * 128, :], ob[:], r=["t_ob"], w=["attT"])
            P.barrier()

    def stage_s5(self, l):
        P, S = self.P, self.S
        Lc = 512
        nch = S // Lc
        GC = 1.5957691216057308
        with contextlib.ExitStack() as st:
            sb = lambda n, shp, dt: self.sb(st, n, shp, dt)
            ioti = sb("s_ioti", [128, Lc + 1], I32)
            iot = sb("s_iot", [128, Lc + 1], F32)
            P.op("pool", lambda e: e.iota(ioti[:], pattern=[[1, Lc + 1]], base=0, channel_multiplier=0), w=["s_ioti"])
            P.op("dve", lambda e: e.tensor_copy(iot[:], ioti[:]), r=["s_ioti"], w=["s_iot"])
            prm = {}
            pti = sb("s_pti", [128, 32], I32)
            ptf = sb("s_ptf", [128, 32], F32)
            for dr in range(2):
                names = ["are", "aim", "ldt", "dt", "ar", "th", "rho", "sn", "cs", "lbr", "lbi", "den", "nr", "cr", "ci", "nci", "t"]
                T = {n: sb("s_%s%d" % (n, dr), [128, 32], F32) for n in names}
                K = {n: "s_%s%d" % (n, dr) for n in names}
                P.dma("sp", T["are"][:], self.s5_are[l, dr], w=[K["are"]])
                P.dma("sp", T["aim"][:], self.s5_aim[l, dr], w=[K["aim"]])
                P.dma("sp", T["ldt"][:], self.s5_ldt[l, dr], w=[K["ldt"]])
                P.op("act", lambda e, T=T: e.activation(out=T["dt"][:], in_=T["ldt"][:], func=AF.Exp), r=[K["ldt"]], w=[K["dt"]])
                tt = lambda o, a, b, op, T=T, K=K: P.op("dve", lambda e: e.tensor_tensor(T[o][:], T[a][:], T[b][:], op), r=[K[a], K[b]], w=[K[o]])
                tt("ar", "are", "dt", ALU.mult)
                tt("th", "aim", "dt", ALU.mult)
                P.op("act", lambda e, T=T: e.activation(out=T["rho"][:], in_=T["ar"][:], func=AF.Exp), r=[K["ar"]], w=[K["rho"]])
                self.sin_reduced(T["th"], K["th"], T["sn"], K["sn"], pti, "s_pti", ptf, "s_ptf", 0.0)
                self.sin_reduced(T["th"], K["th"], T["cs"], K["cs"], pti, "s_pti", ptf, "s_ptf", math.pi / 2)
                tt("lbr", "rho", "cs", ALU.mult)
                tt("lbi", "rho", "sn", ALU.mult)
                tt("t", "are", "are", ALU.mult)
                tt("den", "aim", "aim", ALU.mult)
                tt("den", "den", "t", ALU.add)
                P.op("dve", lambda e, T=T: e.reciprocal(T["den"][:], T["den"][:]), r=[K["den"]], w=[K["den"]])
                P.op("dve", lambda e, T=T: e.tensor_scalar(T["nr"][:], T["lbr"][:], -1.0, None, ALU.add), r=[K["lbr"]], w=[K["nr"]])
                tt("cr", "nr", "are", ALU.mult)
                tt("t", "lbi", "aim", ALU.mult)
                tt("cr", "cr", "t", ALU.add)
                tt("cr", "cr", "den", ALU.mult)
                tt("ci", "lbi", "are", ALU.mult)
                tt("t", "nr", "aim", ALU.mult)
                tt("ci", "ci", "t", ALU.subtract)
                tt("ci", "ci", "den", ALU.mult)
                P.op("dve", lambda e, T=T: e.tensor_scalar(T["nci"][:], T["ci"][:], -1.0, None, ALU.mult), r=[K["ci"]], w=[K["nci"]])
                prm[dr] = (T, K)
            uP = sb("s_uP", [32, S], BF16)
            yacc = sb("s_yacc", [32, S], F32)
            dcol = sb("s_dcol", [32, 1], F32)
            Bre = sb("s_Bre", [32, 128], BF16)
            Bim = sb("s_Bim", [32, 128], BF16)
            Cre = sb("s_Cre", [128, 32], F32)
            Cim = sb("s_Cim", [128, 32], F32)
            Ct = sb("s_Ct", [128, 32], F32)
            Cpr = sb("s_Cpr", [128, 32], BF16)
            Cni = sb("s_Cni", [128, 32], BF16)
            ang = sb("s_ang", [128, Lc + 1], F32)
            tsn = sb("s_tsn", [128, Lc + 1], F32)
            tcs = sb("s_tcs", [128, Lc + 1], F32)
            tti = sb("s_tti", [128, Lc + 1], I32)
            ttf = sb("s_ttf", [128, Lc + 1], F32)
            rhob = sb("s_rhob", [128, Lc], F32)
            nsnt = sb("s_nsn", [128, Lc], F32)
            q1 = [sb("s_q1%d" % i, [128, Lc], F32) for i in range(2)]
            q2 = [sb("s_q2%d" % i, [128, Lc], F32) for i in range(2)]
            q3 = [sb("s_q3%d" % i, [128, Lc], F32) for i in range(2)]
            q4 = [sb("s_q4%d" % i, [128, Lc], F32) for i in range(2)]
            ytmp = sb("s_ytmp", [32, Lc], F32)
            W = [sb("s_W%d" % i, [128, 2, Lc], F32) for i in range(2)]
            sgn = sb("s_sgn", [128, 2], F32)
            it2 = sb("s_it2", [128, 2], F32)
            m1 = [sb("s_m1%d" % i, [128, Lc], BF16) for i in range(2)]
            m2 = [sb("s_m2%d" % i, [128, Lc], BF16) for i in range(2)]
            m3 = [sb("s_m3%d" % i, [128, Lc], BF16) for i in range(2)]
            m4 = [sb("s_m4%d" % i, [128, Lc], BF16) for i in range(2)]
            ini = [sb("s_ini%d" % i, [128, 2], F32) for i in range(2)]
            it_ = sb("s_it", [128, 1], F32)
            GW = min(S, 2048)
            g1 = sb("s_g1", [32, GW], F32)
            g2 = sb("s_g2", [32, GW], F32)
            yo = sb("s_yo", [32, GW], BF16)
            ps_br = [self.ps(st, "s_psbr%d" % i, [128, Lc], F32) for i in range(2)]
            ps_bi = [self.ps(st, "s_psbi%d" % i, [128, Lc], F32) for i in range(2)]
            ps_btr = self.ps(st, "s_psbtr", [128, Lc], F32)
            ps_bti = self.ps(st, "s_psbti", [128, Lc], F32)
            ps_y = self.ps(st, "s_psy", [32, Lc], F32)
            cnt = 0
            import os
            for gp in range(1 if 's5one' in os.environ.get('DBG', '') else 32):
                P.dma("sp", uP[:], self.uT[gp * 32:(gp + 1) * 32, :], r=["uT"], w=["s_uP"])
                P.dma("sp", dcol[:], self.s5_d[l, gp], w=["s_dcol"])
                for dr in range(2):
                    T, K = prm[dr]
                    col = lambda n, T=T, gp=gp: T[n][:, gp:gp + 1]
                    P.dma("pool", Bre[:], self.s5_bre[l, dr, gp], w=["s_Bre"])
                    P.dma("pool", Bim[:], self.s5_bim[l, dr, gp], w=["s_Bim"])
                    P.dma("sp", Cre[:], self.s5_cre[l, dr, gp], w=["s_Cre"])
                    P.dma("sp", Cim[:], self.s5_cim[l, dr, gp], w=["s_Cim"])
                    P.op("dve", lambda e, col=col: e.tensor_scalar(Ct[:], Cim[:], col("ci"), None, ALU.mult), r=["s_Cim", K["ci"]], w=["s_Ct"])
                    P.op("dve", lambda e, col=col: e.scalar_tensor_tensor(Cpr[:], Cre[:], col("cr"), Ct[:], ALU.mult, ALU.subtract),
                         r=["s_Cre", "s_Ct", K["cr"]], w=["s_Cpr"])
                    P.op("dve", lambda e, col=col: e.tensor_scalar(Ct[:], Cim[:], col("cr"), None, ALU.mult), r=["s_Cim", K["cr"]], w=["s_Ct"])
                    P.op("dve", lambda e, col=col: e.scalar_tensor_tensor(Cni[:], Cre[:], col("nci"), Ct[:], ALU.mult, ALU.subtract),
                         r=["s_Cre", "s_Ct", K["nci"]], w=["s_Cni"])
                    P.op("dve", lambda e, col=col: e.tensor_scalar(ang[:], iot[:], col("th"), None, ALU.mult), r=["s_iot", K["th"]], w=["s_ang"])
                    self.sin_reduced(ang, "s_ang", tsn, "s_tsn", tti, "s_tti", ttf, "s_ttf", 0.0)
                    self.sin_reduced(ang, "s_ang", tcs, "s_tcs", tti, "s_tti", ttf, "s_ttf", math.pi / 2)
                    P.op("dve", lambda e, col=col: e.tensor_scalar(rhob[:], iot[:, 0:Lc], 0.0, col("rho"), ALU.mult, ALU.add),
                         r=["s_iot", K["rho"]], w=["s_rhob"])
                    P.op("dve", lambda e: e.tensor_scalar(nsnt[:], tsn[:, 0:Lc], -1.0, None, ALU.mult), r=["s_tsn"], w=["s_nsn"])
                    P.op("dve", lambda e: e.tensor_scalar(sgn[:, 0:1], tsn[:, Lc:Lc + 1], -1.0, None, ALU.mult), r=["s_tsn"], w=["s_sgn"])
                    P.op("dve", lambda e: e.tensor_copy(sgn[:, 1:2], tsn[:, Lc:Lc + 1]), r=["s_tsn"], w=["s_sgn"])
                    sn, cs = tsn[:, 0:Lc], tcs[:, 0:Lc]
                    snL, csL = tsn[:, Lc:Lc + 1], tcs[:, Lc:Lc + 1]
                    prev = None
                    rv = (lambda ap: ap) if dr == 0 else (lambda ap: ap[:, ::-1])
                    bufs = []
                    for ci_ in range(nch):
                        bufs.append(cnt % 2)
                        cnt += 1

                    nsn = nsnt[:, 0:Lc]

                    def front(ci_, dr=dr, rv=rv, cs=cs, sn=sn, nsn=nsn, bufs=bufs):
                        c = ci_ if dr == 0 else nch - 1 - ci_
                        b = bufs[ci_]
                        csl = slice(c * Lc, (c + 1) * Lc)
                        kb = lambda n, b=b: "s_%s%d" % (n, b)
                        P.op("pe", lambda e, b=b, csl=csl: e.matmul(ps_br[b][:], Bre[:], uP[:, csl], start=True, stop=True), r=["s_Bre", "s_uP"], w=[kb("psbr")])
                        P.op("pe", lambda e, b=b, csl=csl: e.matmul(ps_bi[b][:], Bim[:], uP[:, csl], start=True, stop=True), r=["s_Bim", "s_uP"], w=[kb("psbi")])
                        P.op("dve", lambda e, b=b, rv=rv, cs=cs: e.tensor_tensor(q1[b][:], rv(ps_br[b][:]), cs, ALU.mult), r=[kb("psbr"), "s_tcs"], w=[kb("q1")])
                        P.op("dve", lambda e, b=b, rv=rv, sn=sn: e.tensor_tensor(q2[b][:], rv(ps_bi[b][:]), sn, ALU.mult), r=[kb("psbi"), "s_tsn"], w=[kb("q2")])
                        P.op("dve", lambda e, b=b, rv=rv, cs=cs: e.tensor_tensor(q3[b][:], rv(ps_bi[b][:]), cs, ALU.mult), r=[kb("psbi"), "s_tcs"], w=[kb("q3")])
                        P.op("dve", lambda e, b=b, rv=rv, nsn=nsn: e.tensor_tensor(q4[b][:], rv(ps_br[b][:]), nsn, ALU.mult), r=[kb("psbr"), "s_nsn"], w=[kb("q4")])

                    def back(ci_, prev, dr=dr, cs=cs, sn=sn, nsn=nsn, snL=snL, csL=csL, bufs=bufs):
                        c = ci_ if dr == 0 else nch - 1 - ci_
                        b = bufs[ci_]
                        csl = slice(c * Lc, (c + 1) * Lc)
                        kb = lambda n, b=b: "s_%s%d" % (n, b)
                        P.op("pe", lambda e, b=b: e.matmul(ps_btr[:], self.ident_f[:], q1[b][:], start=True, stop=False), r=["ident_f", kb("q1")], w=["s_psbtr"])
                        P.op("pe", lambda e, b=b: e.matmul(ps_btr[:], self.ident_f[:], q2[b][:], start=False, stop=True), r=["ident_f", kb("q2")], w=["s_psbtr"])
                        P.op("pe", lambda e, b=b: e.matmul(ps_bti[:], self.ident_f[:], q3[b][:], start=True, stop=False), r=["ident_f", kb("q3")], w=["s_psbti"])
                        P.op("pe", lambda e, b=b: e.matmul(ps_bti[:], self.ident_f[:], q4[b][:], start=False, stop=True), r=["ident_f", kb("q4")], w=["s_psbti"])
                        if prev is None:
                            P.op("dve", lambda e, b=b: e.memset(ini[b][:], 0.0), w=[kb("ini")])
                        else:
                            pb = prev
                            wend = W[pb][:, :, Lc - 1]
                            wsw = W[pb][:, ::-1, Lc - 1]
                            P.op("dve", lambda e, wsw=wsw: e.tensor_tensor(it2[:], wsw, sgn[:], ALU.mult), r=["s_wr%d" % pb, "s_wi%d" % pb, "s_sgn"], w=["s_it2"])
                            P.op("dve", lambda e, b=b, wend=wend, csL=csL: e.scalar_tensor_tensor(ini[b][:], wend, csL, it2[:], ALU.mult, ALU.add),
                                 r=["s_wr%d" % pb, "s_wi%d" % pb, "s_tcs", "s_it2"], w=[kb("ini")])
                        P.op("dve", lambda e, b=b: e.tensor_tensor_scan(W[b][:, 0, :], rhob[:], ps_btr[:], ini[b][:, 0:1], ALU.mult, ALU.add),
                             r=["s_rhob", "s_psbtr", kb("ini")], w=[kb("wr")])
                        P.op("dve", lambda e, b=b: e.tensor_tensor_scan(W[b][:, 1, :], rhob[:], ps_bti[:], ini[b][:, 1:2], ALU.mult, ALU.add),
                             r=["s_rhob", "s_psbti", kb("ini")], w=[kb("wi")])
                        P.op("dve", lambda e, b=b, cs=cs: e.tensor_tensor(m1[b][:], W[b][:, 0, :], cs, ALU.mult), r=[kb("wr"), "s_tcs"], w=[kb("m1")])
                        P.op("dve", lambda e, b=b, nsn=nsn: e.tensor_tensor(m2[b][:], W[b][:, 1, :], nsn, ALU.mult), r=[kb("wi"), "s_nsn"], w=[kb("m2")])
                        P.op("dve", lambda e, b=b, sn=sn: e.tensor_tensor(m3[b][:], W[b][:, 0, :], sn, ALU.mult), r=[kb("wr"), "s_tsn"], w=[kb("m3")])
                        P.op("dve", lambda e, b=b, cs=cs: e.tensor_tensor(m4[b][:], W[b][:, 1, :], cs, ALU.mult), r=[kb("wi"), "s_tcs"], w=[kb("m4")])
                        P.op("pe", lambda e, b=b: e.matmul(ps_y[:], Cpr[:], m1[b][:], start=True, stop=False), r=["s_Cpr", kb("m1")], w=["s_psy"])
                        P.op("pe", lambda e, b=b: e.matmul(ps_y[:], Cpr[:], m2[b][:], start=False, stop=False), r=["s_Cpr", kb("m2")], w=["s_psy"])
                        P.op("pe", lambda e, b=b: e.matmul(ps_y[:], Cni[:], m3[b][:], start=False, stop=False), r=["s_Cni", kb("m3")], w=["s_psy"])
                        P.op("pe", lambda e, b=b: e.matmul(ps_y[:], Cni[:], m4[b][:], start=False, stop=True), r=["s_Cni", kb("m4")], w=["s_psy"])
                        if dr == 0:
                            P.op("act", lambda e, csl=csl: e.copy(yacc[:, csl], ps_y[:]), r=["s_psy"], w=["s_yacc"])
                        else:
                            P.op("act", lambda e: e.copy(ytmp[:], ps_y[:]), r=["s_psy"], w=["s_ytmp"])
                            P.op("dve", lambda e, csl=csl: e.tensor_tensor(yacc[:, csl][:, ::-1], yacc[:, csl][:, ::-1], ytmp[:], ALU.add),
                                 r=["s_ytmp", "s_yacc"], w=["s_yacc"])
                        return b

                    front(0)
                    prev = None
                    for ci_ in range(nch):
                        if ci_ + 1 < nch:
                            front(ci_ + 1)
                        prev = back(ci_, prev)
                for gc in range(S // GW):
                    gsl = slice(gc * GW, (gc + 1) * GW)
                    P.op("dve", lambda e, gsl=gsl: e.scalar_tensor_tensor(g1[:], uP[:, gsl], dcol[:, 0:1], yacc[:, gsl], ALU.mult, ALU.add), r=["s_uP", "s_dcol", "s_yacc"], w=["s_g1"])
                    if self.stop == "s5raw":
                        P.dma("sp", self.dbg_y[gp * 32:(gp + 1) * 32, gsl], g1[:], r=["s_g1"], w=["dbg_y"])
                    P.op("pool", lambda e: e.tensor_tensor(g2[:], g1[:], g1[:], ALU.mult), r=["s_g1"], w=["s_g2"])
                    P.op("pool", lambda e: e.tensor_scalar(g2[:], g2[:], 0.044715, 1.0, ALU.mult, ALU.add), r=["s_g2"], w=["s_g2"])
                    P.op("pool", lambda e: e.tensor_tensor(g2[:], g2[:], g1[:], ALU.mult), r=["s_g2", "s_g1"], w=["s_g2"])
                    P.op("act", lambda e: e.activation(out=g2[:], in_=g2[:], func=AF.Sigmoid, scale=GC), r=["s_g2"], w=["s_g2"])
                    P.op("pool", lambda e: e.tensor_tensor(yo[:], g2[:], g1[:], ALU.mult), r=["s_g2", "s_g1"], w=["s_yo"])
                    P.dma("sp", self.ygT[gp * 32:(gp + 1) * 32, gsl], yo[:], r=["s_yo"], w=["ygT"])
            P.barrier()

    def stage_c1(self, l):
        P, S, TS = self.P, self.S, self.TS
        nsub = TS // 512
        for sup in range(S // TS):
            T0 = sup * TS
            with contextlib.ExitStack() as st:
                sb = lambda n, shp, dt: self.sb(st, n, shp, dt)
                aT = sb("c_aT", [128, 4, TS], BF16)
                yT = sb("c_yT", [128, 8, TS], BF16)
                P.dma("sp", aT[:], self.attT[:, T0:T0 + TS].rearrange("(kc p) t -> p kc t", p=128), r=["attT"], w=["c_aT"])
                P.dma("sp", yT[:], self.ygT[:, T0:T0 + TS].rearrange("(kc p) t -> p kc t", p=128), r=["ygT"], w=["c_yT"])
                wa = [sb("c_wa%d" % i, [128, 4, 128], BF16) for i in range(2)]
                wv = [sb("c_wv%d" % i, [128, 8, 128], BF16) for i in range(2)]
                wg = [sb("c_wg%d" % i, [128, 8, 128], BF16) for i in range(2)]
                ga = [sb("c_ga%d" % i, [128, TS], BF16) for i in range(2)]
                gs = [sb("c_gs%d" % i, [128, TS], BF16) for i in range(2)]
                mo = [sb("c_mo%d" % i, [128, TS], BF16) for i in range(2)]
                sg = sb("c_sg", [128, 512], F32)
                sbr = sb("c_sbr", [128, 512], F32)
                ta = sb("c_ta", [128, 512], F32)
                psA = [self.ps(st, "c_psA%d" % i, [128, 512], F32) for i in range(2)]
                psV = [self.ps(st, "c_psV%d" % i, [128, 512], F32) for i in range(2)]
                psG = [self.ps(st, "c_psG%d" % i, [128, 512], F32) for i in range(2)]
                cnt = 0
                for m in range(16):
                    wb_ = m % 2
                    cs_ = slice(m * 128, (m + 1) * 128)
                    P.dma("pool", wa[wb_][:], self.w_attn_br[l, :, cs_].rearrange("(kc p) n -> p kc n", p=128), w=["c_wa%d" % wb_])
                    P.dma("pool", wv[wb_][:], self.w_ssm_br[l, :, cs_].rearrange("(kc p) n -> p kc n", p=128), w=["c_wv%d" % wb_])
                    P.dma("pool", wg[wb_][:], self.w_ssm_br[l, :, 2048 + m * 128:2048 + (m + 1) * 128].rearrange("(kc p) n -> p kc n", p=128), w=["c_wg%d" % wb_])
                    P.dma("sp", ga[wb_][:], self.gT[m * 128:(m + 1) * 128, T0:T0 + TS], r=["gT"], w=["c_ga%d" % wb_])
                    P.dma("sp", gs[wb_][:], self.gT[2048 + m * 128:2048 + (m + 1) * 128, T0:T0 + TS], r=["gT"], w=["c_gs%d" % wb_])
                    for sub in range(nsub):
                        b = cnt % 2
                        cnt += 1
                        tsl = slice(sub * 512, (sub + 1) * 512)
                        for kc in range(4):
                            P.op("pe", lambda e, b=b, wb_=wb_, kc=kc, tsl=tsl: e.matmul(psA[b][:], wa[wb_][:, kc, :], aT[:, kc, tsl], start=(kc == 0), stop=(kc == 3)),
                                 r=["c_wa%d" % wb_, "c_aT"], w=["c_psA%d" % b])
                        for kc in range(8):
                            P.op("pe", lambda e, b=b, wb_=wb_, kc=kc, tsl=tsl: e.matmul(psV[b][:], wv[wb_][:, kc, :], yT[:, kc, tsl], start=(kc == 0), stop=(kc == 7)),
                                 r=["c_wv%d" % wb_, "c_yT"], w=["c_psV%d" % b])
                        for kc in range(8):
                            P.op("pe", lambda e, b=b, wb_=wb_, kc=kc, tsl=tsl: e.matmul(psG[b][:], wg[wb_][:, kc, :], yT[:, kc, tsl], start=(kc == 0), stop=(kc == 7)),
                                 r=["c_wg%d" % wb_, "c_yT"], w=["c_psG%d" % b])
                        P.op("act", lambda e, b=b: e.activation(out=sg[:], in_=psG[b][:], func=AF.Sigmoid), r=["c_psG%d" % b], w=["c_sg"])
                        P.op("dve", lambda e, b=b: e.tensor_tensor(sbr[:], psV[b][:], sg[:], ALU.mult), r=["c_psV%d" % b, "c_sg"], w=["c_sbr"])
                        P.op("dve", lambda e, wb_=wb_, tsl=tsl: e.tensor_tensor(sbr[:], sbr[:], gs[wb_][:, tsl], ALU.mult), r=["c_sbr", "c_gs%d" % wb_], w=["c_sbr"])
                        P.op("dve", lambda e, b=b, wb_=wb_, tsl=tsl: e.tensor_tensor(ta[:], psA[b][:], ga[wb_][:, tsl], ALU.mult), r=["c_psA%d" % b, "c_ga%d" % wb_], w=["c_ta"])
                        P.op("dve", lambda e, wb_=wb_, tsl=tsl: e.tensor_tensor(mo[wb_][:, tsl], ta[:], sbr[:], ALU.add), r=["c_ta", "c_sbr"], w=["c_mo%d" % wb_])
                    P.dma("sp", self.mergedT[m * 128:(m + 1) * 128, T0:T0 + TS], mo[wb_][:], r=["c_mo%d" % wb_], w=["mergedT"])
                P.barrier()

    def stage_c2(self, l):
        P, S = self.P, self.S
        with contextlib.ExitStack() as st:
            sb = lambda n, shp, dt: self.sb(st, n, shp, dt)
            W = sb("o_W", [128, 16, 2048], BF16)
            for c in range(8):
                P.dma("pool", W[:, :, c * 256:(c + 1) * 256], self.w_out[l, :, c * 256:(c + 1) * 256].rearrange("(kc p) n -> p kc n", p=128), w=[("o_W", c)])
            wkeys = [("o_W", c) for c in range(8)]
            mT = [sb("o_mT%d" % i, [128, 16, 128], BF16) for i in range(2)]
            hb = [sb("o_hb%d" % i, [128, 2048], F32) for i in range(2)]
            psm = [self.ps(st, "o_ps%d" % i, [128, 512], F32) for i in range(4)]
            for tt in range(S // 128):
                b = tt % 2
                t0 = tt * 128
                P.dma("sp", mT[b][:], self.mergedT[:, t0:t0 + 128].rearrange("(kc p) t -> p kc t", p=128), r=["mergedT"], w=["o_mT%d" % b])
                P.dma("sp", hb[b][:], self.h[t0:t0 + 128, :], r=["h"], w=["o_hb%d" % b])
                for c4 in range(4):
                    for kc in range(16):
                        P.op("pe", lambda e, b=b, c4=c4, kc=kc: e.matmul(psm[c4][:], mT[b][:, kc, :], W[:, kc, c4 * 512:(c4 + 1) * 512], start=(kc == 0), stop=(kc == 15)),
                             r=["o_mT%d" % b] + wkeys[c4 * 2:c4 * 2 + 2], w=["o_ps%d" % c4])
                    P.op("dve", lambda e, b=b, c4=c4: e.tensor_tensor(hb[b][:, c4 * 512:(c4 + 1) * 512], hb[b][:, c4 * 512:(c4 + 1) * 512], psm[c4][:], ALU.add),
                         r=["o_ps%d" % c4, "o_hb%d" % b], w=["o_hb%d" % b])
                P.dma("sp", self.h[t0:t0 + 128, :], hb[b][:], r=["o_hb%d" % b], w=["h"])
            P.barrier()

    def norm_tile(self, hb, hbk, gb, gbk, xn, xnk, ss, ssk, rs, rsk, junk, junkk):
        P = self.P
        P.op("act", lambda e: e.activation(out=junk[:], in_=hb[:], func=AF.Square, accum_out=ss[:, 0:1]), r=[hbk], w=[junkk, ssk])
        P.op("act", lambda e: e.activation(out=rs[:], in_=ss[:], func=AF.Sqrt, scale=1.0 / D, bias=self.epsb[:, 0:1]), r=[ssk, "epsb"], w=[rsk])
        P.op("dve", lambda e: e.reciprocal(rs[:], rs[:]), r=[rsk], w=[rsk])
        P.op("dve", lambda e: e.scalar_tensor_tensor(xn, hb[:], rs[:, 0:1], gb[:], ALU.mult, ALU.mult), r=[hbk, rsk, gbk], w=[xnk])

    def transpose_tile(self, xn, xnk, pT, pTk, dst_fn, dkey, cnt0=0):
        P = self.P
        for g4 in range(4):
            pb = (cnt0 + g4) % 2
            for j in range(4):
                kc = g4 * 4 + j
                P.op("pe", lambda e, pb=pb, j=j, kc=kc: e.transpose(pT[pb][:, j, :], xn[:, kc * 128:(kc + 1) * 128], self.ident_b[:]),
                     r=[xnk, "ident_b"], w=[pTk % pb])
            if g4 % 2 == 0:
                P.op("act", lambda e, pb=pb, g4=g4: e.copy(dst_fn(g4), pT[pb][:]), r=[pTk % pb], w=[dkey])
            else:
                P.op("dve", lambda e, pb=pb, g4=g4: e.tensor_copy(dst_fn(g4), pT[pb][:]), r=[pTk % pb], w=[dkey])

    def stage_moe(self, l):
        P, S = self.P, self.S
        NT = S // 128
        C = S // 8
        NCT = C // 128
        RW = 2112
        BIG = float(1 << 20)
        NIT = 34
        with contextlib.ExitStack() as st0:
            aff = self.sb(st0, "m_aff", [128, NT, 16], F32)
            posi = self.p_posi
            with contextlib.ExitStack() as st:
                sb = lambda n, shp, dt: self.sb(st, n, shp, dt)
                gb = sb("m_gb", [128, D], F32)
                P.dma("sp", gb[:], self.norm_ffn[l:l + 1, :].partition_broadcast(128), w=["m_gb"])
                wr = sb("m_wr", [128, 16, 16], BF16)
                P.dma("pool", wr[:], self.w_router[l].rearrange("(kc p) n -> p kc n", p=128), w=["m_wr"])
                hb = [sb("m_hb%d" % i, [128, D], F32) for i in range(2)]
                junk = sb("m_junk", [128, D], BF16)
                ss = [sb("m_ss%d" % i, [128, 1], F32) for i in range(2)]
                rs = [sb("m_rs%d" % i, [128, 1], F32) for i in range(2)]
                xrow = [sb("m_xrow%d" % i, [128, RW], BF16) for i in range(2)]
                xT = [sb("m_xT%d" % i, [128, 16, 128], BF16) for i in range(2)]
                ex = sb("m_ex", [128, 16], F32)
                sm = sb("m_sm", [128, 1], F32)
                pT = [self.ps(st, "m_pT%d" % i, [128, 4, 128], BF16) for i in range(2)]
                psl = [self.ps(st, "m_psl%d" % i, [128, 16], F32) for i in range(2)]
                for tt in range(NT):
                    b = tt % 2
                    t0 = tt * 128
                    k = lambda n, b=b: "m_%s%d" % (n, b)
                    P.dma("sp", hb[b][:], self.h[t0:t0 + 128, :], r=["h"], w=[k("hb")])
                    self.norm_tile(hb[b], k("hb"), gb, "m_gb", xrow[b][:, 0:D], k("xrow"), ss[b], k("ss"), rs[b], k("rs"), junk, "m_junk")
                    self.transpose_tile(xrow[b][:, 0:D], k("xrow"), pT, "m_pT%d", (lambda g4, b=b: xT[b][:, g4 * 4:(g4 + 1) * 4, :]), k("xT"), cnt0=tt * 4)
                    for kc in range(16):
                        P.op("pe", lambda e, b=b, kc=kc: e.matmul(psl[b][:], xT[b][:, kc, :], wr[:, kc, :], start=(kc == 0), stop=(kc == 15)),
                             r=[k("xT"), "m_wr"], w=[k("psl")])
                    P.op("act", lambda e, b=b: e.activation(out=ex[:], in_=psl[b][:], func=AF.Exp, accum_out=sm[:, 0:1]), r=[k("psl")], w=["m_ex", "m_sm"])
                    P.op("dve", lambda e: e.reciprocal(sm[:], sm[:]), r=["m_sm"], w=["m_sm"])
                    P.op("dve", lambda e, tt=tt: e.tensor_scalar(aff[:, tt, :], ex[:], sm[:, 0:1], None, ALU.mult), r=["m_ex", "m_sm"], w=["m_aff"])
                    P.op("dve", lambda e, b=b, tt=tt: e.tensor_copy(xrow[b][:, D:D + 32].bitcast(F32), aff[:, tt, :]), r=["m_aff"], w=[k("xrow")])
                    P.op("pool", lambda e, b=b, t0=t0: e.iota(xrow[b][:, D + 32:D + 34].bitcast(I32), pattern=[[0, 1]], base=t0, channel_multiplier=1), w=[k("xrow")])
                    P.dma("sp", self.xrows[t0:t0 + 128, :], xrow[b][:], r=[k("xrow")], w=["xrows"])
                P.barrier()
            with contextlib.ExitStack() as st:
                sb = lambda n, shp, dt: self.sb(st, n, shp, dt)
                ltri = sb("m_ltri", [128, 128], BF16)
                P.dma("pool", ltri[:], self.c_ltri, w=["m_ltri"])
                lo = sb("m_lo", [128, 16], F32)
                hi = sb("m_hi", [128, 16], F32)
                mid = sb("m_mid", [128, 16], F32)
                cmpt = sb("m_cmp", [128, NT, 16], F32)
                cntp = sb("m_cntp", [128, 16], BF16)
                cntpf = sb("m_cntpf", [128, 16], F32)
                gei = sb("m_gei", [128, 16], I32)
                lti = sb("m_lti", [128, 16], I32)
                pst = self.ps(st, "m_pst", [128, 16], F32)
                P.op("dve", lambda e: e.memset(lo[:], 0.0), w=["m_lo"])
                P.op("dve", lambda e: e.memset(hi[:], 1.0), w=["m_hi"])
                affv = aff[:].rearrange("p t e -> p e t")
                cmpv = cmpt[:].rearrange("p t e -> p e t")

                def count_ge(thr, thrk):
                    P.op("dve", lambda e: e.tensor_tensor(cmpt[:], aff[:], thr[:].unsqueeze(1).to_broadcast([128, NT, 16]), ALU.is_ge),
                         r=["m_aff", thrk], w=["m_cmp"])
                    P.op("dve", lambda e: e.tensor_reduce(cntpf[:], cmpv, AX.X, ALU.add), r=["m_cmp"], w=["m_cntpf"])
                    P.op("dve", lambda e: e.tensor_copy(cntp[:], cntpf[:]), r=["m_cntpf"], w=["m_cntp"])
                for it in range(NIT):
                    P.op("dve", lambda e: e.tensor_tensor(mid[:], lo[:], hi[:], ALU.add), r=["m_lo", "m_hi"], w=["m_mid"])
                    P.op("dve", lambda e: e.tensor_scalar(mid[:], mid[:], 0.5, None, ALU.mult), r=["m_mid"], w=["m_mid"])
                    count_ge(mid, "m_mid")
                    P.op("pe", lambda e: e.matmul(pst[:], self.ones_b[:], cntp[:], start=True, stop=True), r=["ones_b", "m_cntp"], w=["m_pst"])
                    P.op("dve", lambda e: e.tensor_scalar(gei[:], pst[:], float(C), None, ALU.is_ge), r=["m_pst"], w=["m_gei"])
                    P.op("dve", lambda e: e.tensor_scalar(lti[:], pst[:], float(C), None, ALU.is_lt), r=["m_pst"], w=["m_lti"])
                    P.op("dve", lambda e: e.copy_predicated(lo[:], gei[:], mid[:]), r=["m_gei", "m_mid", "m_lo"], w=["m_lo"])
                    P.op("dve", lambda e: e.copy_predicated(hi[:], lti[:], mid[:]), r=["m_lti", "m_mid", "m_hi"], w=["m_hi"])
                count_ge(lo, "m_lo")
                P.op("pe", lambda e: e.matmul(pst[:], ltri[:], cntp[:], start=True, stop=True), r=["m_ltri", "m_cntp"], w=["m_pst"])
                offs = sb("m_offs", [128, 16], F32)
                P.op("act", lambda e: e.copy(offs[:], pst[:]), r=["m_pst"], w=["m_offs"])
                cum = sb("m_cum", [128, NT, 16], F32)
                cumv = cum[:].rearrange("p t e -> p e t")
                onesr = sb("m_onesr", [128, NT], F32)
                P.op("dve", lambda e: e.memset(onesr[:], 1.0), w=["m_onesr"])
                for ex_ in range(16):
                    P.op("dve", lambda e, ex_=ex_: e.tensor_tensor_scan(cumv[:, ex_, :], onesr[:], cmpv[:, ex_, :], 0.0, ALU.mult, ALU.add),
                         r=["m_cmp", "m_onesr"], w=["m_cum"])
                P.op("dve", lambda e: e.tensor_tensor(cum[:], cum[:], cmpt[:], ALU.subtract), r=["m_cum", "m_cmp"], w=["m_cum"])
                P.op("dve", lambda e: e.tensor_tensor(cum[:], cum[:], offs[:].unsqueeze(1).to_broadcast([128, NT, 16]), ALU.add), r=["m_cum", "m_offs"], w=["m_cum"])
                P.op("dve", lambda e: e.tensor_scalar(cum[:], cum[:], -BIG, None, ALU.add), r=["m_cum"], w=["m_cum"])
                P.op("dve", lambda e: e.tensor_tensor(cum[:], cum[:], cmpt[:], ALU.mult), r=["m_cum", "m_cmp"], w=["m_cum"])
                P.op("dve", lambda e: e.tensor_scalar(cum[:], cum[:], BIG, None, ALU.add), r=["m_cum"], w=["m_cum"])
                P.op("dve", lambda e: e.tensor_copy(posi[:], cum[:].rearrange("p t e -> p (t e)")), r=["m_cum"], w=["m_posi"])
                if self.dbg:
                    P.dma("sp", self.dbg_posi, posi[:], r=["m_posi"], w=["dbg_posi"])
                    P.dma("sp", self.dbg_aff, aff[:].rearrange("p t e -> p (t e)"), r=["m_aff"], w=["dbg_aff"])
                P.barrier()
            with contextlib.ExitStack() as st:
                xr = self.p_xr
                for tt in range(NT):
                    b = tt % 2
                    P.dma("sp", xr[b][:], self.xrows[tt * 128:(tt + 1) * 128, :], r=["xrows"], w=["m_dr%d" % b])
                    for ex_ in range(16):
                        col = tt * 16 + ex_
                        P.dma_fn("pool", lambda e, b=b, ex_=ex_, col=col: e.indirect_dma_start(
                            out=self.xg[ex_][:, :], out_offset=bass.IndirectOffsetOnAxis(ap=posi[:, col:col + 1], axis=0),
                            in_=xr[b][:], in_offset=None, bounds_check=self.reg(e, C - 1), oob_is_err=False),
                            r=["m_dr%d" % b, "m_posi"], w=[("xg", ex_)])
                P.barrier()
        with contextlib.ExitStack() as st:
            sb = lambda n, shp, dt: self.sb(st, n, shp, dt)
            xgT = sb("e_xgT", [128, 16, C], BF16)
            hidT = sb("e_hidT", [128, 8, C], BF16)
            Wd = sb("e_Wd", [128, 8, D], BF16)
            wgb = [sb("e_wg%d" % i, [128, 16, 128], BF16) for i in range(2)]
            wub = [sb("e_wu%d" % i, [128, 16, 128], BF16) for i in range(2)]
            xrw = [sb("e_xr%d" % i, [128, RW], BF16) for i in range(2)]
            gates = sb("e_gates", [128, NCT], F32)
            tid = self.p_tid
            sg = sb("e_sg", [128, 512], F32)
            yrow = self.p_yrow
            pT = [self.ps(st, "e_pT%d" % i, [128, 4, 128], BF16) for i in range(2)]
            psG = self.ps(st, "e_psG", [128, 512], F32)
            psU = self.ps(st, "e_psU", [128, 512], F32)
            psY = [self.ps(st, "e_psY%d" % i, [128, 512], F32) for i in range(2)]
            nsubc = max(1, C // 512)
            subw = min(C, 512)
            tcnt = 0
            ycnt = 0
            for ex_ in range(16):
                for c in range(8):
                    P.dma("pool", Wd[:, :, c * 256:(c + 1) * 256], self.w_exp_down[l, ex_, :, c * 256:(c + 1) * 256].rearrange("(kc p) n -> p kc n", p=128), w=[("e_Wd", c)])
                for ct in range(NCT):
                    b = ct % 2
                    P.dma("sp", xrw[b][:], self.xg[ex_][ct * 128:(ct + 1) * 128, :], r=[("xg", ex_)], w=["e_xr%d" % b])
                    self.transpose_tile(xrw[b][:, 0:D], "e_xr%d" % b, pT, "e_pT%d", (lambda g4, ct=ct: xgT[:, g4 * 4:(g4 + 1) * 4, ct * 128:(ct + 1) * 128]), ("e_xgT", ct), cnt0=tcnt)
                    tcnt += 4
                    P.op("dve", lambda e, b=b, ct=ct, ex_=ex_: e.tensor_copy(gates[:, ct:ct + 1], xrw[b][:, D + 2 * ex_:D + 2 * ex_ + 2].bitcast(F32)), r=["e_xr%d" % b], w=["e_gates"])
                    P.op("dve", lambda e, b=b, ct=ct: e.tensor_copy(tid[:, ct:ct + 1], xrw[b][:, D + 32:D + 34].bitcast(I32)), r=["e_xr%d" % b], w=["e_tid"])
                xkeys = [("e_xgT", ct) for ct in range(NCT)]
                for fb in range(8):
                    wb_ = fb % 2
                    P.dma("pool", wgb[wb_][:], self.w_exp_gate[l, ex_, :, fb * 128:(fb + 1) * 128].rearrange("(kc p) n -> p kc n", p=128), w=["e_wg%d" % wb_])
                    P.dma("pool", wub[wb_][:], self.w_exp_up[l, ex_, :, fb * 128:(fb + 1) * 128].rearrange("(kc p) n -> p kc n", p=128), w=["e_wu%d" % wb_])
                    for sub in range(nsubc):
                        tsl = slice(sub * subw, (sub + 1) * subw)
                        for kc in range(16):
                            P.op("pe", lambda e, wb_=wb_, kc=kc, tsl=tsl: e.matmul(psG[:, 0:subw], wgb[wb_][:, kc, :], xgT[:, kc, tsl], start=(kc == 0), stop=(kc == 15)),
                                 r=["e_wg%d" % wb_] + xkeys, w=["e_psG"])
                        for kc in range(16):
                            P.op("pe", lambda e, wb_=wb_, kc=kc, tsl=tsl: e.matmul(psU[:, 0:subw], wub[wb_][:, kc, :], xgT[:, kc, tsl], start=(kc == 0), stop=(kc == 15)),
                                 r=["e_wu%d" % wb_] + xkeys, w=["e_psU"])
                        P.op("act", lambda e: e.activation(out=sg[:, 0:subw], in_=psG[:, 0:subw], func=AF.Silu), r=["e_psG"], w=["e_sg"])
                        P.op("dve", lambda e, fb=fb, tsl=tsl: e.tensor_tensor(hidT[:, fb, tsl], sg[:, 0:subw], psU[:, 0:subw], ALU.mult), r=["e_sg", "e_psU"], w=[("e_hidT", fb)])
                hkeys = [("e_hidT", fb) for fb in range(8)]
                for ct in range(NCT):
                    yb = ycnt % 2
                    ycnt += 1
                    for c4 in range(4):
                        pb = c4 % 2
                        for fc in range(8):
                            P.op("pe", lambda e, pb=pb, fc=fc, ct=ct, c4=c4: e.matmul(psY[pb][:], hidT[:, fc, ct * 128:(ct + 1) * 128], Wd[:, fc, c4 * 512:(c4 + 1) * 512], start=(fc == 0), stop=(fc == 7)),
                                 r=hkeys + [("e_Wd", 2 * c4), ("e_Wd", 2 * c4 + 1)], w=["e_psY%d" % pb])
                        P.op("dve" if c4 % 2 == 0 else "act",
                             (lambda e, pb=pb, yb=yb, c4=c4, ct=ct: e.tensor_scalar(yrow[yb][:, c4 * 512:(c4 + 1) * 512], psY[pb][:], gates[:, ct:ct + 1], None, ALU.mult)) if c4 % 2 == 0 else
                             (lambda e, pb=pb, yb=yb, c4=c4, ct=ct: e.activation(out=yrow[yb][:, c4 * 512:(c4 + 1) * 512], in_=psY[pb][:], func=AF.Copy, scale=gates[:, ct:ct + 1])),
                             r=["e_psY%d" % pb, "e_gates"], w=["e_yrow%d" % yb])
                    P.dma_fn("pool", lambda e, yb=yb, ct=ct: e.indirect_dma_start(
                        out=self.h[:, :], out_offset=bass.IndirectOffsetOnAxis(ap=tid[:, ct:ct + 1], axis=0),
                        in_=yrow[yb][:], in_offset=None, bounds_check=self.reg(e, S - 1), oob_is_err=True, compute_op=ALU.add),
                        r=["e_yrow%d" % yb, "e_tid"], w=["h"])
            P.barrier()

    def stage_ple(self, l):
        P, S = self.P, self.S
        with contextlib.ExitStack() as st:
            sb = lambda n, shp, dt: self.sb(st, n, shp, dt)
            Wg = sb("l_Wg", [128, 16, D], BF16)
            for c in range(8):
                P.dma("pool", Wg[:, :, c * 256:(c + 1) * 256], self.w_ple_gate[l, :, c * 256:(c + 1) * 256].rearrange("(kc p) n -> p kc n", p=128), w=[("l_Wg", c)])
            Wp = sb("l_Wp", [128, 2, D], BF16)
            for c in range(2):
                P.dma("pool", Wp[:, :, c * 1024:(c + 1) * 1024], self.w_ple_proj[l, :, c * 1024:(c + 1) * 1024].rearrange("(kc p) n -> p kc n", p=128), w=[("l_Wp", c)])
            gb = sb("l_gb", [128, D], F32)
            P.dma("sp", gb[:], self.norm_ple[l:l + 1, :].partition_broadcast(128), w=["l_gb"])
            hb = [sb("l_hb%d" % i, [128, D], F32) for i in range(2)]
            junk = sb("l_junk", [128, D], BF16)
            ss = [sb("l_ss%d" % i, [128, 1], F32) for i in range(2)]
            rs = [sb("l_rs%d" % i, [128, 1], F32) for i in range(2)]
            xn = [sb("l_xn%d" % i, [128, D], BF16) for i in range(2)]
            hT = [sb("l_hT%d" % i, [128, 16, 128], BF16) for i in range(2)]
            pTt = [sb("l_pTt%d" % i, [128, 2, 128], BF16) for i in range(2)]
            sg = sb("l_sg", [128, 512], F32)
            pT = [self.ps(st, "l_pT%d" % i, [128, 4, 128], BF16) for i in range(2)]
            psG = [self.ps(st, "l_psG%d" % i, [128, 512], F32) for i in range(2)]
            psP = [self.ps(st, "l_psP%d" % i, [128, 512], F32) for i in range(2)]
            for tt in range(S // 128):
                b = tt % 2
                t0 = tt * 128
                k = lambda n, b=b: "l_%s%d" % (n, b)
                P.dma("sp", hb[b][:], self.h[t0:t0 + 128, :], r=["h"], w=[k("hb")])
                P.dma("pool", pTt[b][:], self.pT[l, :, t0:t0 + 128].rearrange("(kc p) t -> p kc t", p=128), w=[k("pTt")])
                self.norm_tile(hb[b], k("hb"), gb, "l_gb", xn[b][:], k("xn"), ss[b], k("ss"), rs[b], k("rs"), junk, "l_junk")
                self.transpose_tile(xn[b][:], k("xn"), pT, "l_pT%d", (lambda g4, b=b: hT[b][:, g4 * 4:(g4 + 1) * 4, :]), k("hT"), cnt0=tt * 4)
                for c4 in range(4):
                    pb = c4 % 2
                    cs_ = slice(c4 * 512, (c4 + 1) * 512)
                    for kc in range(16):
                        P.op("pe", lambda e, b=b, pb=pb, kc=kc, cs_=cs_: e.matmul(psG[pb][:], hT[b][:, kc, :], Wg[:, kc, cs_], start=(kc == 0), stop=(kc == 15)),
                             r=[k("hT"), ("l_Wg", 2 * c4), ("l_Wg", 2 * c4 + 1)], w=["l_psG%d" % pb])
                    for kc in range(2):
                        P.op("pe", lambda e, b=b, pb=pb, kc=kc, cs_=cs_: e.matmul(psP[pb][:], pTt[b][:, kc, :], Wp[:, kc, cs_], start=(kc == 0), stop=(kc == 1)),
                             r=[k("pTt"), ("l_Wp", c4 // 2)], w=["l_psP%d" % pb])
                    P.op("act", lambda e, pb=pb: e.activation(out=sg[:], in_=psG[pb][:], func=AF.Sigmoid), r=["l_psG%d" % pb], w=["l_sg"])
                    P.op("dve", lambda e, pb=pb: e.tensor_tensor(sg[:], sg[:], psP[pb][:], ALU.mult), r=["l_sg", "l_psP%d" % pb], w=["l_sg"])
                    P.op("dve", lambda e, b=b, cs_=cs_: e.tensor_tensor(hb[b][:, cs_], hb[b][:, cs_], sg[:], ALU.add), r=["l_sg", k("hb")], w=[k("hb")])
                P.dma("sp", self.h[t0:t0 + 128, :], hb[b][:], r=[k("hb")], w=["h"])
            P.barrier()

    def build(self):
        self.declare()
        P = self.P
        self.load_consts()
        self.pib = self.sb(self.stack, "pib", [128, 1], F32)
        P.op("pool", lambda e: e.memset(self.pib[:], math.pi), w=["pib"])
        self.p_posi = self.sb(self.stack, "m_posi", [128, (self.S // 128) * 16], I32)
        self.p_xr = [self.sb(self.stack, "m_dr%d" % i, [128, 2112], BF16) for i in range(2)]
        self.p_tid = self.sb(self.stack, "e_tid", [128, max(1, self.S // 1024)], I32)
        self.p_yrow = [self.sb(self.stack, "e_yrow%d" % i, [128, D], F32) for i in range(2)]
        self.epsb = self.sb(self.stack, "epsb", [128, 1], F32)
        P.op("pool", lambda e: e.memset(self.epsb[:], EPS), w=["epsb"])
        P.dma("sp", self.h[:, :], self.x[:, :], w=["h"])
        if self.stop != "init":
            self.rope_tables()
        for l in range(self.L):
            if self.stop in ("init", "rope"):
                break
            self.stage_a(l)
            if self.stop in ("a", "a1"):
                break
            self.stage_attn(l)
            if self.stop == "attn":
                break
            self.stage_s5(l)
            if self.stop in ("s5", "s5raw"):
                break
            self.stage_c1(l)
            self.stage_c2(l)
            if self.stop == "c":
                break
            self.stage_moe(l)
            if self.stop == "moe":
                break
            self.stage_ple(l)
        P.finish()
        P.emit()
        self.stack.close()
        return self.nc


SEQ = 8192
DEPTH = 4
N_CORES = 8


def kernel(**inputs):
    inp = {k: np.asarray(v) for k, v in inputs.items()}
    B = inp["x"].shape[0]
    m = MK(SEQ, DEPTH, dbg=False)
    nc = m.build()
    consts = make_consts()
    s5 = prep_s5(inp)
    shared = {}
    for name in ["norm_mix", "w_in", "q_norm", "k_norm", "w_attn_br", "w_ssm_br", "w_out", "norm_ffn", "w_router",
                 "w_exp_gate", "w_exp_up", "w_exp_down", "norm_ple", "w_ple_gate", "w_ple_proj"]:
        shared[name] = np.ascontiguousarray(inp[name], dtype=np.float32)
    shared.update(s5)
    for k, v in consts.items():
        shared["c_" + k] = v
    per_b = []
    for b in range(B):
        per_b.append({
            "x": np.ascontiguousarray(inp["x"][b], dtype=np.float32),
            "pT": np.ascontiguousarray(inp["p"][:, b].transpose(0, 2, 1), dtype=np.float32),
            "positions": np.ascontiguousarray(inp["positions"][b:b + 1], dtype=np.int32),
        })
    in_maps = []
    for c in range(N_CORES):
        b = (c * B) // N_CORES
        d = dict(shared)
        d.update(per_b[b])
        in_maps.append({k: v for k, v in d.items() if k in m.din})
    res = run_bass_kernel_spmd(nc, in_maps, core_ids=list(range(N_CORES)))
    outs = []
    for b in range(B):
        c = (b * N_CORES) // B
        outs.append(np.asarray(res.results[c]["h"], dtype=np.float32))
    return np.stack(outs, axis=0)
```
